# Optimizing a Trainium2 kernel written in Bass

```python
import math
import jax, jax.numpy as jnp
from jax import lax
import numpy as np

D_MODEL = 1024
BATCH = 2
SEQ = 16384
DEPTH = 2

SSM_GROUPS = 32
SSM_GROUP_CH = 16
SSM_WIDTH = SSM_GROUPS * SSM_GROUP_CH
SSM_STATE = 64
DT_MIN = 1e-3
DT_MAX = 1e-1
ATTN_HEADS = 4
ATTN_HEAD_DIM = 64
ATTN_V_DIM = 2 * ATTN_HEAD_DIM
QK_WIDTH = ATTN_HEADS * 2 * ATTN_HEAD_DIM
ATTN_WIDTH = ATTN_HEADS * ATTN_V_DIM
Q_BLOCK = 128
NEG_BIG = -1e30
IN_SIZES = (SSM_WIDTH, QK_WIDTH, QK_WIDTH, ATTN_WIDTH, D_MODEL, D_MODEL)
IN_WIDTH = sum(IN_SIZES)
IN_SPLITS = [int(s) for s in np.cumsum(IN_SIZES)[:-1]]
PEER_HEADS = 8
PEER_N_KEYS = 128
PEER_N_EXPERTS = PEER_N_KEYS * PEER_N_KEYS
PEER_KEY_DIM = 256
PEER_HALF = PEER_KEY_DIM // 2
PEER_TOPK = 16
PEER_TOKEN_BLOCK = 128
RMS_EPS = 1e-6

kernel_name = 'hybrid_s5_diffattn_peer_adaln'


def rms_norm(x, w):
    xf = x.astype(jnp.float32)
    y = xf * lax.rsqrt(jnp.mean(xf * xf, axis=-1, keepdims=True) + RMS_EPS)
    return (y * w.astype(jnp.float32)).astype(x.dtype)


def _complex_affine_combine(left, right):
    a1r, a1i, b1r, b1i = left
    a2r, a2i, b2r, b2i = right
    ar = a2r * a1r - a2i * a1i
    ai = a2r * a1i + a2i * a1r
    br = a2r * b1r - a2i * b1i + b2r
    bi = a2r * b1i + a2i * b1r + b2i
    return (ar, ai, br, bi)


def s5_branch(u, log_dt, a_re, a_im, b_re, b_im, c_re, c_im, d_skip, glu_w):
    f32 = jnp.float32
    bsz, s_len, _ = u.shape
    uf = u.astype(f32).reshape(bsz, s_len, SSM_GROUPS, SSM_GROUP_CH)
    dt = jnp.exp(log_dt.astype(f32))[:, None]
    lr = a_re.astype(f32)
    li = a_im.astype(f32)
    mag = jnp.exp(lr * dt)
    ab_re = mag * jnp.cos(li * dt)
    ab_im = mag * jnp.sin(li * dt)
    den = lr * lr + li * li
    nr = ab_re - 1.0
    f_re = (nr * lr + ab_im * li) / den
    f_im = (ab_im * lr - nr * li) / den
    br = b_re.astype(f32)
    bi = b_im.astype(f32)
    bb_re = f_re[..., None] * br - f_im[..., None] * bi
    bb_im = f_re[..., None] * bi + f_im[..., None] * br
    bu_re = jnp.einsum('bsgh,gph->sbgp', uf, bb_re)
    bu_im = jnp.einsum('bsgh,gph->sbgp', uf, bb_im)
    shp = (s_len, 1, SSM_GROUPS, SSM_STATE)
    a_r = jnp.broadcast_to(ab_re[None, None], shp)
    a_i = jnp.broadcast_to(ab_im[None, None], shp)
    _, _, xr, xi = lax.associative_scan(_complex_affine_combine, (a_r, a_i, bu_re, bu_im), axis=0)
    y = (jnp.einsum('sbgp,ghp->bsgh', xr, c_re.astype(f32))
         - jnp.einsum('sbgp,ghp->bsgh', xi, c_im.astype(f32))
         + d_skip.astype(f32) * uf)
    y = jax.nn.gelu(y.reshape(bsz, s_len, SSM_WIDTH))
    val, gate = jnp.split(y @ glu_w.astype(f32), 2, axis=-1)
    return (val * jax.nn.sigmoid(gate)).astype(u.dtype)


def diff_attention(q, k, v, q_norm_w, k_norm_w, lq1, lk1, lq2, lk2, subln_w, lambda_init):
    f32 = jnp.float32
    bsz, s_len, _ = q.shape
    q = rms_norm(q.reshape(bsz, s_len, ATTN_HEADS, 2, ATTN_HEAD_DIM), q_norm_w)
    k = rms_norm(k.reshape(bsz, s_len, ATTN_HEADS, 2, ATTN_HEAD_DIM), k_norm_w)
    vf = v.reshape(bsz, s_len, ATTN_HEADS, ATTN_V_DIM).astype(f32)
    kf = k.astype(f32)
    lam = (jnp.exp(jnp.sum(lq1.astype(f32) * lk1.astype(f32)))
           - jnp.exp(jnp.sum(lq2.astype(f32) * lk2.astype(f32))) + lambda_init)
    scale = ATTN_HEAD_DIM ** -0.5
    n_blk = s_len // Q_BLOCK
    qb = q.astype(f32).reshape(bsz, n_blk, Q_BLOCK, ATTN_HEADS, 2, ATTN_HEAD_DIM).transpose(1, 0, 2, 3, 4, 5)
    key_pos = jnp.arange(s_len)

    def block(args):
        q_blk, blk_idx = args
        s = jnp.einsum('bqhcd,bkhcd->bhcqk', q_blk, kf) * scale
        q_pos = blk_idx * Q_BLOCK + jnp.arange(Q_BLOCK)
        causal = key_pos[None, :] <= q_pos[:, None]
        s = jnp.where(causal, s, NEG_BIG)
        p = jax.nn.softmax(s, axis=-1)
        w = p[:, :, 0] - lam * p[:, :, 1]
        return jnp.einsum('bhqk,bkhd->bqhd', w, vf)

    o = lax.map(block, (qb, jnp.arange(n_blk)))
    o = o.transpose(1, 0, 2, 3, 4).reshape(bsz, s_len, ATTN_HEADS, ATTN_V_DIM)
    o = rms_norm(o, subln_w) * (1.0 - lambda_init)
    return o.reshape(bsz, s_len, ATTN_WIDTH).astype(v.dtype)


def peer_ffn(h, w_q, sub_k1, sub_k2, u_tab, v_tab):
    f32 = jnp.float32
    bsz, s_len, d = h.shape
    n_tok = bsz * s_len
    n_blk = n_tok // PEER_TOKEN_BLOCK
    hb = h.reshape(n_blk, PEER_TOKEN_BLOCK, d)

    def block(xb):
        q = (xb @ w_q).reshape(PEER_TOKEN_BLOCK, PEER_HEADS, 2, PEER_HALF)
        s1 = jnp.einsum('thd,nd->thn', q[:, :, 0], sub_k1).astype(f32)
        s2 = jnp.einsum('thd,nd->thn', q[:, :, 1], sub_k2).astype(f32)
        v1, i1 = lax.top_k(s1, PEER_TOPK)
        v2, i2 = lax.top_k(s2, PEER_TOPK)
        cand = (v1[..., :, None] + v2[..., None, :]).reshape(PEER_TOKEN_BLOCK, PEER_HEADS, PEER_TOPK * PEER_TOPK)
        sc, ci = lax.top_k(cand, PEER_TOPK)
        e1 = jnp.take_along_axis(i1, ci // PEER_TOPK, axis=-1)
        e2 = jnp.take_along_axis(i2, ci % PEER_TOPK, axis=-1)
        expert = e1 * PEER_N_KEYS + e2
        g = jax.nn.softmax(sc, axis=-1)
        u_sel = u_tab[expert]
        act = jax.nn.gelu(jnp.einsum('thkd,td->thk', u_sel, xb).astype(f32))
        v_sel = v_tab[expert]
        return jnp.einsum('thk,thkd->td', (g * act).astype(v_sel.dtype), v_sel)

    out = lax.map(block, hb)
    return out.reshape(bsz, s_len, d).astype(h.dtype)


def setup_inputs(seed: int = 0) -> dict:
    key = jax.random.key(seed)
    ks = jax.random.split(key, 32)
    L, D = DEPTH, D_MODEL
    G, H, P = SSM_GROUPS, SSM_GROUP_CH, SSM_STATE
    nrm = lambda k, shp, s: jax.random.normal(k, shp, jnp.float32) * s
    return {
        'x': nrm(ks[0], (BATCH, SEQ, D), 1.0),
        'c': nrm(ks[1], (BATCH, D), 1.0),
        'ada_w': nrm(ks[2], (L, D, 6 * D), 0.5 * D ** -0.5),
        'ada_b': nrm(ks[3], (L, 6 * D), 0.02),
        'norm1_w': 1.0 + nrm(ks[4], (L, D), 0.02),
        'norm2_w': 1.0 + nrm(ks[5], (L, D), 0.02),
        'w_in': nrm(ks[6], (L, D, IN_WIDTH), D ** -0.5),
        'ssm_log_dt': jax.random.uniform(ks[7], (L, G), jnp.float32, math.log(DT_MIN), math.log(DT_MAX)),
        'ssm_a_re': -0.5 + nrm(ks[8], (L, G, P), 0.01),
        'ssm_a_im': jnp.pi * jnp.arange(P, dtype=jnp.float32)[None, None, :] + nrm(ks[9], (L, G, P), 0.01),
        'ssm_b_re': nrm(ks[10], (L, G, P, H), (2 * H) ** -0.5),
        'ssm_b_im': nrm(ks[11], (L, G, P, H), (2 * H) ** -0.5),
        'ssm_c_re': nrm(ks[12], (L, G, H, P), (2 * P) ** -0.5),
        'ssm_c_im': nrm(ks[13], (L, G, H, P), (2 * P) ** -0.5),
        'ssm_d': nrm(ks[14], (L, G, H), 1.0),
        'ssm_glu_w': nrm(ks[15], (L, SSM_WIDTH, 2 * D), SSM_WIDTH ** -0.5),
        'q_norm_w': 1.0 + nrm(ks[16], (L, ATTN_HEAD_DIM), 0.02),
        'k_norm_w': 1.0 + nrm(ks[17], (L, ATTN_HEAD_DIM), 0.02),
        'lambda_q1': nrm(ks[18], (L, ATTN_HEAD_DIM), 0.1),
        'lambda_k1': nrm(ks[19], (L, ATTN_HEAD_DIM), 0.1),
        'lambda_q2': nrm(ks[20], (L, ATTN_HEAD_DIM), 0.1),
        'lambda_k2': nrm(ks[21], (L, ATTN_HEAD_DIM), 0.1),
        'subln_w': 1.0 + nrm(ks[22], (L, ATTN_V_DIM), 0.02),
        'attn_up_w': nrm(ks[23], (L, ATTN_WIDTH, D), ATTN_WIDTH ** -0.5),
        'w_out': nrm(ks[24], (L, D, D), D ** -0.5),
        'peer_wq': nrm(ks[25], (L, D, PEER_HEADS * PEER_KEY_DIM), D ** -0.5),
        'peer_k1': nrm(ks[26], (L, PEER_N_KEYS, PEER_HALF), PEER_HALF ** -0.5),
        'peer_k2': nrm(ks[27], (L, PEER_N_KEYS, PEER_HALF), PEER_HALF ** -0.5),
        'peer_u': nrm(ks[28], (L, PEER_N_EXPERTS, D), D ** -0.5),
        'peer_v': nrm(ks[29], (L, PEER_N_EXPERTS, D), PEER_HEADS ** -0.5),
    }


def reference(x, c, ada_w, ada_b, norm1_w, norm2_w, w_in, ssm_log_dt, ssm_a_re, ssm_a_im,
              ssm_b_re, ssm_b_im, ssm_c_re, ssm_c_im, ssm_d, ssm_glu_w, q_norm_w, k_norm_w,
              lambda_q1, lambda_k1, lambda_q2, lambda_k2, subln_w, attn_up_w, w_out,
              peer_wq, peer_k1, peer_k2, peer_u, peer_v):
    c_act = jax.nn.silu(c)
    for l in range(DEPTH):
        mod = c_act @ ada_w[l] + ada_b[l]
        sh1, sc1, g1, sh2, sc2, g2 = jnp.split(mod, 6, axis=-1)
        h = rms_norm(x, norm1_w[l]) * (1.0 + sc1[:, None, :]) + sh1[:, None, :]
        z = h @ w_in[l]
        u_ssm, q, k, v, gate_ssm, gate_attn = jnp.split(z, IN_SPLITS, axis=-1)
        y_ssm = s5_branch(u_ssm, ssm_log_dt[l], ssm_a_re[l], ssm_a_im[l], ssm_b_re[l], ssm_b_im[l],
                          ssm_c_re[l], ssm_c_im[l], ssm_d[l], ssm_glu_w[l])
        lambda_init = 0.8 - 0.6 * math.exp(-0.3 * l)
        y_att = diff_attention(q, k, v, q_norm_w[l], k_norm_w[l], lambda_q1[l], lambda_k1[l],
                               lambda_q2[l], lambda_k2[l], subln_w[l], lambda_init)
        y_att = y_att @ attn_up_w[l]
        merged = jax.nn.sigmoid(gate_ssm) * y_ssm + jax.nn.sigmoid(gate_attn) * y_att
        x = x + g1[:, None, :] * (merged @ w_out[l])
        h2 = rms_norm(x, norm2_w[l]) * (1.0 + sc2[:, None, :]) + sh2[:, None, :]
        x = x + g2[:, None, :] * peer_ffn(h2, peer_wq[l], peer_k1[l], peer_k2[l], peer_u[l], peer_v[l])
    return x
```

```python
import math
import numpy as np
import ml_dtypes
from contextlib import ExitStack
import concourse.bass as bass
import concourse.mybir as mybir
from concourse.bass_utils import run_bass_kernel_spmd

F32 = mybir.dt.float32
BF16 = mybir.dt.bfloat16
U32 = mybir.dt.uint32
I32 = mybir.dt.int32
AF = mybir.ActivationFunctionType
ALU = mybir.AluOpType
AX = mybir.AxisListType
NPBF = ml_dtypes.bfloat16


class Prog:
    ENG = ('pe', 'act', 'dve', 'pool', 'sp')

    def __init__(self, nc):
        self.nc = nc
        self.es = ExitStack()
        self.ops = {e: [] for e in self.ENG}
        self.cnt = {e: 0 for e in self.ENG}
        self.sem = {}
        self.dcnt = {}
        self.lastw = {}
        self.rd = {}
        self.waited = {e: {} for e in self.ENG}
        self.nuniq = 0

    def _sem(self, name):
        if name not in self.sem:
            self.sem[name] = self.es.enter_context(self.nc.semaphore(name))
        return self.sem[name]

    def sb(self, name, shape, dt):
        return self.es.enter_context(self.nc.sbuf_tensor(name, shape, dt))

    def ps(self, name, shape, dt=F32):
        return self.es.enter_context(self.nc.psum_tensor(name, shape, dt))

    def op(self, eng, fn, reads=(), writes=(), dma=None):
        waits = {}

        def need(tok, hazard):
            s, v, e, isdma = tok
            if not isdma and dma is None and e == eng:
                if hazard != 'raw' or eng == 'pe':
                    return
            if waits.get(s, 0) < v:
                waits[s] = v

        for r in reads:
            if r in self.lastw:
                need(self.lastw[r], 'raw')
        for w in writes:
            if w in self.lastw:
                need(self.lastw[w], 'waw')
            for s, tok in self.rd.get(w, {}).items():
                need(tok, 'war')
        wl = []
        for s, v in waits.items():
            if self.waited[eng].get(s, 0) < v:
                self.waited[eng][s] = v
                wl.append((s, v))
        if dma is None:
            sname = 'e_' + eng
            self._sem(sname)
            self.cnt[eng] += 1
            tok = (sname, self.cnt[eng], eng, False)
            inc = (sname, 1)
        else:
            sname = 'd_' + dma
            self._sem(sname)
            self.dcnt[sname] = self.dcnt.get(sname, 0) + 16
            tok = (sname, self.dcnt[sname], eng, True)
            inc = (sname, 16)
        for w in writes:
            self.lastw[w] = tok
            self.rd[w] = {}
        for r in reads:
            d = self.rd.setdefault(r, {})
            d[tok[0]] = tok
        self.ops[eng].append((wl, fn, inc))

    def finish(self):
        wl = [(s, v) for s, v in self.dcnt.items()]
        self.ops['sp'].append((wl, None, None))

    def emit(self):
        nc = self.nc
        P = self

        def run(name):
            def f(e):
                for wl, fn, inc in P.ops[name]:
                    for s, v in wl:
                        e.wait_ge(P.sem[s], v)
                    if fn is not None:
                        ins = fn(e)
                        ins.then_inc(P.sem[inc[0]], inc[1])
            return f

        with nc.Block() as block:
            block.tensor(run('pe'))
            block.scalar(run('act'))
            block.vector(run('dve'))
            block.gpsimd(run('pool'))
            block.sync(run('sp'))
        self.es.close()


T = 4096
NB = T // 512


def build_A():
    nc = bass.Bass("TRN2", target_bir_lowering=False)
    P = Prog(nc)
    D = lambda n, s, dt, k: nc.dram_tensor(n, s, dt, kind=k).ap()
    xT = D("xT", [1024, T], F32, "ExternalInput")
    cb = D("cb", [128, 8], F32, "ExternalInput")
    adaw = D("adaw", [1024, 2048], F32, "ExternalInput")
    adab = D("adab", [128, 16], F32, "ExternalInput")
    n1w = D("n1w", [128, 8], F32, "ExternalInput")
    win = D("win", [1024, 4096], F32, "ExternalInput")
    qkw = D("qkw", [128, 2], F32, "ExternalInput")
    uT = D("uT", [512, T], F32, "ExternalOutput")
    qT = D("qT", [512, T], BF16, "ExternalOutput")
    kT = D("kT", [512, T], BF16, "ExternalOutput")
    vT = D("vT", [512, T], BF16, "ExternalOutput")
    gT = D("gT", [2048, T], F32, "ExternalOutput")

    wbf = P.sb("wbf", [128, 8, 4096], BF16)
    stg = [P.sb(f"stg{i}", [128, 2048], F32) for i in range(2)]
    c_sb = P.sb("c_sb", [128, 8], F32)
    ca_sb = P.sb("ca_sb", [128, 8], F32)
    adab_sb = P.sb("adab_sb", [128, 16], F32)
    n1w_sb = P.sb("n1w_sb", [128, 8], F32)
    qkw_sb = P.sb("qkw_sb", [128, 2], F32)
    modT = P.sb("modT", [128, 16], F32)
    wmod = P.sb("wmod", [128, 8], F32)
    ones = P.sb("ones", [128, 128], F32)
    blk = P.sb("blk", [128, 128], F32)
    epsb = P.sb("epsb", [128, 1], F32)
    xb = [P.sb(f"xb{i}", [128, 8, 512], F32) for i in range(2)]
    sq = [P.sb(f"sq{i}", [128, 512], F32) for i in range(2)]
    rstd = P.sb("rstd", [128, 512], F32)
    tmp = [P.sb(f"tmp{i}", [128, 512], F32) for i in range(2)]
    hT = P.sb("hT", [128, 8, 512], BF16)
    of32 = [P.sb(f"of32_{i}", [128, 512], F32) for i in range(4)]
    obf = [P.sb(f"obf_{i}", [128, 512], BF16) for i in range(4)]
    sq2 = [P.sb(f"sq2_{i}", [128, 512], F32) for i in range(2)]
    r2 = [P.sb(f"r2_{i}", [128, 512], F32) for i in range(2)]
    pz = [P.ps(f"pz{i}", [128, 512]) for i in range(4)]
    pss = [P.ps(f"pss{i}", [128, 512]) for i in range(2)]
    pn = P.ps("pn", [128, 512])
    pm = P.ps("pm", [128, 16])

    P.op('pool', lambda e: e.memset(ones[:], 1.0), writes=['ones'])
    P.op('pool', lambda e: e.memset(blk[:], 0.0), writes=['blk'])
    P.op('pool', lambda e: e.memset(blk[0:64, 0:64], 1.0), writes=['blk'])
    P.op('pool', lambda e: e.memset(blk[64:128, 64:128], 1.0), writes=['blk'])
    P.op('pool', lambda e: e.memset(epsb[:], 1e-6), writes=['epsb'])
    for nm, dst, src in (('c', c_sb, cb), ('adab', adab_sb, adab), ('n1w', n1w_sb, n1w), ('qkw', qkw_sb, qkw)):
        P.op('sp', (lambda dst, src: lambda e: e.dma_start(out=dst[:], in_=src))(dst, src), writes=[nm], dma='small')
    P.op('act', lambda e: e.activation(out=ca_sb[:], in_=c_sb[:], func=AF.Silu), reads=['c'], writes=['ca'])

    for q4 in range(4):
        xs = q4 % 2
        P.op('sp', (lambda xs, q4: lambda e: e.dma_start(out=xb[xs][:], in_=adaw[:, q4 * 512:(q4 + 1) * 512].rearrange("(kc p) t -> p kc t", p=128)))(xs, q4),
             writes=[('xb', xs)], dma=f'xb{xs}')
        for mm in range(4):
            m = q4 * 4 + mm
            for kc in range(8):
                P.op('pe', (lambda xs, kc, m, mm: lambda e: e.matmul(pm[:, m:m + 1], lhsT=xb[xs][:, kc, mm * 128:(mm + 1) * 128],
                                                                  rhs=ca_sb[:, kc:kc + 1], start=(kc == 0), stop=(kc == 7)))(xs, kc, m, mm),
                     reads=[('xb', xs), 'ca'], writes=['pm'])
    P.op('dve', lambda e: e.tensor_tensor(out=modT[:], in0=pm[:], in1=adab_sb[:], op=ALU.add),
         reads=['pm', 'adab'], writes=['modT'])
    P.op('dve', lambda e: e.scalar_tensor_tensor(out=wmod[:], in0=modT[:, 8:16], scalar=1.0, in1=n1w_sb[:],
                                                op0=ALU.add, op1=ALU.mult),
         reads=['modT', 'n1w'], writes=['wmod'])

    for kc in range(8):
        for hh in range(2):
            s = (kc * 2 + hh) % 2
            P.op('sp', (lambda s, kc, hh: lambda e: e.dma_start(out=stg[s][:], in_=win[kc * 128:(kc + 1) * 128, hh * 2048:(hh + 1) * 2048]))(s, kc, hh),
                 writes=[('stg', s)], dma=f'stg{s}')
            P.op('pool', (lambda s, kc, hh: lambda e: e.tensor_copy(out=wbf[:, kc, hh * 2048:(hh + 1) * 2048], in_=stg[s][:]))(s, kc, hh),
                 reads=[('stg', s)], writes=[('wbf', kc, hh)])

    oi = 0
    for tb in range(NB):
        xs = tb % 2
        tsl = slice(tb * 512, (tb + 1) * 512)
        P.op('sp', (lambda xs, tsl: lambda e: e.dma_start(out=xb[xs][:], in_=xT[:, tsl].rearrange("(kc p) t -> p kc t", p=128)))(xs, tsl),
             writes=[('xb', xs)], dma=f'xb{xs}')
        for kc in range(8):
            s = kc % 2
            P.op('act', (lambda xs, kc, s: lambda e: e.activation(out=sq[s][:], in_=xb[xs][:, kc, :], func=AF.Square))(xs, kc, s),
                 reads=[('xb', xs)], writes=[('sq', s)])
            P.op('pe', (lambda kc, s: lambda e: e.matmul(pn[:], lhsT=ones[:], rhs=sq[s][:], start=(kc == 0), stop=(kc == 7)))(kc, s),
                 reads=[('sq', s), 'ones'], writes=['pn'])
        P.op('act', lambda e: e.activation(out=rstd[:], in_=pn[:], func=AF.Sqrt, bias=epsb[:], scale=1.0 / 1024.0),
             reads=['pn', 'epsb'], writes=['rstd'])
        P.op('dve', lambda e: e.reciprocal(out=rstd[:], in_=rstd[:]), reads=['rstd'], writes=['rstd'])
        for kc in range(8):
            s = kc % 2
            P.op('dve', (lambda xs, kc, s: lambda e: e.tensor_tensor(out=tmp[s][:], in0=xb[xs][:, kc, :], in1=rstd[:], op=ALU.mult))(xs, kc, s),
                 reads=[('xb', xs), 'rstd'], writes=[('tmp', s)])
            P.op('act', (lambda kc, s: lambda e: e.activation(out=hT[:, kc, :], in_=tmp[s][:], func=AF.Identity,
                                                             bias=modT[:, kc:kc + 1], scale=wmod[:, kc:kc + 1]))(kc, s),
                 reads=[('tmp', s), 'modT', 'wmod'], writes=[('hT', kc)])
        for m in range(32):
            zs = m % 4
            for kc in range(8):
                P.op('pe', (lambda zs, kc, m: lambda e: e.matmul(pz[zs][:], lhsT=wbf[:, kc, m * 128:(m + 1) * 128], rhs=hT[:, kc, :],
                                                                start=(kc == 0), stop=(kc == 7)))(zs, kc, m),
                     reads=[('hT', kc), ('wbf', kc, m // 16)], writes=[('pz', zs)])
            o = oi % 4
            oi += 1
            msl = slice((m % 4) * 128, (m % 4 + 1) * 128)
            if m < 4:
                P.op('dve', (lambda zs, o: lambda e: e.tensor_copy(out=of32[o][:], in_=pz[zs][:]))(zs, o),
                     reads=[('pz', zs)], writes=[('of32', o)])
                P.op('sp', (lambda o, msl, tsl: lambda e: e.dma_start(out=uT[msl, tsl], in_=of32[o][:]))(o, msl, tsl),
                     reads=[('of32', o)], dma=f'of32_{o}')
            elif m < 12:
                s = m % 2
                wcol = 0 if m < 8 else 1
                dst = qT if m < 8 else kT
                P.op('act', (lambda zs, s: lambda e: e.activation(out=sq2[s][:], in_=pz[zs][:], func=AF.Square))(zs, s),
                     reads=[('pz', zs)], writes=[('sq2', s)])
                P.op('pe', (lambda s: lambda e: e.matmul(pss[s][:], lhsT=blk[:], rhs=sq2[s][:], start=True, stop=True))(s),
                     reads=[('sq2', s), 'blk'], writes=[('pss', s)])
                P.op('act', (lambda s: lambda e: e.activation(out=r2[s][:], in_=pss[s][:], func=AF.Sqrt, bias=epsb[:], scale=1.0 / 64.0))(s),
                     reads=[('pss', s), 'epsb'], writes=[('r2', s)])
                P.op('dve', (lambda s: lambda e: e.reciprocal(out=r2[s][:], in_=r2[s][:]))(s), reads=[('r2', s)], writes=[('r2', s)])
                P.op('dve', (lambda zs, s, o, wcol: lambda e: e.scalar_tensor_tensor(out=obf[o][:], in0=pz[zs][:], scalar=qkw_sb[:, wcol:wcol + 1],
                                                                                    in1=r2[s][:], op0=ALU.mult, op1=ALU.mult))(zs, s, o, wcol),
                     reads=[('pz', zs), ('r2', s), 'qkw'], writes=[('obf', o)])
                P.op('sp', (lambda o, msl, tsl, dst: lambda e: e.dma_start(out=dst[msl, tsl], in_=obf[o][:]))(o, msl, tsl, dst),
                     reads=[('obf', o)], dma=f'obf_{o}')
            elif m < 16:
                P.op('act', (lambda zs, o: lambda e: e.activation(out=obf[o][:], in_=pz[zs][:], func=AF.Identity))(zs, o),
                     reads=[('pz', zs)], writes=[('obf', o)])
                P.op('sp', (lambda o, msl, tsl: lambda e: e.dma_start(out=vT[msl, tsl], in_=obf[o][:]))(o, msl, tsl),
                     reads=[('obf', o)], dma=f'obf_{o}')
            else:
                gsl = slice((m - 16) * 128, (m - 15) * 128)
                P.op('act', (lambda zs, o: lambda e: e.activation(out=of32[o][:], in_=pz[zs][:], func=AF.Sigmoid))(zs, o),
                     reads=[('pz', zs)], writes=[('of32', o)])
                P.op('sp', (lambda o, gsl, tsl: lambda e: e.dma_start(out=gT[gsl, tsl], in_=of32[o][:]))(o, gsl, tsl),
                     reads=[('of32', o)], dma=f'of32_{o}')
    P.finish()
    P.emit()
    return nc


def layout_A(inp, l, r, xT_full):
    b, j = r // 4, r % 4
    f = lambda a: np.ascontiguousarray(a, dtype=np.float32)
    qn = np.tile(inp['q_norm_w'][l], 2)
    kn = np.tile(inp['k_norm_w'][l], 2)
    return {
        "xT": f(xT_full[b][:, j * T:(j + 1) * T]),
        "cb": f(inp['c'][b].reshape(8, 128).T),
        "adaw": f(inp['ada_w'][l][:, 0:2048]),
        "adab": f(inp['ada_b'][l][0:2048].reshape(16, 128).T),
        "n1w": f(inp['norm1_w'][l].reshape(8, 128).T),
        "win": f(inp['w_in'][l]),
        "qkw": f(np.stack([qn, kn], axis=1)),
    }


S = 16384
L = 1024
NSEG = S // L
PI = math.pi


def build_B1(nseg=NSEG):
    nc = bass.Bass("TRN2", target_bir_lowering=False)
    P = Prog(nc)
    D = lambda n, s, dt, k: nc.dram_tensor(n, s, dt, kind=k).ap()
    uT = D("uT", [128, S], F32, "ExternalInput")
    prm = D("prm", [128, 12], F32, "ExternalInput")
    bpad = D("bpad", [128, 8, 128], F32, "ExternalInput")
    cpad = D("cpad", [128, 8, 128], F32, "ExternalInput")
    dsk = D("dsk", [128, 1], F32, "ExternalInput")
    yT = D("yT", [128, S], BF16, "ExternalOutput")

    prm_sb = P.sb("prm_sb", [128, 12], F32)
    bpad_sb = P.sb("bpad_sb", [128, 8, 128], F32)
    cpad_sb = P.sb("cpad_sb", [128, 8, 128], F32)
    dsk_sb = P.sb("dsk_sb", [128, 1], F32)
    sm = P.sb("sm", [128, 64], F32)
    npi = P.sb("npi", [128, 1], F32)
    qi_sb = P.sb("qi_sb", [128, 4], I32)
    ident = P.sb("ident", [128, 128], F32)
    iot = P.sb("iot", [128, 128], F32)
    bbp = P.sb("bbp", [128, 8, 128], F32)
    btmp = P.sb("btmp", [128, 128], F32)
    W = P.sb("W", [128, 8, 128], F32)
    Ec = P.sb("Ec", [128, 4, L], F32)
    Es = P.sb("Es", [128, 4, L], F32)
    etmp = P.sb("etmp", [128, L], F32)
    rot = P.sb("rot", [128, 8], F32)
    carry = P.sb("carry", [128, 8], F32)
    ctmp = P.sb("ctmp", [128, 4], F32)
    ub = [P.sb(f"ub{i}", [128, L], F32) for i in range(2)]
    m = [[P.sb(f"m{k}_{i}", [128, 512], F32) for i in range(2)] for k in range(4)]
    win_r = P.sb("win_r", [128, L], F32)
    win_i = P.sb("win_i", [128, L], F32)
    w_r = P.sb("w_r", [128, L], F32)
    w_i = P.sb("w_i", [128, L], F32)
    dd = [P.sb(f"dd{k}", [128, L], F32) for k in range(4)]
    xr = P.sb("xr", [128, 4, L], F32)
    xi = P.sb("xi", [128, 4, L], F32)
    ysb = [P.sb(f"ysb{i}", [128, 512], F32) for i in range(2)]
    gt = [P.sb(f"gt{i}", [128, 512], F32) for i in range(2)]
    gs = [P.sb(f"gs{i}", [128, 512], F32) for i in range(2)]
    yo = [P.sb(f"yo{i}", [128, 512], BF16) for i in range(2)]
    psr = [P.ps(f"psr{i}", [128, 512]) for i in range(2)]
    psi = [P.ps(f"psi{i}", [128, 512]) for i in range(2)]
    psy = [P.ps(f"psy{i}", [128, 512]) for i in range(2)]
    pst = P.ps("pst", [128, 128])

    col = lambda k: sm[:, k:k + 1]
    for nm, dst, src in (('prm', prm_sb, prm), ('bpad', bpad_sb, bpad), ('cpad', cpad_sb, cpad), ('dsk', dsk_sb, dsk)):
        P.op('sp', (lambda dst, src: lambda e: e.dma_start(out=dst[:], in_=src))(dst, src), writes=[nm], dma='small')
    P.op('pool', lambda e: e.memset(npi[:], -PI), writes=['npi'])
    P.op('pool', lambda e: e.memset(carry[:], 0.0), writes=['carry'])
    P.op('pool', lambda e: e.iota(iot[:], pattern=[[1, 128]], base=0, channel_multiplier=-1, allow_small_or_imprecise_dtypes=True), writes=['iot'])
    P.op('dve', lambda e: e.tensor_single_scalar(out=ident[:], in_=iot[:], scalar=0.0, op=ALU.is_equal), reads=['iot'], writes=['ident'])
    P.op('act', lambda e: e.mul(out=cpad_sb[:, 4:8, :], in_=cpad_sb[:, 4:8, :], mul=-1.0), reads=['cpad'], writes=['cpad'])

    c4 = lambda k: sm[:, k:k + 4]
    ldt, are, aim = prm_sb[:, 0:4], prm_sb[:, 4:8], prm_sb[:, 8:12]
    seq = []
    def dv(fn, reads=('sm',), writes=('sm',)):
        P.op('dve', fn, reads=list(reads) + ['prm'], writes=list(writes))
    def ac(fn):
        P.op('act', fn, reads=['sm', 'prm', 'npi'], writes=['sm'])
    ac(lambda e: e.activation(out=c4(0), in_=ldt, func=AF.Exp))
    dv(lambda e: e.tensor_tensor(out=c4(44), in0=are, in1=c4(0), op=ALU.mult))
    ac(lambda e: e.activation(out=c4(4), in_=c4(44), func=AF.Exp))
    dv(lambda e: e.tensor_tensor(out=c4(8), in0=aim, in1=c4(0), op=ALU.mult))
    def red_sin(dst, shift):
        dv(lambda e: e.tensor_scalar_add(out=c4(52), in0=c4(8), scalar1=shift))
        dv(lambda e: e.tensor_scalar_mul(out=c4(44), in0=c4(52), scalar1=1.0 / (2 * PI)))
        P.op('dve', lambda e: e.tensor_copy(out=qi_sb[:], in_=c4(44)), reads=['sm'], writes=['qi'])
        P.op('dve', lambda e: e.tensor_copy(out=c4(44), in_=qi_sb[:]), reads=['qi'], writes=['sm'])
        dv(lambda e: e.scalar_tensor_tensor(out=c4(48), in0=c4(44), scalar=-2 * PI, in1=c4(52), op0=ALU.mult, op1=ALU.add))
        dv(lambda e: e.tensor_single_scalar(out=c4(44), in_=c4(48), scalar=PI, op=ALU.is_gt))
        dv(lambda e: e.scalar_tensor_tensor(out=c4(48), in0=c4(44), scalar=-2 * PI, in1=c4(48), op0=ALU.mult, op1=ALU.add))
        dv(lambda e: e.tensor_single_scalar(out=c4(44), in_=c4(48), scalar=-PI, op=ALU.is_lt))
        dv(lambda e: e.scalar_tensor_tensor(out=c4(48), in0=c4(44), scalar=2 * PI, in1=c4(48), op0=ALU.mult, op1=ALU.add))
        ac(lambda e: e.activation(out=c4(dst), in_=c4(48), func=AF.Sin))
    red_sin(16, 0.0)
    red_sin(12, 0.5 * PI)
    dv(lambda e: e.tensor_tensor(out=c4(20), in0=c4(4), in1=c4(12), op=ALU.mult))
    dv(lambda e: e.tensor_tensor(out=c4(24), in0=c4(4), in1=c4(16), op=ALU.mult))
    dv(lambda e: e.tensor_tensor(out=c4(44), in0=are, in1=are, op=ALU.mult))
    dv(lambda e: e.tensor_tensor(out=c4(48), in0=aim, in1=aim, op=ALU.mult))
    dv(lambda e: e.tensor_tensor(out=c4(28), in0=c4(44), in1=c4(48), op=ALU.add))
    dv(lambda e: e.reciprocal(out=c4(28), in_=c4(28)))
    dv(lambda e: e.tensor_scalar_add(out=c4(32), in0=c4(20), scalar1=-1.0))
    dv(lambda e: e.tensor_tensor(out=c4(44), in0=c4(32), in1=are, op=ALU.mult))
    dv(lambda e: e.tensor_tensor(out=c4(48), in0=c4(24), in1=aim, op=ALU.mult))
    dv(lambda e: e.tensor_tensor(out=c4(44), in0=c4(44), in1=c4(48), op=ALU.add))
    dv(lambda e: e.tensor_tensor(out=c4(36), in0=c4(44), in1=c4(28), op=ALU.mult))
    dv(lambda e: e.tensor_tensor(out=c4(44), in0=c4(24), in1=are, op=ALU.mult))
    dv(lambda e: e.tensor_tensor(out=c4(48), in0=c4(32), in1=aim, op=ALU.mult))
    dv(lambda e: e.tensor_tensor(out=c4(44), in0=c4(44), in1=c4(48), op=ALU.subtract))
    dv(lambda e: e.tensor_tensor(out=c4(40), in0=c4(44), in1=c4(28), op=ALU.mult))

    for gp in range(4):
        fre, fim = col(36 + gp), col(40 + gp)
        P.op('dve', (lambda gp, fim: lambda e: e.tensor_scalar_mul(out=btmp[:], in0=bpad_sb[:, 4 + gp, :], scalar1=fim))(gp, fim),
             reads=['sm', 'bpad'], writes=['btmp'])
        P.op('dve', (lambda gp, fre: lambda e: e.scalar_tensor_tensor(out=bbp[:, gp, :], in0=bpad_sb[:, gp, :], scalar=fre, in1=btmp[:],
                                                                      op0=ALU.mult, op1=ALU.subtract))(gp, fre),
             reads=['sm', 'bpad', 'btmp'], writes=[('bbp', gp)])
        P.op('dve', (lambda gp, fim: lambda e: e.tensor_scalar_mul(out=btmp[:], in0=bpad_sb[:, gp, :], scalar1=fim))(gp, fim),
             reads=['sm', 'bpad'], writes=['btmp'])
        P.op('dve', (lambda gp, fre: lambda e: e.scalar_tensor_tensor(out=bbp[:, 4 + gp, :], in0=bpad_sb[:, 4 + gp, :], scalar=fre, in1=btmp[:],
                                                                      op0=ALU.mult, op1=ALU.add))(gp, fre),
             reads=['sm', 'bpad', 'btmp'], writes=[('bbp', 4 + gp)])
        for k in (gp, 4 + gp):
            P.op('pe', (lambda k: lambda e: e.transpose(out=pst[:], in_=bbp[:, k, :], identity=ident[:]))(k),
                 reads=[('bbp', k), 'ident'], writes=['pst'])
            P.op('act', (lambda k: lambda e: e.copy(out=W[:, k, :], in_=pst[:]))(k), reads=['pst'], writes=[('W', k)])

    for gp in range(4):
        c1, s1 = col(12 + gp), col(16 + gp)
        ec, es = Ec[:, gp, :], Es[:, gp, :]
        tg = ('E', gp)
        P.op('pool', (lambda ec: lambda e: e.memset(ec[:, 0:1], 1.0))(ec), writes=[tg])
        P.op('pool', (lambda es: lambda e: e.memset(es[:, 0:1], 0.0))(es), writes=[tg])
        P.op('dve', (lambda ec, c1: lambda e: e.tensor_copy(out=ec[:, 1:2], in_=c1))(ec, c1), reads=['sm', tg], writes=[tg])
        P.op('dve', (lambda es, s1: lambda e: e.tensor_copy(out=es[:, 1:2], in_=s1))(es, s1), reads=['sm', tg], writes=[tg])
        n = 2
        while n <= L:
            P.op('dve', (lambda es, n, s1: lambda e: e.tensor_tensor(out=ctmp[:, 2:3], in0=es[:, n - 1:n], in1=s1, op=ALU.mult))(es, n, s1),
                 reads=[tg, 'sm'], writes=['ctmp'])
            P.op('dve', (lambda ec, n, c1: lambda e: e.scalar_tensor_tensor(out=ctmp[:, 0:1], in0=ec[:, n - 1:n], scalar=c1, in1=ctmp[:, 2:3],
                                                                           op0=ALU.mult, op1=ALU.subtract))(ec, n, c1),
                 reads=[tg, 'sm', 'ctmp'], writes=['ctmp'])
            P.op('dve', (lambda es, n, c1: lambda e: e.tensor_tensor(out=ctmp[:, 2:3], in0=es[:, n - 1:n], in1=c1, op=ALU.mult))(es, n, c1),
                 reads=[tg, 'sm', 'ctmp'], writes=['ctmp'])
            P.op('dve', (lambda ec, n, s1: lambda e: e.scalar_tensor_tensor(out=ctmp[:, 1:2], in0=ec[:, n - 1:n], scalar=s1, in1=ctmp[:, 2:3],
                                                                           op0=ALU.mult, op1=ALU.add))(ec, n, s1),
                 reads=[tg, 'sm', 'ctmp'], writes=['ctmp'])
            if n == L:
                P.op('dve', (lambda gp: lambda e: e.tensor_copy(out=rot[:, gp:gp + 1], in_=ctmp[:, 0:1]))(gp), reads=['ctmp'], writes=['rot'])
                P.op('dve', (lambda gp: lambda e: e.tensor_copy(out=rot[:, 4 + gp:5 + gp], in_=ctmp[:, 1:2]))(gp), reads=['ctmp'], writes=['rot'])
                break
            P.op('dve', (lambda es, n: lambda e: e.tensor_scalar_mul(out=etmp[:, 0:n], in0=es[:, 0:n], scalar1=ctmp[:, 1:2]))(es, n),
                 reads=[tg, 'ctmp'], writes=['etmp'])
            P.op('dve', (lambda ec, n: lambda e: e.scalar_tensor_tensor(out=ec[:, n:2 * n], in0=ec[:, 0:n], scalar=ctmp[:, 0:1], in1=etmp[:, 0:n],
                                                                       op0=ALU.mult, op1=ALU.subtract))(ec, n),
                 reads=[tg, 'ctmp', 'etmp'], writes=[tg])
            P.op('dve', (lambda ec, n: lambda e: e.tensor_scalar_mul(out=etmp[:, 0:n], in0=ec[:, 0:n], scalar1=ctmp[:, 1:2]))(ec, n),
                 reads=[tg, 'ctmp'], writes=['etmp'])
            P.op('dve', (lambda es, n: lambda e: e.scalar_tensor_tensor(out=es[:, n:2 * n], in0=es[:, 0:n], scalar=ctmp[:, 0:1], in1=etmp[:, 0:n],
                                                                       op0=ALU.mult, op1=ALU.add))(es, n),
                 reads=[tg, 'ctmp', 'etmp'], writes=[tg])
            n *= 2

    mi = 0
    yi = 0
    for seg in range(nseg):
        us = seg % 2
        ssl = slice(seg * L, (seg + 1) * L)
        P.op('sp', (lambda us, ssl: lambda e: e.dma_start(out=ub[us][:], in_=uT[:, ssl]))(us, ssl), writes=[('ub', us)], dma=f'ub{us}')
        for gp in range(4):
            tg = ('E', gp)
            for tb in range(L // 512):
                bs = mi % 2
                mi += 1
                fs = slice(tb * 512, (tb + 1) * 512)
                P.op('pe', (lambda gp, us, fs, bs: lambda e: e.matmul(psr[bs][:], lhsT=W[:, gp, :], rhs=ub[us][:, fs], start=True, stop=True))(gp, us, fs, bs),
                     reads=[('W', gp), ('ub', us)], writes=[('psr', bs)])
                P.op('pe', (lambda gp, us, fs, bs: lambda e: e.matmul(psi[bs][:], lhsT=W[:, 4 + gp, :], rhs=ub[us][:, fs], start=True, stop=True))(gp, us, fs, bs),
                     reads=[('W', 4 + gp), ('ub', us)], writes=[('psi', bs)])
                ecs, ess = Ec[:, gp, fs], Es[:, gp, fs]
                for k, (src, srcn, tab) in enumerate(((psr, 'psr', ecs), (psi, 'psi', ess), (psi, 'psi', ecs), (psr, 'psr', ess))):
                    P.op('dve', (lambda k, src, tab, bs: lambda e: e.tensor_tensor(out=m[k][bs][:], in0=src[bs][:], in1=tab, op=ALU.mult))(k, src, tab, bs),
                         reads=[(srcn, bs), tg], writes=[('m', k, bs)])
                P.op('pool', (lambda bs, fs: lambda e: e.tensor_tensor(out=win_r[:, fs], in0=m[0][bs][:], in1=m[1][bs][:], op=ALU.add))(bs, fs),
                     reads=[('m', 0, bs), ('m', 1, bs)], writes=[('win_r', tb)])
                P.op('pool', (lambda bs, fs: lambda e: e.tensor_tensor(out=win_i[:, fs], in0=m[2][bs][:], in1=m[3][bs][:], op=ALU.subtract))(bs, fs),
                     reads=[('m', 2, bs), ('m', 3, bs)], writes=[('win_i', tb)])
            rho = sm[:, 4 + gp:5 + gp].to_broadcast([128, L])
            P.op('dve', (lambda gp, rho: lambda e: e.tensor_tensor_scan(out=w_r[:], data0=rho, data1=win_r[:], initial=carry[:, gp:gp + 1],
                                                                       op0=ALU.mult, op1=ALU.add))(gp, rho),
                 reads=[('win_r', 0), ('win_r', 1), 'sm', 'carry'], writes=['w_r'])
            P.op('dve', (lambda gp, rho: lambda e: e.tensor_tensor_scan(out=w_i[:], data0=rho, data1=win_i[:], initial=carry[:, 4 + gp:5 + gp],
                                                                       op0=ALU.mult, op1=ALU.add))(gp, rho),
                 reads=[('win_i', 0), ('win_i', 1), 'sm', 'carry'], writes=['w_i'])
            rc, rs_ = rot[:, gp:gp + 1], rot[:, 4 + gp:5 + gp]
            P.op('dve', (lambda rs_: lambda e: e.tensor_tensor(out=ctmp[:, 0:1], in0=w_i[:, L - 1:L], in1=rs_, op=ALU.mult))(rs_),
                 reads=['w_i', 'rot'], writes=['ctmp'])
            P.op('dve', (lambda rc: lambda e: e.tensor_tensor(out=ctmp[:, 1:2], in0=w_i[:, L - 1:L], in1=rc, op=ALU.mult))(rc),
                 reads=['w_i', 'rot'], writes=['ctmp'])
            P.op('dve', (lambda gp, rc: lambda e: e.scalar_tensor_tensor(out=carry[:, gp:gp + 1], in0=w_r[:, L - 1:L], scalar=rc, in1=ctmp[:, 0:1],
                                                                        op0=ALU.mult, op1=ALU.subtract))(gp, rc),
                 reads=['w_r', 'rot', 'ctmp'], writes=['carry'])
            P.op('dve', (lambda gp, rs_: lambda e: e.scalar_tensor_tensor(out=carry[:, 4 + gp:5 + gp], in0=w_r[:, L - 1:L], scalar=rs_, in1=ctmp[:, 1:2],
                                                                         op0=ALU.mult, op1=ALU.add))(gp, rs_),
                 reads=['w_r', 'rot', 'ctmp'], writes=['carry'])
            ecl, esl = Ec[:, gp, :], Es[:, gp, :]
            P.op('pool', (lambda ecl: lambda e: e.tensor_tensor(out=dd[0][:], in0=w_r[:], in1=ecl, op=ALU.mult))(ecl), reads=['w_r', tg], writes=[('dd', 0)])
            P.op('pool', (lambda esl: lambda e: e.tensor_tensor(out=dd[1][:], in0=w_i[:], in1=esl, op=ALU.mult))(esl), reads=['w_i', tg], writes=[('dd', 1)])
            P.op('pool', (lambda gp: lambda e: e.tensor_tensor(out=xr[:, gp, :], in0=dd[0][:], in1=dd[1][:], op=ALU.subtract))(gp),
                 reads=[('dd', 0), ('dd', 1)], writes=[('xr', gp)])
            P.op('pool', (lambda esl: lambda e: e.tensor_tensor(out=dd[2][:], in0=w_r[:], in1=esl, op=ALU.mult))(esl), reads=['w_r', tg], writes=[('dd', 2)])
            P.op('pool', (lambda ecl: lambda e: e.tensor_tensor(out=dd[3][:], in0=w_i[:], in1=ecl, op=ALU.mult))(ecl), reads=['w_i', tg], writes=[('dd', 3)])
            P.op('pool', (lambda gp: lambda e: e.tensor_tensor(out=xi[:, gp, :], in0=dd[2][:], in1=dd[3][:], op=ALU.add))(gp),
                 reads=[('dd', 2), ('dd', 3)], writes=[('xi', gp)])
        for tb in range(L // 512):
            ys = yi % 2
            yi += 1
            fs = slice(tb * 512, (tb + 1) * 512)
            for gp in range(4):
                P.op('pe', (lambda gp, fs, ys: lambda e: e.matmul(psy[ys][:], lhsT=cpad_sb[:, gp, :], rhs=xr[:, gp, fs], start=(gp == 0), stop=False))(gp, fs, ys),
                     reads=['cpad', ('xr', gp)], writes=[('psy', ys)])
                P.op('pe', (lambda gp, fs, ys: lambda e: e.matmul(psy[ys][:], lhsT=cpad_sb[:, 4 + gp, :], rhs=xi[:, gp, fs], start=False, stop=(gp == 3)))(gp, fs, ys),
                     reads=['cpad', ('xi', gp)], writes=[('psy', ys)])
            P.op('dve', (lambda us, fs, ys: lambda e: e.scalar_tensor_tensor(out=ysb[ys][:], in0=ub[us][:, fs], scalar=dsk_sb[:, 0:1], in1=psy[ys][:],
                                                                            op0=ALU.mult, op1=ALU.add))(us, fs, ys),
                 reads=[('ub', us), 'dsk', ('psy', ys)], writes=[('ysb', ys)])
            P.op('pool', (lambda ys: lambda e: e.tensor_tensor(out=gt[ys][:], in0=ysb[ys][:], in1=ysb[ys][:], op=ALU.mult))(ys),
                 reads=[('ysb', ys)], writes=[('gt', ys)])
            P.op('pool', (lambda ys: lambda e: e.tensor_scalar(out=gt[ys][:], in0=gt[ys][:], scalar1=0.044715, scalar2=1.0, op0=ALU.mult, op1=ALU.add))(ys),
                 reads=[('gt', ys)], writes=[('gt', ys)])
            P.op('pool', (lambda ys: lambda e: e.tensor_tensor(out=gt[ys][:], in0=gt[ys][:], in1=ysb[ys][:], op=ALU.mult))(ys),
                 reads=[('gt', ys), ('ysb', ys)], writes=[('gt', ys)])
            P.op('act', (lambda ys: lambda e: e.activation(out=gs[ys][:], in_=gt[ys][:], func=AF.Sigmoid, scale=2.0 * math.sqrt(2.0 / PI)))(ys),
                 reads=[('gt', ys)], writes=[('gs', ys)])
            P.op('dve', (lambda ys: lambda e: e.tensor_tensor(out=yo[ys][:], in0=ysb[ys][:], in1=gs[ys][:], op=ALU.mult))(ys),
                 reads=[('ysb', ys), ('gs', ys)], writes=[('yo', ys)])
            osl = slice(seg * L + tb * 512, seg * L + (tb + 1) * 512)
            P.op('sp', (lambda ys, osl: lambda e: e.dma_start(out=yT[:, osl], in_=yo[ys][:]))(ys, osl), reads=[('yo', ys)], dma=f'yo{ys}')
    P.finish()
    P.emit()
    return nc


def layout_B1(inp, l, r, uT_full):
    b, gb = r // 4, r % 4
    f = lambda a: np.ascontiguousarray(a, dtype=np.float32)
    g0 = gb * 8
    prm = np.zeros((128, 12), np.float32)
    bpad = np.zeros((128, 8, 128), np.float32)
    cpad = np.zeros((128, 8, 128), np.float32)
    for gp in range(4):
        for gi in range(2):
            gl = 2 * gp + gi
            g = g0 + gl
            ps = slice(gi * 64, (gi + 1) * 64)
            prm[ps, gp] = inp['ssm_log_dt'][l][g]
            prm[ps, 4 + gp] = inp['ssm_a_re'][l][g]
            prm[ps, 8 + gp] = inp['ssm_a_im'][l][g]
            cs = slice(gl * 16, (gl + 1) * 16)
            bpad[ps, gp, cs] = inp['ssm_b_re'][l][g]
            bpad[ps, 4 + gp, cs] = inp['ssm_b_im'][l][g]
            cpad[ps, gp, cs] = inp['ssm_c_re'][l][g].T
            cpad[ps, 4 + gp, cs] = inp['ssm_c_im'][l][g].T
    return {
        "uT": f(uT_full[b][gb * 128:(gb + 1) * 128, :]),
        "prm": prm, "bpad": bpad, "cpad": cpad,
        "dsk": f(inp['ssm_d'][l].reshape(512)[gb * 128:(gb + 1) * 128].reshape(128, 1)),
    }


S = 16384


def build_B2(l, nqb=32):
    lam_init = 0.8 - 0.6 * math.exp(-0.3 * l)
    nc = bass.Bass("TRN2", target_bir_lowering=False)
    P = Prog(nc)
    D = lambda n, s, dt, k: nc.dram_tensor(n, s, dt, kind=k).ap()
    qT = D("qT", [128, S], BF16, "ExternalInput")
    kT = D("kT", [128, S], BF16, "ExternalInput")
    V = D("V", [128, 128, 128], BF16, "ExternalInput")
    lqk = D("lqk", [64, 4], F32, "ExternalInput")
    subw = D("subw", [128, 1], F32, "ExternalInput")
    oT = D("oT", [128, S], BF16, "ExternalOutput")

    kT_sb = P.sb("kT_sb", [128, S], BF16)
    V_sb = P.sb("V_sb", [128, 128, 128], BF16)
    qb_sb = [P.sb(f"qb{i}", [128, 512], BF16) for i in range(2)]
    lqk_sb = P.sb("lqk_sb", [64, 4], F32)
    subw_sb = P.sb("subw_sb", [128, 1], F32)
    prods = P.sb("prods", [64, 2], F32)
    E = P.sb("E", [128, 2], F32)
    nlam = P.sb("nlam", [128, 1], F32)
    onesf = P.sb("onesf", [128, 128], F32)
    epsb = P.sb("epsb", [128, 1], F32)
    iot = P.sb("iot", [128, 512], F32)
    masks = [P.sb(f"mask{j}", [128, 512], BF16) for j in range(4)]
    NPT = 3
    pT = [[P.sb(f"pT{c}_{i}", [128, 512], BF16) for i in range(NPT)] for c in range(2)]
    acc = [P.sb(f"acc{c}", [128, 512], F32) for c in range(2)]
    rl = [P.sb(f"rl{c}", [128, 512], F32) for c in range(2)]
    t01 = [P.sb(f"t01_{c}", [128, 512], F32) for c in range(2)]
    o_sb = P.sb("o_sb", [128, 512], F32)
    osq = P.sb("osq", [128, 512], F32)
    rs = P.sb("rs", [128, 512], F32)
    on = [P.sb(f"on{i}", [128, 512], BF16) for i in range(2)]
    ps_s = [[P.ps(f"ps_s{c}_{i}", [128, 512]) for i in range(2)] for c in range(2)]
    ps_o = [P.ps(f"ps_o{c}", [128, 512]) for c in range(2)]
    ps_l = [P.ps(f"ps_l{c}", [128, 512]) for c in range(2)]

    P.op('pool', lambda e: e.memset(onesf[:], 1.0), writes=['onesf'])
    P.op('pool', lambda e: e.memset(epsb[:], 1e-6), writes=['epsb'])
    P.op('pool', lambda e: e.iota(iot[:], pattern=[[1, 512]], base=0, channel_multiplier=-1,
                                  allow_small_or_imprecise_dtypes=True), writes=['iot'])
    for j in range(4):
        P.op('dve', (lambda j: lambda e: e.tensor_single_scalar(out=masks[j][:], in_=iot[:], scalar=float(128 * j), op=ALU.is_ge))(j),
             reads=['iot'], writes=[('mask', j)])
    P.op('sp', lambda e: e.dma_start(out=lqk_sb[:], in_=lqk), writes=['lqk'], dma='small')
    P.op('sp', lambda e: e.dma_start(out=subw_sb[:], in_=subw), writes=['subw'], dma='small')
    for i in range(4):
        P.op('sp', (lambda i: lambda e: e.dma_start(out=kT_sb[:, i * 4096:(i + 1) * 4096], in_=kT[:, i * 4096:(i + 1) * 4096]))(i),
             writes=[('kT', i)], dma='kT')
        P.op('sp', (lambda i: lambda e: e.dma_start(out=V_sb[:, i * 32:(i + 1) * 32, :], in_=V[:, i * 32:(i + 1) * 32, :]))(i),
             writes=[('V', i)], dma='V')
    P.op('dve', lambda e: e.tensor_tensor(out=prods[:, 0:1], in0=lqk_sb[:, 0:1], in1=lqk_sb[:, 1:2], op=ALU.mult),
         reads=['lqk'], writes=['prods'])
    P.op('dve', lambda e: e.tensor_tensor(out=prods[:, 1:2], in0=lqk_sb[:, 2:3], in1=lqk_sb[:, 3:4], op=ALU.mult),
         reads=['lqk'], writes=['prods'])
    P.op('pe', lambda e: e.matmul(ps_l[0][:, 0:2], lhsT=onesf[0:64, :], rhs=prods[:], start=True, stop=True),
         reads=['prods', 'onesf'], writes=[('ps_l', 0)])
    P.op('act', lambda e: e.activation(out=E[:], in_=ps_l[0][:, 0:2], func=AF.Exp), reads=[('ps_l', 0)], writes=['E'])
    P.op('dve', lambda e: e.tensor_tensor(out=nlam[:], in0=E[:, 1:2], in1=E[:, 0:1], op=ALU.subtract), reads=['E'], writes=['nlam'])
    P.op('dve', lambda e: e.tensor_scalar_add(out=nlam[:], in0=nlam[:], scalar1=-lam_init), reads=['nlam'], writes=['nlam'])

    it = 0
    for qb in range(nqb):
        qs = qb % 2
        qsl = slice(qb * 512, (qb + 1) * 512)
        P.op('sp', (lambda qs, qsl: lambda e: e.dma_start(out=qb_sb[qs][:], in_=qT[:, qsl]))(qs, qsl),
             writes=[('qb', qs)], dma=f'qb{qs}')
        nkt = 4 * (qb + 1)
        for kt in range(nkt):
            jd = kt - 4 * qb
            for c in range(2):
                sb_i = it % 2
                pi = it % NPT
                csl = slice(c * 64, (c + 1) * 64)
                P.op('pe', (lambda c, sb_i, kt, qs, csl: lambda e: e.matmul(ps_s[c][sb_i][:], lhsT=kT_sb[csl, kt * 128:(kt + 1) * 128],
                                                                           rhs=qb_sb[qs][csl, :], start=True, stop=True))(c, sb_i, kt, qs, csl),
                     reads=[('kT', kt // 32), ('qb', qs)], writes=[('ps_s', c, sb_i)])
                P.op('act', (lambda c, sb_i, pi: lambda e: e.activation(out=pT[c][pi][:], in_=ps_s[c][sb_i][:], func=AF.Exp, scale=0.125))(c, sb_i, pi),
                     reads=[('ps_s', c, sb_i)], writes=[('pT', c, pi)])
                if jd >= 0:
                    P.op('pool', (lambda c, pi, jd: lambda e: e.tensor_tensor(out=pT[c][pi][:], in0=pT[c][pi][:], in1=masks[jd][:], op=ALU.mult))(c, pi, jd),
                         reads=[('pT', c, pi), ('mask', jd)], writes=[('pT', c, pi)])
                P.op('pe', (lambda c, pi, kt, nkt: lambda e: e.matmul(ps_o[c][:], lhsT=V_sb[:, kt, :], rhs=pT[c][pi][:],
                                                                     start=(kt == 0), stop=(kt == nkt - 1)))(c, pi, kt, nkt),
                     reads=[('pT', c, pi), ('V', kt // 32)], writes=[('ps_o', c)])
                ae = 'dve' if c == 0 else 'pool'
                if kt == 0:
                    P.op(ae, (lambda c, pi: lambda e: e.tensor_copy(out=acc[c][:], in_=pT[c][pi][:]))(c, pi),
                         reads=[('pT', c, pi)], writes=[('acc', c)])
                else:
                    P.op(ae, (lambda c, pi: lambda e: e.tensor_tensor(out=acc[c][:], in0=acc[c][:], in1=pT[c][pi][:], op=ALU.add))(c, pi),
                         reads=[('pT', c, pi), ('acc', c)], writes=[('acc', c)])
            it += 1
        for c in range(2):
            P.op('pe', (lambda c: lambda e: e.matmul(ps_l[c][:], lhsT=onesf[:], rhs=acc[c][:], start=True, stop=True))(c),
                 reads=[('acc', c), 'onesf'], writes=[('ps_l', c)])
            P.op('dve', (lambda c: lambda e: e.reciprocal(out=rl[c][:], in_=ps_l[c][:]))(c), reads=[('ps_l', c)], writes=[('rl', c)])
            P.op('dve', (lambda c: lambda e: e.tensor_tensor(out=t01[c][:], in0=ps_o[c][:], in1=rl[c][:], op=ALU.mult))(c),
                 reads=[('ps_o', c), ('rl', c)], writes=[('t01', c)])
        P.op('dve', lambda e: e.scalar_tensor_tensor(out=o_sb[:], in0=t01[1][:], scalar=nlam[:, 0:1], in1=t01[0][:], op0=ALU.mult, op1=ALU.add),
             reads=[('t01', 0), ('t01', 1), 'nlam'], writes=['o_sb'])
        P.op('act', lambda e: e.activation(out=osq[:], in_=o_sb[:], func=AF.Square), reads=['o_sb'], writes=['osq'])
        P.op('pe', lambda e: e.matmul(ps_l[0][:], lhsT=onesf[:], rhs=osq[:], start=True, stop=True),
             reads=['osq', 'onesf'], writes=[('ps_l', 0)])
        P.op('act', lambda e: e.activation(out=rs[:], in_=ps_l[0][:], func=AF.Sqrt, bias=epsb[:], scale=1.0 / 128.0),
             reads=[('ps_l', 0), 'epsb'], writes=['rs'])
        P.op('dve', lambda e: e.reciprocal(out=rs[:], in_=rs[:]), reads=['rs'], writes=['rs'])
        P.op('dve', lambda e: e.tensor_scalar_mul(out=rs[:], in0=rs[:], scalar1=1.0 - lam_init), reads=['rs'], writes=['rs'])
        os_ = qb % 2
        P.op('dve', (lambda os_: lambda e: e.scalar_tensor_tensor(out=on[os_][:], in0=o_sb[:], scalar=subw_sb[:, 0:1], in1=rs[:], op0=ALU.mult, op1=ALU.mult))(os_),
             reads=['o_sb', 'rs', 'subw'], writes=[('on', os_)])
        P.op('sp', (lambda os_, qsl: lambda e: e.dma_start(out=oT[:, qsl], in_=on[os_][:]))(os_, qsl),
             reads=[('on', os_)], dma=f'on{os_}')
    P.finish()
    P.emit()
    return nc


def layout_B2(inp, l, r, qT_full, kT_full, vT_full):
    b, h = r // 4, r % 4
    hs = slice(h * 128, (h + 1) * 128)
    Vh = np.ascontiguousarray(vT_full[b][hs, :].T)
    Vl = np.ascontiguousarray(Vh.reshape(128, 128, 128).transpose(1, 0, 2))
    f = lambda a: np.ascontiguousarray(a, dtype=np.float32)
    return {
        "qT": np.ascontiguousarray(qT_full[b][hs, :]),
        "kT": np.ascontiguousarray(kT_full[b][hs, :]),
        "V": Vl,
        "lqk": f(np.stack([inp['lambda_q1'][l], inp['lambda_k1'][l], inp['lambda_q2'][l], inp['lambda_k2'][l]], axis=1)),
        "subw": f(inp['subln_w'][l].reshape(128, 1)),
    }


T = 4096
NB = T // 512


def build_C1():
    nc = bass.Bass("TRN2", target_bir_lowering=False)
    P = Prog(nc)
    D = lambda n, s, dt, k: nc.dram_tensor(n, s, dt, kind=k).ap()
    xT = D("xT", [1024, T], F32, "ExternalInput")
    ysT = D("ysT", [512, T], BF16, "ExternalInput")
    oT = D("oT", [512, T], BF16, "ExternalInput")
    gT = D("gT", [2048, T], F32, "ExternalInput")
    cb = D("cb", [128, 8], F32, "ExternalInput")
    adaw = D("adaw", [1024, 1024], F32, "ExternalInput")
    adab = D("adab", [128, 8], F32, "ExternalInput")
    gluw = D("gluw", [512, 2048], F32, "ExternalInput")
    upw = D("upw", [512, 1024], F32, "ExternalInput")
    wout = D("wout", [1024, 1024], F32, "ExternalInput")
    x1T = D("x1T", [1024, T], F32, "ExternalOutput")

    glu_bf = P.sb("glu_bf", [128, 4, 2048], BF16)
    up_bf = P.sb("up_bf", [128, 4, 1024], BF16)
    wo_bf = P.sb("wo_bf", [128, 8, 1024], BF16)
    stg = [P.sb(f"stg{i}", [128, 2048], F32) for i in range(2)]
    c_sb = P.sb("c_sb", [128, 8], F32)
    ca_sb = P.sb("ca_sb", [128, 8], F32)
    adab_sb = P.sb("adab_sb", [128, 8], F32)
    g1 = P.sb("g1", [128, 8], F32)
    xb = [P.sb(f"xb{i}", [128, 8, 512], F32) for i in range(2)]
    ysb = [P.sb(f"ysb{i}", [128, 4, 512], BF16) for i in range(2)]
    ob = [P.sb(f"ob{i}", [128, 4, 512], BF16) for i in range(2)]
    gb = [P.sb(f"gb{i}", [128, 2, 512], F32) for i in range(2)]
    sg = [P.sb(f"sg{i}", [128, 512], F32) for i in range(2)]
    t1 = [P.sb(f"t1_{i}", [128, 512], F32) for i in range(2)]
    t2 = [P.sb(f"t2_{i}", [128, 512], F32) for i in range(2)]
    mg = P.sb("mg", [128, 8, 512], BF16)
    xo = [P.sb(f"xo{i}", [128, 512], F32) for i in range(2)]
    pb = [P.ps(f"pb{i}", [128, 512]) for i in range(7)]
    pm = P.ps("pm", [128, 8])
    bank = [0]

    def nb():
        bank[0] = (bank[0] + 1) % 7
        return bank[0]

    for nm, dst, src in (('c', c_sb, cb), ('adab', adab_sb, adab)):
        P.op('sp', (lambda dst, src: lambda e: e.dma_start(out=dst[:], in_=src))(dst, src), writes=[nm], dma='small')
    P.op('act', lambda e: e.activation(out=ca_sb[:], in_=c_sb[:], func=AF.Silu), reads=['c'], writes=['ca'])
    for q2 in range(2):
        xs = q2 % 2
        P.op('sp', (lambda xs, q2: lambda e: e.dma_start(out=xb[xs][:], in_=adaw[:, q2 * 512:(q2 + 1) * 512].rearrange("(kc p) t -> p kc t", p=128)))(xs, q2),
             writes=[('xb', xs)], dma=f'xb{xs}')
        for mm in range(4):
            m = q2 * 4 + mm
            for kc in range(8):
                P.op('pe', (lambda xs, kc, m, mm: lambda e: e.matmul(pm[:, m:m + 1], lhsT=xb[xs][:, kc, mm * 128:(mm + 1) * 128],
                                                                  rhs=ca_sb[:, kc:kc + 1], start=(kc == 0), stop=(kc == 7)))(xs, kc, m, mm),
                     reads=[('xb', xs), 'ca'], writes=['pm'])
    P.op('dve', lambda e: e.tensor_tensor(out=g1[:], in0=pm[:], in1=adab_sb[:], op=ALU.add), reads=['pm', 'adab'], writes=['g1'])

    si = 0
    def load_w(src, rows_kc, ncols, dst, tag):
        nonlocal si
        for kc in range(rows_kc):
            for c0 in range(0, ncols, 2048):
                cw = min(2048, ncols - c0)
                s = si % 2
                si += 1
                P.op('sp', (lambda s, kc, c0, cw: lambda e: e.dma_start(out=stg[s][:, 0:cw], in_=src[kc * 128:(kc + 1) * 128, c0:c0 + cw]))(s, kc, c0, cw),
                     writes=[('stg', s)], dma=f'stg{s}')
                P.op('pool', (lambda s, kc, c0, cw: lambda e: e.tensor_copy(out=dst[:, kc, c0:c0 + cw], in_=stg[s][:, 0:cw]))(s, kc, c0, cw),
                     reads=[('stg', s)], writes=[tag])
    load_w(gluw, 4, 2048, glu_bf, 'glu')
    load_w(upw, 4, 1024, up_bf, 'up')
    load_w(wout, 8, 1024, wo_bf, 'wo')

    gi = 0
    oi = 0
    for tb in range(NB):
        xs = tb % 2
        tsl = slice(tb * 512, (tb + 1) * 512)
        P.op('sp', (lambda xs, tsl: lambda e: e.dma_start(out=xb[xs][:], in_=xT[:, tsl].rearrange("(kc p) t -> p kc t", p=128)))(xs, tsl),
             writes=[('xb', xs)], dma=f'xb{xs}')
        P.op('sp', (lambda xs, tsl: lambda e: e.dma_start(out=ysb[xs][:], in_=ysT[:, tsl].rearrange("(kc p) t -> p kc t", p=128)))(xs, tsl),
             writes=[('ysb', xs)], dma=f'ysb{xs}')
        P.op('sp', (lambda xs, tsl: lambda e: e.dma_start(out=ob[xs][:], in_=oT[:, tsl].rearrange("(kc p) t -> p kc t", p=128)))(xs, tsl),
             writes=[('ob', xs)], dma=f'ob{xs}')
        for m in range(8):
            gs_ = gi % 2
            gi += 1
            P.op('sp', (lambda gs_, m, tsl: lambda e: e.dma_start(out=gb[gs_][:], in_=gT[:, tsl].rearrange("(two r) t -> r two t", two=2)[m * 128:(m + 1) * 128]))(gs_, m, tsl),
                 writes=[('gb', gs_)], dma=f'gb{gs_}')
            bv, bg, ba = nb(), nb(), nb()
            for kc in range(4):
                P.op('pe', (lambda bv, kc, m, xs: lambda e: e.matmul(pb[bv][:], lhsT=glu_bf[:, kc, m * 128:(m + 1) * 128], rhs=ysb[xs][:, kc, :],
                                                                    start=(kc == 0), stop=(kc == 3)))(bv, kc, m, xs),
                     reads=['glu', ('ysb', xs)], writes=[('pb', bv)])
            for kc in range(4):
                P.op('pe', (lambda bg, kc, m, xs: lambda e: e.matmul(pb[bg][:], lhsT=glu_bf[:, kc, 1024 + m * 128:1024 + (m + 1) * 128], rhs=ysb[xs][:, kc, :],
                                                                    start=(kc == 0), stop=(kc == 3)))(bg, kc, m, xs),
                     reads=['glu', ('ysb', xs)], writes=[('pb', bg)])
            for kc in range(4):
                P.op('pe', (lambda ba, kc, m, xs: lambda e: e.matmul(pb[ba][:], lhsT=up_bf[:, kc, m * 128:(m + 1) * 128], rhs=ob[xs][:, kc, :],
                                                                    start=(kc == 0), stop=(kc == 3)))(ba, kc, m, xs),
                     reads=['up', ('ob', xs)], writes=[('pb', ba)])
            s = m % 2
            P.op('act', (lambda s, bg: lambda e: e.activation(out=sg[s][:], in_=pb[bg][:], func=AF.Sigmoid))(s, bg),
                 reads=[('pb', bg)], writes=[('sg', s)])
            P.op('dve', (lambda s, bv: lambda e: e.tensor_tensor(out=t1[s][:], in0=pb[bv][:], in1=sg[s][:], op=ALU.mult))(s, bv),
                 reads=[('pb', bv), ('sg', s)], writes=[('t1', s)])
            P.op('pool', (lambda s, gs_: lambda e: e.tensor_tensor(out=t1[s][:], in0=t1[s][:], in1=gb[gs_][:, 0, :], op=ALU.mult))(s, gs_),
                 reads=[('t1', s), ('gb', gs_)], writes=[('t1', s)])
            P.op('dve', (lambda s, ba, gs_: lambda e: e.tensor_tensor(out=t2[s][:], in0=pb[ba][:], in1=gb[gs_][:, 1, :], op=ALU.mult))(s, ba, gs_),
                 reads=[('pb', ba), ('gb', gs_)], writes=[('t2', s)])
            P.op('pool', (lambda s, m: lambda e: e.tensor_tensor(out=mg[:, m, :], in0=t1[s][:], in1=t2[s][:], op=ALU.add))(s, m),
                 reads=[('t1', s), ('t2', s)], writes=[('mg', m)])
        for m in range(8):
            bo = nb()
            for kc in range(8):
                P.op('pe', (lambda bo, kc, m: lambda e: e.matmul(pb[bo][:], lhsT=wo_bf[:, kc, m * 128:(m + 1) * 128], rhs=mg[:, kc, :],
                                                                start=(kc == 0), stop=(kc == 7)))(bo, kc, m),
                     reads=['wo', ('mg', kc)], writes=[('pb', bo)])
            o = oi % 2
            oi += 1
            P.op('dve', (lambda o, bo, m, xs: lambda e: e.scalar_tensor_tensor(out=xo[o][:], in0=pb[bo][:], scalar=g1[:, m:m + 1], in1=xb[xs][:, m, :],
                                                                              op0=ALU.mult, op1=ALU.add))(o, bo, m, xs),
                 reads=[('pb', bo), 'g1', ('xb', xs)], writes=[('xo', o)])
            P.op('sp', (lambda o, m, tsl: lambda e: e.dma_start(out=x1T[m * 128:(m + 1) * 128, tsl], in_=xo[o][:]))(o, m, tsl),
                 reads=[('xo', o)], dma=f'xo{o}')
    P.finish()
    P.emit()
    return nc


def layout_C1(inp, l, r, xT_full, ysT_full, oT_full, gT_core):
    b, j = r // 4, r % 4
    f = lambda a: np.ascontiguousarray(a, dtype=np.float32)
    sl = slice(j * T, (j + 1) * T)
    return {
        "xT": f(xT_full[b][:, sl]),
        "ysT": np.ascontiguousarray(ysT_full[b][:, sl]),
        "oT": np.ascontiguousarray(oT_full[b][:, sl]),
        "gT": gT_core,
        "cb": f(inp['c'][b].reshape(8, 128).T),
        "adaw": f(inp['ada_w'][l][:, 2048:3072]),
        "adab": f(inp['ada_b'][l][2048:3072].reshape(8, 128).T),
        "gluw": f(inp['ssm_glu_w'][l]),
        "upw": f(inp['attn_up_w'][l]),
        "wout": f(inp['w_out'][l]),
    }


T = 4096
NB = T // 512
NE = 16384
NSLOT = 4
NEG = -1.0e30


def build_C2(ntile=32):
    nc = bass.Bass("TRN2", target_bir_lowering=False)
    P = Prog(nc)
    D = lambda n, s, dt, k: nc.dram_tensor(n, s, dt, kind=k).ap()
    x1T = D("x1T", [1024, T], F32, "ExternalInput")
    cb = D("cb", [128, 8], F32, "ExternalInput")
    adaw = D("adaw", [1024, 3072], F32, "ExternalInput")
    adab = D("adab", [128, 16], F32, "ExternalInput")
    g2b = D("g2b", [1, 1024], F32, "ExternalInput")
    n2w = D("n2w", [128, 8], F32, "ExternalInput")
    wq = D("wq", [1024, 2048], F32, "ExternalInput")
    kTd = D("kT", [128, 2, 128], F32, "ExternalInput")
    utab = D("utab", [NE, 1024], F32, "ExternalInput")
    vtab = D("vtab", [NE, 1024], F32, "ExternalInput")
    x2 = D("x2", [T, 1024], F32, "ExternalOutput")

    wq_bf = P.sb("wq_bf", [128, 8, 2048], BF16)
    stg = [P.sb(f"stg{i}", [128, 2048], F32) for i in range(2)]
    c_sb = P.sb("c_sb", [128, 8], F32)
    ca_sb = P.sb("ca_sb", [128, 8], F32)
    ca_bc = P.sb("ca_bc", [128, 8, 128], F32)
    adab_sb = P.sb("adab_sb", [128, 16], F32)
    n2w_sb = P.sb("n2w_sb", [128, 8], F32)
    kT_sb = P.sb("kT_sb", [128, 2, 128], F32)
    modT = P.sb("modT", [128, 16], F32)
    wmod = P.sb("wmod", [128, 8], F32)
    g2bc = P.sb("g2bc", [128, 1024], F32)
    g2bias = P.sb("g2bias", [128, 1024], F32)
    ones = P.sb("ones", [128, 128], F32)
    epsb = P.sb("epsb", [128, 1], F32)
    ident = P.sb("ident", [128, 128], F32)
    iot = P.sb("iot", [128, 128], F32)
    xb = [P.sb(f"xb{i}", [128, 8, 512], F32) for i in range(2)]
    sq = [P.sb(f"sq{i}", [128, 512], F32) for i in range(2)]
    rstd = P.sb("rstd", [128, 512], F32)
    tmp = [P.sb(f"tmp{i}", [128, 512], F32) for i in range(2)]
    h2T = P.sb("h2T", [128, 8, 512], F32)
    h2Tbf = P.sb("h2Tbf", [128, 8, 512], BF16)
    h2_tok = P.sb("h2_tok", [128, 1024], F32)
    x1_tok = P.sb("x1_tok", [128, 1024], F32)
    qc = [P.sb(f"qc{i}", [128, 128], F32) for i in range(3)]
    s_sb = P.sb("s_sb", [128, 16, 128], F32)
    s2 = P.sb("s2", [128, 128], F32)
    v16 = P.sb("v16", [128, 16, 16], F32)
    i16 = P.sb("i16", [128, 16, 16], U32)
    if32 = P.sb("if32", [128, 16, 16], F32)
    i1s = P.sb("i1s", [128, 8, 16], F32)
    cand = [P.sb(f"cand{i}", [128, 16, 16], F32) for i in range(2)]
    cand2 = P.sb("cand2", [128, 256], F32)
    eid = [P.sb(f"eid{i}", [128, 16, 16], F32) for i in range(2)]
    junk256 = P.sb("junk256", [128, 256], F32)
    sc = P.sb("sc", [128, 8, 16], F32)
    ef = P.sb("ef", [128, 128], F32)
    idx = P.sb("idx", [128, 128], I32)
    gex = P.sb("gex", [128, 8, 16], F32)
    gsum = P.sb("gsum", [128, 8], F32)
    gw = P.sb("gw", [128, 128], F32)
    act = P.sb("act", [128, 128], F32)
    gt = P.sb("gt", [128, 128], F32)
    wts = P.sb("wts", [128, 128], F32)
    ug = [P.sb(f"ug{i}", [128, 1024], F32) for i in range(NSLOT)]
    vg = [P.sb(f"vg{i}", [128, 1024], F32) for i in range(NSLOT)]
    junk = P.sb("junk", [128, 1024], F32)
    acc = P.sb("acc", [128, 1024], F32)
    xo = P.sb("xo", [128, 1024], F32)
    pb = [P.ps(f"pb{i}", [128, 512]) for i in range(7)]
    pm = P.ps("pm", [128, 16])
    bank = [0]

    def nb():
        bank[0] = (bank[0] + 1) % 7
        return bank[0]

    P.op('pool', lambda e: e.memset(ones[:], 1.0), writes=['ones'])
    P.op('pool', lambda e: e.memset(epsb[:], 1e-6), writes=['epsb'])
    P.op('pool', lambda e: e.iota(iot[:], pattern=[[1, 128]], base=0, channel_multiplier=-1, allow_small_or_imprecise_dtypes=True), writes=['iot'])
    P.op('dve', lambda e: e.tensor_single_scalar(out=ident[:], in_=iot[:], scalar=0.0, op=ALU.is_equal), reads=['iot'], writes=['ident'])
    for nm, dst, src in (('c', c_sb, cb), ('adab', adab_sb, adab), ('n2w', n2w_sb, n2w), ('kT', kT_sb, kTd)):
        P.op('sp', (lambda dst, src: lambda e: e.dma_start(out=dst[:], in_=src))(dst, src), writes=[nm], dma='small')
    P.op('sp', lambda e: e.dma_start(out=g2bias[:], in_=g2b.partition_broadcast(128)), writes=['g2bias'], dma='small')
    P.op('act', lambda e: e.activation(out=ca_sb[:], in_=c_sb[:], func=AF.Silu), reads=['c'], writes=['ca'])
    for kc in range(8):
        P.op('dve', (lambda kc: lambda e: e.tensor_copy(out=ca_bc[:, kc, :], in_=ca_sb[:, kc:kc + 1].to_broadcast([128, 128])))(kc),
             reads=['ca'], writes=['ca_bc'])
    for q4 in range(4):
        xs = q4 % 2
        P.op('sp', (lambda xs, q4: lambda e: e.dma_start(out=xb[xs][:], in_=adaw[:, q4 * 512:(q4 + 1) * 512].rearrange("(kc p) t -> p kc t", p=128)))(xs, q4),
             writes=[('xb', xs)], dma=f'xb{xs}')
        for mm in range(4):
            m = q4 * 4 + mm
            for kc in range(8):
                P.op('pe', (lambda xs, kc, m, mm: lambda e: e.matmul(pm[:, m:m + 1], lhsT=xb[xs][:, kc, mm * 128:(mm + 1) * 128],
                                                                  rhs=ca_sb[:, kc:kc + 1], start=(kc == 0), stop=(kc == 7)))(xs, kc, m, mm),
                     reads=[('xb', xs), 'ca'], writes=['pm'])
    P.op('dve', lambda e: e.tensor_tensor(out=modT[:], in0=pm[:], in1=adab_sb[:], op=ALU.add), reads=['pm', 'adab'], writes=['modT'])
    P.op('dve', lambda e: e.scalar_tensor_tensor(out=wmod[:], in0=modT[:, 8:16], scalar=1.0, in1=n2w_sb[:], op0=ALU.add, op1=ALU.mult),
         reads=['modT', 'n2w'], writes=['wmod'])
    for q2 in range(2):
        xs = q2 % 2
        bq = nb()
        P.op('sp', (lambda xs, q2: lambda e: e.dma_start(out=xb[xs][:], in_=adaw[:, 2048 + q2 * 512:2048 + (q2 + 1) * 512].rearrange("(kc p) t -> p kc t", p=128)))(xs, q2),
             writes=[('xb', xs)], dma=f'xb{xs}')
        for kc in range(8):
            P.op('pe', (lambda xs, kc, bq: lambda e: e.matmul(pb[bq][:], lhsT=ca_bc[:, kc, :], rhs=xb[xs][:, kc, :], start=(kc == 0), stop=(kc == 7)))(xs, kc, bq),
                 reads=[('xb', xs), 'ca_bc'], writes=[('pb', bq)])
        P.op('dve', (lambda q2, bq: lambda e: e.tensor_tensor(out=g2bc[:, q2 * 512:(q2 + 1) * 512], in0=pb[bq][:], in1=g2bias[:, q2 * 512:(q2 + 1) * 512], op=ALU.add))(q2, bq),
             reads=[('pb', bq), 'g2bias'], writes=['g2bc'])
    for kc in range(8):
        s = kc % 2
        P.op('sp', (lambda s, kc: lambda e: e.dma_start(out=stg[s][:], in_=wq[kc * 128:(kc + 1) * 128, :]))(s, kc), writes=[('stg', s)], dma=f'stg{s}')
        P.op('pool', (lambda s, kc: lambda e: e.tensor_copy(out=wq_bf[:, kc, :], in_=stg[s][:]))(s, kc), reads=[('stg', s)], writes=['wq'])

    qi = 0
    ti = 0
    for tb in range(NB):
        if ti >= ntile:
            break
        xs = tb % 2
        tsl = slice(tb * 512, (tb + 1) * 512)
        P.op('sp', (lambda xs, tsl: lambda e: e.dma_start(out=xb[xs][:], in_=x1T[:, tsl].rearrange("(kc p) t -> p kc t", p=128)))(xs, tsl),
             writes=[('xb', xs)], dma=f'xb{xs}')
        bn = nb()
        for kc in range(8):
            s = kc % 2
            P.op('act', (lambda xs, kc, s: lambda e: e.activation(out=sq[s][:], in_=xb[xs][:, kc, :], func=AF.Square))(xs, kc, s),
                 reads=[('xb', xs)], writes=[('sq', s)])
            P.op('pe', (lambda kc, s, bn: lambda e: e.matmul(pb[bn][:], lhsT=ones[:], rhs=sq[s][:], start=(kc == 0), stop=(kc == 7)))(kc, s, bn),
                 reads=[('sq', s), 'ones'], writes=[('pb', bn)])
        P.op('act', (lambda bn: lambda e: e.activation(out=rstd[:], in_=pb[bn][:], func=AF.Sqrt, bias=epsb[:], scale=1.0 / 1024.0))(bn),
             reads=[('pb', bn), 'epsb'], writes=['rstd'])
        P.op('dve', lambda e: e.reciprocal(out=rstd[:], in_=rstd[:]), reads=['rstd'], writes=['rstd'])
        for kc in range(8):
            s = kc % 2
            P.op('dve', (lambda xs, kc, s: lambda e: e.tensor_tensor(out=tmp[s][:], in0=xb[xs][:, kc, :], in1=rstd[:], op=ALU.mult))(xs, kc, s),
                 reads=[('xb', xs), 'rstd'], writes=[('tmp', s)])
            P.op('act', (lambda kc, s: lambda e: e.activation(out=h2T[:, kc, :], in_=tmp[s][:], func=AF.Identity,
                                                             bias=modT[:, kc:kc + 1], scale=wmod[:, kc:kc + 1]))(kc, s),
                 reads=[('tmp', s), 'modT', 'wmod'], writes=[('h2T', kc)])
            P.op('pool', (lambda kc: lambda e: e.tensor_copy(out=h2Tbf[:, kc, :], in_=h2T[:, kc, :]))(kc), reads=[('h2T', kc)], writes=[('h2Tbf', kc)])
        for st in range(4):
            if ti >= ntile:
                break
            ssl = slice(st * 128, (st + 1) * 128)
            for src, srcn, dst, dstn in ((h2T, 'h2T', h2_tok, 'h2_tok'), (xb[xs], ('xb', xs), x1_tok, 'x1_tok')):
                for half in range(2):
                    bt = nb()
                    for k4 in range(4):
                        kc = half * 4 + k4
                        rd = [(srcn, kc)] if srcn == 'h2T' else [srcn]
                        P.op('pe', (lambda src, kc, k4, bt, ssl: lambda e: e.transpose(out=pb[bt][:, k4 * 128:(k4 + 1) * 128], in_=src[:, kc, ssl], identity=ident[:]))(src, kc, k4, bt, ssl),
                             reads=rd + ['ident'], writes=[('pb', bt)])
                    P.op('act', (lambda dst, half, bt: lambda e: e.copy(out=dst[:, half * 512:(half + 1) * 512], in_=pb[bt][:]))(dst, half, bt),
                         reads=[('pb', bt)], writes=[dstn])
            bs = None
            for hs in range(16):
                side = hs % 2
                bq = nb()
                for kc in range(8):
                    P.op('pe', (lambda bq, kc, hs, ssl: lambda e: e.matmul(pb[bq][:, 0:128], lhsT=wq_bf[:, kc, hs * 128:(hs + 1) * 128], rhs=h2Tbf[:, kc, ssl],
                                                                          start=(kc == 0), stop=(kc == 7)))(bq, kc, hs, ssl),
                         reads=['wq', ('h2Tbf', kc)], writes=[('pb', bq)])
                q_ = qi % 3
                qi += 1
                P.op('act', (lambda q_, bq: lambda e: e.copy(out=qc[q_][:], in_=pb[bq][:, 0:128]))(q_, bq), reads=[('pb', bq)], writes=[('qc', q_)])
                if hs % 4 == 0:
                    bs = nb()
                P.op('pe', (lambda bs, q_, hs, side: lambda e: e.matmul(pb[bs][:, (hs % 4) * 128:(hs % 4 + 1) * 128], lhsT=qc[q_][:], rhs=kT_sb[:, side, :],
                                                                       start=True, stop=True))(bs, q_, hs, side),
                     reads=[('qc', q_), 'kT'], writes=[('pb', bs)])
                if hs % 4 == 3:
                    g4 = hs // 4
                    P.op('act', (lambda bs, g4: lambda e: e.copy(out=s_sb[:, g4 * 4:(g4 + 1) * 4, :], in_=pb[bs][:].rearrange("p (a b) -> p a b", a=4)))(bs, g4),
                         reads=[('pb', bs)], writes=[('s_sb', g4)])
            for hs in range(16):
                rs_ = [('s_sb', hs // 4)]
                P.op('dve', (lambda hs: lambda e: e.max(out=v16[:, hs, 0:8], in_=s_sb[:, hs, :]))(hs), reads=rs_, writes=[('v16', hs)])
                P.op('dve', (lambda hs: lambda e: e.max_index(out=i16[:, hs, 0:8], in_max=v16[:, hs, 0:8], in_values=s_sb[:, hs, :]))(hs),
                     reads=rs_ + [('v16', hs)], writes=[('i16', hs)])
                P.op('dve', (lambda hs: lambda e: e.match_replace(out=s2[:], in_to_replace=v16[:, hs, 0:8], in_values=s_sb[:, hs, :], imm_value=NEG))(hs),
                     reads=rs_ + [('v16', hs)], writes=['s2'])
                P.op('dve', (lambda hs: lambda e: e.max(out=v16[:, hs, 8:16], in_=s2[:]))(hs), reads=['s2'], writes=[('v16', hs)])
                P.op('dve', (lambda hs: lambda e: e.max_index(out=i16[:, hs, 8:16], in_max=v16[:, hs, 8:16], in_values=s2[:]))(hs),
                     reads=['s2', ('v16', hs)], writes=[('i16', hs)])
            allv = [('v16', hs) for hs in range(16)]
            alli = [('i16', hs) for hs in range(16)]
            P.op('dve', lambda e: e.tensor_copy(out=if32[:], in_=i16[:]), reads=alli, writes=['if32'])
            P.op('dve', lambda e: e.tensor_scalar_mul(out=i1s[:], in0=if32[:].rearrange("p (h s) k -> p h s k", s=2)[:, :, 0, :], scalar1=128.0),
                 reads=['if32'], writes=['i1s'])
            for h in range(8):
                cs = h % 2
                cf = cand[cs][:].rearrange("p a b -> p (a b)")
                efl = eid[cs][:].rearrange("p a b -> p (a b)")
                P.op('dve', (lambda h, cs: lambda e: e.tensor_tensor(out=cand[cs][:], in0=v16[:, 2 * h, :].unsqueeze(2).to_broadcast([128, 16, 16]),
                                                                    in1=v16[:, 2 * h + 1, :].unsqueeze(1).to_broadcast([128, 16, 16]), op=ALU.add))(h, cs),
                     reads=allv, writes=[('cand', cs)])
                P.op('pool', (lambda h, cs: lambda e: e.tensor_tensor(out=eid[cs][:], in0=i1s[:, h, :].unsqueeze(2).to_broadcast([128, 16, 16]),
                                                                     in1=if32[:, 2 * h + 1, :].unsqueeze(1).to_broadcast([128, 16, 16]), op=ALU.add))(h, cs),
                     reads=['i1s', 'if32'], writes=[('eid', cs)])
                P.op('dve', (lambda h, cf: lambda e: e.max(out=sc[:, h, 0:8], in_=cf))(h, cf), reads=[('cand', cs)], writes=[('sc', h)])
                P.op('dve', (lambda h, cf: lambda e: e.match_replace(out=cand2[:], in_to_replace=sc[:, h, 0:8], in_values=cf, imm_value=NEG))(h, cf),
                     reads=[('cand', cs), ('sc', h)], writes=['cand2'])
                P.op('dve', (lambda h: lambda e: e.max(out=sc[:, h, 8:16], in_=cand2[:]))(h), reads=['cand2'], writes=[('sc', h)])
                for k in range(16):
                    j = h * 16 + k
                    P.op('dve', (lambda h, k, j, cf, efl: lambda e: e.scalar_tensor_tensor(out=junk256[:], in0=cf, scalar=sc[:, h, k:k + 1], in1=efl,
                                                                                        op0=ALU.is_equal, op1=ALU.mult, accum_out=ef[:, j:j + 1]))(h, k, j, cf, efl),
                         reads=[('cand', cs), ('eid', cs), ('sc', h)], writes=['ef'])
            allsc = [('sc', h) for h in range(8)]
            P.op('dve', lambda e: e.tensor_scalar(out=ef[:], in0=ef[:], scalar1=float(NE - 1), scalar2=0.0, op0=ALU.min, op1=ALU.max), reads=['ef'], writes=['ef'])
            P.op('dve', lambda e: e.tensor_copy(out=idx[:], in_=ef[:]), reads=['ef'], writes=['idx'])
            P.op('dve', lambda e: e.tensor_tensor(out=gex[:], in0=sc[:], in1=sc[:, :, 0:1].to_broadcast([128, 8, 16]), op=ALU.subtract),
                 reads=allsc, writes=['gex'])
            P.op('act', lambda e: e.activation(out=gex[:], in_=gex[:], func=AF.Exp), reads=['gex'], writes=['gex'])
            P.op('dve', lambda e: e.tensor_reduce(out=gsum[:], in_=gex[:], axis=AX.X, op=ALU.add), reads=['gex'], writes=['gsum'])
            P.op('dve', lambda e: e.reciprocal(out=gsum[:], in_=gsum[:]), reads=['gsum'], writes=['gsum'])
            P.op('dve', lambda e: e.tensor_tensor(out=gw[:].rearrange("p (h k) -> p h k", h=8), in0=gex[:], in1=gsum[:].unsqueeze(2).to_broadcast([128, 8, 16]), op=ALU.mult),
                 reads=['gex', 'gsum'], writes=['gw'])
            for j in range(128):
                sl_ = j % NSLOT
                P.op('pool', (lambda sl_, j: lambda e: e.indirect_dma_start(out=ug[sl_][:], out_offset=None, in_=utab,
                                                                          in_offset=bass.IndirectOffsetOnAxis(ap=idx[:, j:j + 1], axis=0)))(sl_, j),
                     reads=['idx'], writes=[('ug', sl_)], dma=f'ug{sl_}')
                P.op('dve', (lambda sl_, j: lambda e: e.scalar_tensor_tensor(out=junk[:], in0=ug[sl_][:], scalar=1.0, in1=h2_tok[:], op0=ALU.mult, op1=ALU.mult,
                                                                           accum_out=act[:, j:j + 1]))(sl_, j),
                     reads=[('ug', sl_), 'h2_tok'], writes=['act'])
            P.op('dve', lambda e: e.tensor_tensor(out=gt[:], in0=act[:], in1=act[:], op=ALU.mult), reads=['act'], writes=['gt'])
            P.op('dve', lambda e: e.tensor_scalar(out=gt[:], in0=gt[:], scalar1=0.044715, scalar2=1.0, op0=ALU.mult, op1=ALU.add), reads=['gt'], writes=['gt'])
            P.op('dve', lambda e: e.tensor_tensor(out=gt[:], in0=gt[:], in1=act[:], op=ALU.mult), reads=['gt', 'act'], writes=['gt'])
            P.op('act', lambda e: e.activation(out=gt[:], in_=gt[:], func=AF.Sigmoid, scale=2.0 * math.sqrt(2.0 / math.pi)), reads=['gt'], writes=['gt'])
            P.op('dve', lambda e: e.tensor_tensor(out=gt[:], in0=gt[:], in1=act[:], op=ALU.mult), reads=['gt', 'act'], writes=['gt'])
            P.op('dve', lambda e: e.tensor_tensor(out=wts[:], in0=gt[:], in1=gw[:], op=ALU.mult), reads=['gt', 'gw'], writes=['wts'])
            for j in range(128):
                sl_ = j % NSLOT
                P.op('pool', (lambda sl_, j: lambda e: e.indirect_dma_start(out=vg[sl_][:], out_offset=None, in_=vtab,
                                                                          in_offset=bass.IndirectOffsetOnAxis(ap=idx[:, j:j + 1], axis=0)))(sl_, j),
                     reads=['idx'], writes=[('vg', sl_)], dma=f'vg{sl_}')
                if j == 0:
                    P.op('dve', (lambda sl_: lambda e: e.tensor_scalar_mul(out=acc[:], in0=vg[sl_][:], scalar1=wts[:, 0:1]))(sl_),
                         reads=[('vg', sl_), 'wts'], writes=['acc'])
                else:
                    P.op('dve', (lambda sl_, j: lambda e: e.scalar_tensor_tensor(out=acc[:], in0=vg[sl_][:], scalar=wts[:, j:j + 1], in1=acc[:],
                                                                               op0=ALU.mult, op1=ALU.add))(sl_, j),
                         reads=[('vg', sl_), 'wts', 'acc'], writes=['acc'])
            P.op('dve', lambda e: e.tensor_tensor(out=xo[:], in0=acc[:], in1=g2bc[:], op=ALU.mult), reads=['acc', 'g2bc'], writes=['xo'])
            P.op('dve', lambda e: e.tensor_tensor(out=xo[:], in0=xo[:], in1=x1_tok[:], op=ALU.add), reads=['xo', 'x1_tok'], writes=['xo'])
            r0 = tb * 512 + st * 128
            P.op('sp', (lambda r0: lambda e: e.dma_start(out=x2[r0:r0 + 128, :], in_=xo[:]))(r0), reads=['xo'], dma='xo')
            ti += 1
    P.finish()
    P.emit()
    return nc


def layout_C2(inp, l, r, x1T_core):
    b, j = r // 4, r % 4
    f = lambda a: np.ascontiguousarray(a, dtype=np.float32)
    return {
        "x1T": f(x1T_core),
        "cb": f(inp['c'][b].reshape(8, 128).T),
        "adaw": f(inp['ada_w'][l][:, 3072:6144]),
        "adab": f(inp['ada_b'][l][3072:5120].reshape(16, 128).T),
        "g2b": f(inp['ada_b'][l][5120:6144].reshape(1, 1024)),
        "n2w": f(inp['norm2_w'][l].reshape(8, 128).T),
        "wq": f(inp['peer_wq'][l]),
        "kT": f(np.stack([inp['peer_k1'][l].T, inp['peer_k2'][l].T], axis=1)),
        "utab": f(inp['peer_u'][l]),
        "vtab": f(inp['peer_v'][l]),
    }


_CACHE = {}


def _prog(name, fn, *a):
    key = (name,) + a
    if key not in _CACHE:
        _CACHE[key] = fn(*a)
    return _CACHE[key]


def _run(nc, in_maps):
    res = run_bass_kernel_spmd(nc, in_maps, core_ids=list(range(8)))
    return res.results


def kernel(**inp):
    inp = {k: np.asarray(v) for k, v in inp.items()}
    x = inp['x']
    xT_full = [np.ascontiguousarray(x[b].T) for b in range(2)]
    x2 = None
    for l in range(2):
        ra = _run(_prog('A', build_A), [layout_A(inp, l, r, xT_full) for r in range(8)])
        cat = lambda key, b: np.concatenate([ra[b * 4 + j][key] for j in range(4)], axis=1)
        uT_full = [cat('uT', b) for b in range(2)]
        qT_full = [cat('qT', b) for b in range(2)]
        kT_full = [cat('kT', b) for b in range(2)]
        vT_full = [cat('vT', b) for b in range(2)]
        rb1 = _run(_prog('B1', build_B1), [layout_B1(inp, l, r, uT_full) for r in range(8)])
        ysT_full = [np.concatenate([rb1[b * 4 + j]['yT'] for j in range(4)], axis=0) for b in range(2)]
        rb2 = _run(_prog('B2', build_B2, l), [layout_B2(inp, l, r, qT_full, kT_full, vT_full) for r in range(8)])
        oT_full = [np.concatenate([rb2[b * 4 + j]['oT'] for j in range(4)], axis=0) for b in range(2)]
        rc1 = _run(_prog('C1', build_C1), [layout_C1(inp, l, r, xT_full, ysT_full, oT_full, ra[r]['gT']) for r in range(8)])
        rc2 = _run(_prog('C2', build_C2), [layout_C2(inp, l, r, rc1[r]['x1T']) for r in range(8)])
        x2 = [np.concatenate([rc2[b * 4 + j]['x2'] for j in range(4)], axis=0) for b in range(2)]
        xT_full = [np.ascontiguousarray(x2[b].T) for b in range(2)]
    return np.stack(x2, axis=0).astype(np.float32)
```

```python
import math
import numpy as np
import ml_dtypes
from contextlib import ExitStack
import concourse.bass as bass
import concourse.mybir as mybir
from concourse.bass_utils import run_bass_kernel_spmd

F32 = mybir.dt.float32
BF16 = mybir.dt.bfloat16
U32 = mybir.dt.uint32
I32 = mybir.dt.int32
AF = mybir.ActivationFunctionType
ALU = mybir.AluOpType
AX = mybir.AxisListType
NPBF = ml_dtypes.bfloat16


class Prog:
    ENG = ('pe', 'act', 'dve', 'pool', 'sp')

    def __init__(self, nc):
        self.nc = nc
        self.es = ExitStack()
        self.ops = {e: [] for e in self.ENG}
        self.cnt = {e: 0 for e in self.ENG}
        self.sem = {}
        self.dcnt = {}
        self.lastw = {}
        self.rd = {}
        self.waited = {e: {} for e in self.ENG}
        self.ph = ExitStack()
        self.phase_id = 0
        self.barrier = {}

    def _sem(self, name):
        if name not in self.sem:
            self.sem[name] = self.es.enter_context(self.nc.semaphore(name))
        return self.sem[name]

    def sb(self, name, shape, dt):
        return self.ph.enter_context(self.nc.sbuf_tensor(f"p{self.phase_id}_{name}", shape, dt))

    def ps(self, name, shape, dt=F32):
        return self.ph.enter_context(self.nc.psum_tensor(f"p{self.phase_id}_{name}", shape, dt))

    def op(self, eng, fn, reads=(), writes=(), dma=None):
        waits = {}

        def need(tok, hazard):
            s, v, e, isdma = tok
            if not isdma and dma is None and e == eng:
                if hazard != 'raw' or eng == 'pe':
                    return
            if waits.get(s, 0) < v:
                waits[s] = v

        for r in reads:
            if r in self.lastw:
                need(self.lastw[r], 'raw')
        for w in writes:
            if w in self.lastw:
                need(self.lastw[w], 'waw')
            for s, tok in self.rd.get(w, {}).items():
                need(tok, 'war')
        wl = []
        for s, v in waits.items():
            if self.waited[eng].get(s, 0) < v:
                self.waited[eng][s] = v
                wl.append((s, v))
        if dma is None:
            sname = 'e_' + eng
            self._sem(sname)
            self.cnt[eng] += 1
            tok = (sname, self.cnt[eng], eng, False)
            inc = (sname, 1)
        else:
            sname = 'd_' + dma
            self._sem(sname)
            self.dcnt[sname] = self.dcnt.get(sname, 0) + 16
            tok = (sname, self.dcnt[sname], eng, True)
            inc = (sname, 16)
        for w in writes:
            self.lastw[w] = tok
            self.rd[w] = {}
        for r in reads:
            d = self.rd.setdefault(r, {})
            d[tok[0]] = tok
        self.ops[eng].append((wl, fn, inc))

    def finish(self):
        wl = [(s, v) for s, v in self.dcnt.items()]
        self.ops['sp'].append((wl, None, None))

    def end_phase(self, final=False):
        if final:
            self.finish()
        nc = self.nc
        P = self
        barrier = dict(self.barrier)

        def run(name):
            def f(e):
                for s_, v in barrier.items():
                    e.wait_ge(P.sem[s_], v)
                for wl, fn, inc in P.ops[name]:
                    for s_, v in wl:
                        e.wait_ge(P.sem[s_], v)
                    if fn is not None:
                        ins = fn(e)
                        ins.then_inc(P.sem[inc[0]], inc[1])
            return f

        with nc.Block() as block:
            block.tensor(run('pe'))
            block.scalar(run('act'))
            block.vector(run('dve'))
            block.gpsimd(run('pool'))
            block.sync(run('sp'))
        self.ph.close()
        self.ph = ExitStack()
        self.phase_id += 1
        self.ops = {e: [] for e in self.ENG}
        self.barrier = {('e_' + e): self.cnt[e] for e in self.ENG if self.cnt[e] > 0}
        self.barrier.update(self.dcnt)
        if final:
            self.es.close()

    def emit(self):
        self.end_phase(final=False)
        self.es.close()


def mkD(nc, pre, ext):
    def D(n, s, dt, k):
        if ext and n in ext:
            return ext[n]
        return nc.dram_tensor(pre + n, s, dt, kind=k).ap()
    return D


T = 4096
NB = T // 512


def build_A(P=None, pre="", ext=None):
    own = P is None
    if own:
        P = Prog(bass.Bass("TRN2", target_bir_lowering=False))
    nc = P.nc
    D = mkD(nc, pre, ext)
    xT = D("xT", [1024, T], F32, "ExternalInput")
    cb = D("cb", [128, 8], F32, "ExternalInput")
    adaw = D("adaw", [1024, 2048], F32, "ExternalInput")
    adab = D("adab", [128, 16], F32, "ExternalInput")
    n1w = D("n1w", [128, 8], F32, "ExternalInput")
    win = D("win", [1024, 4096], F32, "ExternalInput")
    qkw = D("qkw", [128, 2], F32, "ExternalInput")
    uT = D("uT", [512, T], F32, "ExternalOutput")
    qT = D("qT", [512, T], BF16, "ExternalOutput")
    kT = D("kT", [512, T], BF16, "ExternalOutput")
    vT = D("vT", [512, T], BF16, "ExternalOutput")
    gT = D("gT", [2048, T], F32, "ExternalOutput")

    wbf = P.sb("wbf", [128, 8, 4096], BF16)
    stg = [P.sb(f"stg{i}", [128, 2048], F32) for i in range(2)]
    c_sb = P.sb("c_sb", [128, 8], F32)
    ca_sb = P.sb("ca_sb", [128, 8], F32)
    adab_sb = P.sb("adab_sb", [128, 16], F32)
    n1w_sb = P.sb("n1w_sb", [128, 8], F32)
    qkw_sb = P.sb("qkw_sb", [128, 2], F32)
    modT = P.sb("modT", [128, 16], F32)
    wmod = P.sb("wmod", [128, 8], F32)
    ones = P.sb("ones", [128, 128], F32)
    blk = P.sb("blk", [128, 128], F32)
    epsb = P.sb("epsb", [128, 1], F32)
    xb = [P.sb(f"xb{i}", [128, 8, 512], F32) for i in range(2)]
    sq = [P.sb(f"sq{i}", [128, 512], F32) for i in range(2)]
    rstd = P.sb("rstd", [128, 512], F32)
    tmp = [P.sb(f"tmp{i}", [128, 512], F32) for i in range(2)]
    hT = P.sb("hT", [128, 8, 512], BF16)
    of32 = [P.sb(f"of32_{i}", [128, 512], F32) for i in range(4)]
    obf = [P.sb(f"obf_{i}", [128, 512], BF16) for i in range(4)]
    sq2 = [P.sb(f"sq2_{i}", [128, 512], F32) for i in range(2)]
    r2 = [P.sb(f"r2_{i}", [128, 512], F32) for i in range(2)]
    pz = [P.ps(f"pz{i}", [128, 512]) for i in range(4)]
    pss = [P.ps(f"pss{i}", [128, 512]) for i in range(2)]
    pn = P.ps("pn", [128, 512])
    pm = P.ps("pm", [128, 16])

    P.op('pool', lambda e: e.memset(ones[:], 1.0), writes=['ones'])
    P.op('pool', lambda e: e.memset(blk[:], 0.0), writes=['blk'])
    P.op('pool', lambda e: e.memset(blk[0:64, 0:64], 1.0), writes=['blk'])
    P.op('pool', lambda e: e.memset(blk[64:128, 64:128], 1.0), writes=['blk'])
    P.op('pool', lambda e: e.memset(epsb[:], 1e-6), writes=['epsb'])
    for nm, dst, src in (('c', c_sb, cb), ('adab', adab_sb, adab), ('n1w', n1w_sb, n1w), ('qkw', qkw_sb, qkw)):
        P.op('sp', (lambda dst, src: lambda e: e.dma_start(out=dst[:], in_=src))(dst, src), writes=[nm], dma='small')
    P.op('act', lambda e: e.activation(out=ca_sb[:], in_=c_sb[:], func=AF.Silu), reads=['c'], writes=['ca'])

    for q4 in range(4):
        xs = q4 % 2
        P.op('sp', (lambda xs, q4: lambda e: e.dma_start(out=xb[xs][:], in_=adaw[:, q4 * 512:(q4 + 1) * 512].rearrange("(kc p) t -> p kc t", p=128)))(xs, q4),
             writes=[('xb', xs)], dma=f'xb{xs}')
        for mm in range(4):
            m = q4 * 4 + mm
            for kc in range(8):
                P.op('pe', (lambda xs, kc, m, mm: lambda e: e.matmul(pm[:, m:m + 1], lhsT=xb[xs][:, kc, mm * 128:(mm + 1) * 128],
                                                                  rhs=ca_sb[:, kc:kc + 1], start=(kc == 0), stop=(kc == 7)))(xs, kc, m, mm),
                     reads=[('xb', xs), 'ca'], writes=['pm'])
    P.op('dve', lambda e: e.tensor_tensor(out=modT[:], in0=pm[:], in1=adab_sb[:], op=ALU.add),
         reads=['pm', 'adab'], writes=['modT'])
    P.op('dve', lambda e: e.scalar_tensor_tensor(out=wmod[:], in0=modT[:, 8:16], scalar=1.0, in1=n1w_sb[:],
                                                op0=ALU.add, op1=ALU.mult),
         reads=['modT', 'n1w'], writes=['wmod'])

    for kc in range(8):
        for hh in range(2):
            s = (kc * 2 + hh) % 2
            P.op('sp', (lambda s, kc, hh: lambda e: e.dma_start(out=stg[s][:], in_=win[kc * 128:(kc + 1) * 128, hh * 2048:(hh + 1) * 2048]))(s, kc, hh),
                 writes=[('stg', s)], dma=f'stg{s}')
            P.op('pool', (lambda s, kc, hh: lambda e: e.tensor_copy(out=wbf[:, kc, hh * 2048:(hh + 1) * 2048], in_=stg[s][:]))(s, kc, hh),
                 reads=[('stg', s)], writes=[('wbf', kc, hh)])

    oi = 0
    for tb in range(NB):
        xs = tb % 2
        tsl = slice(tb * 512, (tb + 1) * 512)
        P.op('sp', (lambda xs, tsl: lambda e: e.dma_start(out=xb[xs][:], in_=xT[:, tsl].rearrange("(kc p) t -> p kc t", p=128)))(xs, tsl),
             writes=[('xb', xs)], dma=f'xb{xs}')
        for kc in range(8):
            s = kc % 2
            P.op('act', (lambda xs, kc, s: lambda e: e.activation(out=sq[s][:], in_=xb[xs][:, kc, :], func=AF.Square))(xs, kc, s),
                 reads=[('xb', xs)], writes=[('sq', s)])
            P.op('pe', (lambda kc, s: lambda e: e.matmul(pn[:], lhsT=ones[:], rhs=sq[s][:], start=(kc == 0), stop=(kc == 7)))(kc, s),
                 reads=[('sq', s), 'ones'], writes=['pn'])
        P.op('act', lambda e: e.activation(out=rstd[:], in_=pn[:], func=AF.Sqrt, bias=epsb[:], scale=1.0 / 1024.0),
             reads=['pn', 'epsb'], writes=['rstd'])
        P.op('dve', lambda e: e.reciprocal(out=rstd[:], in_=rstd[:]), reads=['rstd'], writes=['rstd'])
        for kc in range(8):
            s = kc % 2
            P.op('dve', (lambda xs, kc, s: lambda e: e.tensor_tensor(out=tmp[s][:], in0=xb[xs][:, kc, :], in1=rstd[:], op=ALU.mult))(xs, kc, s),
                 reads=[('xb', xs), 'rstd'], writes=[('tmp', s)])
            P.op('act', (lambda kc, s: lambda e: e.activation(out=hT[:, kc, :], in_=tmp[s][:], func=AF.Identity,
                                                             bias=modT[:, kc:kc + 1], scale=wmod[:, kc:kc + 1]))(kc, s),
                 reads=[('tmp', s), 'modT', 'wmod'], writes=[('hT', kc)])
        for m in range(32):
            zs = m % 4
            for kc in range(8):
                P.op('pe', (lambda zs, kc, m: lambda e: e.matmul(pz[zs][:], lhsT=wbf[:, kc, m * 128:(m + 1) * 128], rhs=hT[:, kc, :],
                                                                start=(kc == 0), stop=(kc == 7)))(zs, kc, m),
                     reads=[('hT', kc), ('wbf', kc, m // 16)], writes=[('pz', zs)])
            o = oi % 4
            oi += 1
            msl = slice((m % 4) * 128, (m % 4 + 1) * 128)
            if m < 4:
                P.op('dve', (lambda zs, o: lambda e: e.tensor_copy(out=of32[o][:], in_=pz[zs][:]))(zs, o),
                     reads=[('pz', zs)], writes=[('of32', o)])
                P.op('sp', (lambda o, msl, tsl: lambda e: e.dma_start(out=uT[msl, tsl], in_=of32[o][:]))(o, msl, tsl),
                     reads=[('of32', o)], dma=f'of32_{o}')
            elif m < 12:
                s = m % 2
                wcol = 0 if m < 8 else 1
                dst = qT if m < 8 else kT
                P.op('act', (lambda zs, s: lambda e: e.activation(out=sq2[s][:], in_=pz[zs][:], func=AF.Square))(zs, s),
                     reads=[('pz', zs)], writes=[('sq2', s)])
                P.op('pe', (lambda s: lambda e: e.matmul(pss[s][:], lhsT=blk[:], rhs=sq2[s][:], start=True, stop=True))(s),
                     reads=[('sq2', s), 'blk'], writes=[('pss', s)])
                P.op('act', (lambda s: lambda e: e.activation(out=r2[s][:], in_=pss[s][:], func=AF.Sqrt, bias=epsb[:], scale=1.0 / 64.0))(s),
                     reads=[('pss', s), 'epsb'], writes=[('r2', s)])
                P.op('dve', (lambda s: lambda e: e.reciprocal(out=r2[s][:], in_=r2[s][:]))(s), reads=[('r2', s)], writes=[('r2', s)])
                P.op('dve', (lambda zs, s, o, wcol: lambda e: e.scalar_tensor_tensor(out=obf[o][:], in0=pz[zs][:], scalar=qkw_sb[:, wcol:wcol + 1],
                                                                                    in1=r2[s][:], op0=ALU.mult, op1=ALU.mult))(zs, s, o, wcol),
                     reads=[('pz', zs), ('r2', s), 'qkw'], writes=[('obf', o)])
                P.op('sp', (lambda o, msl, tsl, dst: lambda e: e.dma_start(out=dst[msl, tsl], in_=obf[o][:]))(o, msl, tsl, dst),
                     reads=[('obf', o)], dma=f'obf_{o}')
            elif m < 16:
                P.op('act', (lambda zs, o: lambda e: e.activation(out=obf[o][:], in_=pz[zs][:], func=AF.Identity))(zs, o),
                     reads=[('pz', zs)], writes=[('obf', o)])
                P.op('sp', (lambda o, msl, tsl: lambda e: e.dma_start(out=vT[msl, tsl], in_=obf[o][:]))(o, msl, tsl),
                     reads=[('obf', o)], dma=f'obf_{o}')
            else:
                gsl = slice((m - 16) * 128, (m - 15) * 128)
                P.op('act', (lambda zs, o: lambda e: e.activation(out=of32[o][:], in_=pz[zs][:], func=AF.Sigmoid))(zs, o),
                     reads=[('pz', zs)], writes=[('of32', o)])
                P.op('sp', (lambda o, gsl, tsl: lambda e: e.dma_start(out=gT[gsl, tsl], in_=of32[o][:]))(o, gsl, tsl),
                     reads=[('of32', o)], dma=f'of32_{o}')
    if own:
        P.end_phase(final=True)
        return nc
    return {}


def layout_A(inp, l, r, xT_full):
    b, j = r // 4, r % 4
    f = lambda a: np.ascontiguousarray(a, dtype=np.float32)
    qn = np.tile(inp['q_norm_w'][l], 2)
    kn = np.tile(inp['k_norm_w'][l], 2)
    return {
        "xT": None if xT_full is None else f(xT_full[b][:, j * T:(j + 1) * T]),
        "cb": f(inp['c'][b].reshape(8, 128).T),
        "adaw": f(inp['ada_w'][l][:, 0:2048]),
        "adab": f(inp['ada_b'][l][0:2048].reshape(16, 128).T),
        "n1w": f(inp['norm1_w'][l].reshape(8, 128).T),
        "win": f(inp['w_in'][l]),
        "qkw": f(np.stack([qn, kn], axis=1)),
    }


S = 16384
L = 1024
NSEG = S // L
PI = math.pi


def build_B1(nseg=NSEG, P=None, pre="", ext=None):
    own = P is None
    if own:
        P = Prog(bass.Bass("TRN2", target_bir_lowering=False))
    nc = P.nc
    D = mkD(nc, pre, ext)
    uT = D("uT", [128, S], F32, "ExternalInput")
    prm = D("prm", [128, 12], F32, "ExternalInput")
    bpad = D("bpad", [128, 8, 128], F32, "ExternalInput")
    cpad = D("cpad", [128, 8, 128], F32, "ExternalInput")
    dsk = D("dsk", [128, 1], F32, "ExternalInput")
    yT = D("yT", [128, S], BF16, "ExternalOutput")

    prm_sb = P.sb("prm_sb", [128, 12], F32)
    bpad_sb = P.sb("bpad_sb", [128, 8, 128], F32)
    cpad_sb = P.sb("cpad_sb", [128, 8, 128], F32)
    dsk_sb = P.sb("dsk_sb", [128, 1], F32)
    sm = P.sb("sm", [128, 64], F32)
    npi = P.sb("npi", [128, 1], F32)
    qi_sb = P.sb("qi_sb", [128, 4], I32)
    ident = P.sb("ident", [128, 128], F32)
    iot = P.sb("iot", [128, 128], F32)
    bbp = P.sb("bbp", [128, 8, 128], F32)
    btmp = P.sb("btmp", [128, 128], F32)
    W = P.sb("W", [128, 8, 128], F32)
    Ec = P.sb("Ec", [128, 4, L], F32)
    Es = P.sb("Es", [128, 4, L], F32)
    etmp = P.sb("etmp", [128, L], F32)
    rot = P.sb("rot", [128, 8], F32)
    carry = P.sb("carry", [128, 8], F32)
    ctmp = P.sb("ctmp", [128, 4], F32)
    ub = [P.sb(f"ub{i}", [128, L], F32) for i in range(2)]
    m = [[P.sb(f"m{k}_{i}", [128, 512], F32) for i in range(2)] for k in range(4)]
    win_r = P.sb("win_r", [128, L], F32)
    win_i = P.sb("win_i", [128, L], F32)
    w_r = P.sb("w_r", [128, L], F32)
    w_i = P.sb("w_i", [128, L], F32)
    dd = [P.sb(f"dd{k}", [128, L], F32) for k in range(4)]
    xr = P.sb("xr", [128, 4, L], F32)
    xi = P.sb("xi", [128, 4, L], F32)
    ysb = [P.sb(f"ysb{i}", [128, 512], F32) for i in range(2)]
    gt = [P.sb(f"gt{i}", [128, 512], F32) for i in range(2)]
    gs = [P.sb(f"gs{i}", [128, 512], F32) for i in range(2)]
    yo = [P.sb(f"yo{i}", [128, 512], BF16) for i in range(2)]
    psr = [P.ps(f"psr{i}", [128, 512]) for i in range(2)]
    psi = [P.ps(f"psi{i}", [128, 512]) for i in range(2)]
    psy = [P.ps(f"psy{i}", [128, 512]) for i in range(2)]
    pst = P.ps("pst", [128, 128])

    col = lambda k: sm[:, k:k + 1]
    for nm, dst, src in (('prm', prm_sb, prm), ('bpad', bpad_sb, bpad), ('cpad', cpad_sb, cpad), ('dsk', dsk_sb, dsk)):
        P.op('sp', (lambda dst, src: lambda e: e.dma_start(out=dst[:], in_=src))(dst, src), writes=[nm], dma='small')
    P.op('pool', lambda e: e.memset(npi[:], -PI), writes=['npi'])
    P.op('pool', lambda e: e.memset(carry[:], 0.0), writes=['carry'])
    P.op('pool', lambda e: e.iota(iot[:], pattern=[[1, 128]], base=0, channel_multiplier=-1, allow_small_or_imprecise_dtypes=True), writes=['iot'])
    P.op('dve', lambda e: e.tensor_single_scalar(out=ident[:], in_=iot[:], scalar=0.0, op=ALU.is_equal), reads=['iot'], writes=['ident'])
    P.op('act', lambda e: e.mul(out=cpad_sb[:, 4:8, :], in_=cpad_sb[:, 4:8, :], mul=-1.0), reads=['cpad'], writes=['cpad'])

    c4 = lambda k: sm[:, k:k + 4]
    ldt, are, aim = prm_sb[:, 0:4], prm_sb[:, 4:8], prm_sb[:, 8:12]
    seq = []
    def dv(fn, reads=('sm',), writes=('sm',)):
        P.op('dve', fn, reads=list(reads) + ['prm'], writes=list(writes))
    def ac(fn):
        P.op('act', fn, reads=['sm', 'prm', 'npi'], writes=['sm'])
    ac(lambda e: e.activation(out=c4(0), in_=ldt, func=AF.Exp))
    dv(lambda e: e.tensor_tensor(out=c4(44), in0=are, in1=c4(0), op=ALU.mult))
    ac(lambda e: e.activation(out=c4(4), in_=c4(44), func=AF.Exp))
    dv(lambda e: e.tensor_tensor(out=c4(8), in0=aim, in1=c4(0), op=ALU.mult))
    def red_sin(dst, shift):
        dv(lambda e: e.tensor_scalar_add(out=c4(52), in0=c4(8), scalar1=shift))
        dv(lambda e: e.tensor_scalar_mul(out=c4(44), in0=c4(52), scalar1=1.0 / (2 * PI)))
        P.op('dve', lambda e: e.tensor_copy(out=qi_sb[:], in_=c4(44)), reads=['sm'], writes=['qi'])
        P.op('dve', lambda e: e.tensor_copy(out=c4(44), in_=qi_sb[:]), reads=['qi'], writes=['sm'])
        dv(lambda e: e.scalar_tensor_tensor(out=c4(48), in0=c4(44), scalar=-2 * PI, in1=c4(52), op0=ALU.mult, op1=ALU.add))
        dv(lambda e: e.tensor_single_scalar(out=c4(44), in_=c4(48), scalar=PI, op=ALU.is_gt))
        dv(lambda e: e.scalar_tensor_tensor(out=c4(48), in0=c4(44), scalar=-2 * PI, in1=c4(48), op0=ALU.mult, op1=ALU.add))
        dv(lambda e: e.tensor_single_scalar(out=c4(44), in_=c4(48), scalar=-PI, op=ALU.is_lt))
        dv(lambda e: e.scalar_tensor_tensor(out=c4(48), in0=c4(44), scalar=2 * PI, in1=c4(48), op0=ALU.mult, op1=ALU.add))
        ac(lambda e: e.activation(out=c4(dst), in_=c4(48), func=AF.Sin))
    red_sin(16, 0.0)
    red_sin(12, 0.5 * PI)
    dv(lambda e: e.tensor_tensor(out=c4(20), in0=c4(4), in1=c4(12), op=ALU.mult))
    dv(lambda e: e.tensor_tensor(out=c4(24), in0=c4(4), in1=c4(16), op=ALU.mult))
    dv(lambda e: e.tensor_tensor(out=c4(44), in0=are, in1=are, op=ALU.mult))
    dv(lambda e: e.tensor_tensor(out=c4(48), in0=aim, in1=aim, op=ALU.mult))
    dv(lambda e: e.tensor_tensor(out=c4(28), in0=c4(44), in1=c4(48), op=ALU.add))
    dv(lambda e: e.reciprocal(out=c4(28), in_=c4(28)))
    dv(lambda e: e.tensor_scalar_add(out=c4(32), in0=c4(20), scalar1=-1.0))
    dv(lambda e: e.tensor_tensor(out=c4(44), in0=c4(32), in1=are, op=ALU.mult))
    dv(lambda e: e.tensor_tensor(out=c4(48), in0=c4(24), in1=aim, op=ALU.mult))
    dv(lambda e: e.tensor_tensor(out=c4(44), in0=c4(44), in1=c4(48), op=ALU.add))
    dv(lambda e: e.tensor_tensor(out=c4(36), in0=c4(44), in1=c4(28), op=ALU.mult))
    dv(lambda e: e.tensor_tensor(out=c4(44), in0=c4(24), in1=are, op=ALU.mult))
    dv(lambda e: e.tensor_tensor(out=c4(48), in0=c4(32), in1=aim, op=ALU.mult))
    dv(lambda e: e.tensor_tensor(out=c4(44), in0=c4(44), in1=c4(48), op=ALU.subtract))
    dv(lambda e: e.tensor_tensor(out=c4(40), in0=c4(44), in1=c4(28), op=ALU.mult))

    for gp in range(4):
        fre, fim = col(36 + gp), col(40 + gp)
        P.op('dve', (lambda gp, fim: lambda e: e.tensor_scalar_mul(out=btmp[:], in0=bpad_sb[:, 4 + gp, :], scalar1=fim))(gp, fim),
             reads=['sm', 'bpad'], writes=['btmp'])
        P.op('dve', (lambda gp, fre: lambda e: e.scalar_tensor_tensor(out=bbp[:, gp, :], in0=bpad_sb[:, gp, :], scalar=fre, in1=btmp[:],
                                                                      op0=ALU.mult, op1=ALU.subtract))(gp, fre),
             reads=['sm', 'bpad', 'btmp'], writes=[('bbp', gp)])
        P.op('dve', (lambda gp, fim: lambda e: e.tensor_scalar_mul(out=btmp[:], in0=bpad_sb[:, gp, :], scalar1=fim))(gp, fim),
             reads=['sm', 'bpad'], writes=['btmp'])
        P.op('dve', (lambda gp, fre: lambda e: e.scalar_tensor_tensor(out=bbp[:, 4 + gp, :], in0=bpad_sb[:, 4 + gp, :], scalar=fre, in1=btmp[:],
                                                                      op0=ALU.mult, op1=ALU.add))(gp, fre),
             reads=['sm', 'bpad', 'btmp'], writes=[('bbp', 4 + gp)])
        for k in (gp, 4 + gp):
            P.op('pe', (lambda k: lambda e: e.transpose(out=pst[:], in_=bbp[:, k, :], identity=ident[:]))(k),
                 reads=[('bbp', k), 'ident'], writes=['pst'])
            P.op('act', (lambda k: lambda e: e.copy(out=W[:, k, :], in_=pst[:]))(k), reads=['pst'], writes=[('W', k)])

    for gp in range(4):
        c1, s1 = col(12 + gp), col(16 + gp)
        ec, es = Ec[:, gp, :], Es[:, gp, :]
        tg = ('E', gp)
        P.op('pool', (lambda ec: lambda e: e.memset(ec[:, 0:1], 1.0))(ec), writes=[tg])
        P.op('pool', (lambda es: lambda e: e.memset(es[:, 0:1], 0.0))(es), writes=[tg])
        P.op('dve', (lambda ec, c1: lambda e: e.tensor_copy(out=ec[:, 1:2], in_=c1))(ec, c1), reads=['sm', tg], writes=[tg])
        P.op('dve', (lambda es, s1: lambda e: e.tensor_copy(out=es[:, 1:2], in_=s1))(es, s1), reads=['sm', tg], writes=[tg])
        n = 2
        while n <= L:
            P.op('dve', (lambda es, n, s1: lambda e: e.tensor_tensor(out=ctmp[:, 2:3], in0=es[:, n - 1:n], in1=s1, op=ALU.mult))(es, n, s1),
                 reads=[tg, 'sm'], writes=['ctmp'])
            P.op('dve', (lambda ec, n, c1: lambda e: e.scalar_tensor_tensor(out=ctmp[:, 0:1], in0=ec[:, n - 1:n], scalar=c1, in1=ctmp[:, 2:3],
                                                                           op0=ALU.mult, op1=ALU.subtract))(ec, n, c1),
                 reads=[tg, 'sm', 'ctmp'], writes=['ctmp'])
            P.op('dve', (lambda es, n, c1: lambda e: e.tensor_tensor(out=ctmp[:, 2:3], in0=es[:, n - 1:n], in1=c1, op=ALU.mult))(es, n, c1),
                 reads=[tg, 'sm', 'ctmp'], writes=['ctmp'])
            P.op('dve', (lambda ec, n, s1: lambda e: e.scalar_tensor_tensor(out=ctmp[:, 1:2], in0=ec[:, n - 1:n], scalar=s1, in1=ctmp[:, 2:3],
                                                                           op0=ALU.mult, op1=ALU.add))(ec, n, s1),
                 reads=[tg, 'sm', 'ctmp'], writes=['ctmp'])
            if n == L:
                P.op('dve', (lambda gp: lambda e: e.tensor_copy(out=rot[:, gp:gp + 1], in_=ctmp[:, 0:1]))(gp), reads=['ctmp'], writes=['rot'])
                P.op('dve', (lambda gp: lambda e: e.tensor_copy(out=rot[:, 4 + gp:5 + gp], in_=ctmp[:, 1:2]))(gp), reads=['ctmp'], writes=['rot'])
                break
            P.op('dve', (lambda es, n: lambda e: e.tensor_scalar_mul(out=etmp[:, 0:n], in0=es[:, 0:n], scalar1=ctmp[:, 1:2]))(es, n),
                 reads=[tg, 'ctmp'], writes=['etmp'])
            P.op('dve', (lambda ec, n: lambda e: e.scalar_tensor_tensor(out=ec[:, n:2 * n], in0=ec[:, 0:n], scalar=ctmp[:, 0:1], in1=etmp[:, 0:n],
                                                                       op0=ALU.mult, op1=ALU.subtract))(ec, n),
                 reads=[tg, 'ctmp', 'etmp'], writes=[tg])
            P.op('dve', (lambda ec, n: lambda e: e.tensor_scalar_mul(out=etmp[:, 0:n], in0=ec[:, 0:n], scalar1=ctmp[:, 1:2]))(ec, n),
                 reads=[tg, 'ctmp'], writes=['etmp'])
            P.op('dve', (lambda es, n: lambda e: e.scalar_tensor_tensor(out=es[:, n:2 * n], in0=es[:, 0:n], scalar=ctmp[:, 0:1], in1=etmp[:, 0:n],
                                                                       op0=ALU.mult, op1=ALU.add))(es, n),
                 reads=[tg, 'ctmp', 'etmp'], writes=[tg])
            n *= 2

    mi = 0
    yi = 0
    for seg in range(nseg):
        us = seg % 2
        ssl = slice(seg * L, (seg + 1) * L)
        P.op('sp', (lambda us, ssl: lambda e: e.dma_start(out=ub[us][:], in_=uT[:, ssl]))(us, ssl), writes=[('ub', us)], dma=f'ub{us}')
        for gp in range(4):
            tg = ('E', gp)
            for tb in range(L // 512):
                bs = mi % 2
                mi += 1
                fs = slice(tb * 512, (tb + 1) * 512)
                P.op('pe', (lambda gp, us, fs, bs: lambda e: e.matmul(psr[bs][:], lhsT=W[:, gp, :], rhs=ub[us][:, fs], start=True, stop=True))(gp, us, fs, bs),
                     reads=[('W', gp), ('ub', us)], writes=[('psr', bs)])
                P.op('pe', (lambda gp, us, fs, bs: lambda e: e.matmul(psi[bs][:], lhsT=W[:, 4 + gp, :], rhs=ub[us][:, fs], start=True, stop=True))(gp, us, fs, bs),
                     reads=[('W', 4 + gp), ('ub', us)], writes=[('psi', bs)])
                ecs, ess = Ec[:, gp, fs], Es[:, gp, fs]
                for k, (src, srcn, tab) in enumerate(((psr, 'psr', ecs), (psi, 'psi', ess), (psi, 'psi', ecs), (psr, 'psr', ess))):
                    P.op('dve', (lambda k, src, tab, bs: lambda e: e.tensor_tensor(out=m[k][bs][:], in0=src[bs][:], in1=tab, op=ALU.mult))(k, src, tab, bs),
                         reads=[(srcn, bs), tg], writes=[('m', k, bs)])
                P.op('pool', (lambda bs, fs: lambda e: e.tensor_tensor(out=win_r[:, fs], in0=m[0][bs][:], in1=m[1][bs][:], op=ALU.add))(bs, fs),
                     reads=[('m', 0, bs), ('m', 1, bs)], writes=[('win_r', tb)])
                P.op('pool', (lambda bs, fs: lambda e: e.tensor_tensor(out=win_i[:, fs], in0=m[2][bs][:], in1=m[3][bs][:], op=ALU.subtract))(bs, fs),
                     reads=[('m', 2, bs), ('m', 3, bs)], writes=[('win_i', tb)])
            rho = sm[:, 4 + gp:5 + gp].to_broadcast([128, L])
            P.op('dve', (lambda gp, rho: lambda e: e.tensor_tensor_scan(out=w_r[:], data0=rho, data1=win_r[:], initial=carry[:, gp:gp + 1],
                                                                       op0=ALU.mult, op1=ALU.add))(gp, rho),
                 reads=[('win_r', 0), ('win_r', 1), 'sm', 'carry'], writes=['w_r'])
            P.op('dve', (lambda gp, rho: lambda e: e.tensor_tensor_scan(out=w_i[:], data0=rho, data1=win_i[:], initial=carry[:, 4 + gp:5 + gp],
                                                                       op0=ALU.mult, op1=ALU.add))(gp, rho),
                 reads=[('win_i', 0), ('win_i', 1), 'sm', 'carry'], writes=['w_i'])
            rc, rs_ = rot[:, gp:gp + 1], rot[:, 4 + gp:5 + gp]
            P.op('dve', (lambda rs_: lambda e: e.tensor_tensor(out=ctmp[:, 0:1], in0=w_i[:, L - 1:L], in1=rs_, op=ALU.mult))(rs_),
                 reads=['w_i', 'rot'], writes=['ctmp'])
            P.op('dve', (lambda rc: lambda e: e.tensor_tensor(out=ctmp[:, 1:2], in0=w_i[:, L - 1:L], in1=rc, op=ALU.mult))(rc),
                 reads=['w_i', 'rot'], writes=['ctmp'])
            P.op('dve', (lambda gp, rc: lambda e: e.scalar_tensor_tensor(out=carry[:, gp:gp + 1], in0=w_r[:, L - 1:L], scalar=rc, in1=ctmp[:, 0:1],
                                                                        op0=ALU.mult, op1=ALU.subtract))(gp, rc),
                 reads=['w_r', 'rot', 'ctmp'], writes=['carry'])
            P.op('dve', (lambda gp, rs_: lambda e: e.scalar_tensor_tensor(out=carry[:, 4 + gp:5 + gp], in0=w_r[:, L - 1:L], scalar=rs_, in1=ctmp[:, 1:2],
                                                                         op0=ALU.mult, op1=ALU.add))(gp, rs_),
                 reads=['w_r', 'rot', 'ctmp'], writes=['carry'])
            ecl, esl = Ec[:, gp, :], Es[:, gp, :]
            P.op('pool', (lambda ecl: lambda e: e.tensor_tensor(out=dd[0][:], in0=w_r[:], in1=ecl, op=ALU.mult))(ecl), reads=['w_r', tg], writes=[('dd', 0)])
            P.op('pool', (lambda esl: lambda e: e.tensor_tensor(out=dd[1][:], in0=w_i[:], in1=esl, op=ALU.mult))(esl), reads=['w_i', tg], writes=[('dd', 1)])
            P.op('pool', (lambda gp: lambda e: e.tensor_tensor(out=xr[:, gp, :], in0=dd[0][:], in1=dd[1][:], op=ALU.subtract))(gp),
                 reads=[('dd', 0), ('dd', 1)], writes=[('xr', gp)])
            P.op('pool', (lambda esl: lambda e: e.tensor_tensor(out=dd[2][:], in0=w_r[:], in1=esl, op=ALU.mult))(esl), reads=['w_r', tg], writes=[('dd', 2)])
            P.op('pool', (lambda ecl: lambda e: e.tensor_tensor(out=dd[3][:], in0=w_i[:], in1=ecl, op=ALU.mult))(ecl), reads=['w_i', tg], writes=[('dd', 3)])
            P.op('pool', (lambda gp: lambda e: e.tensor_tensor(out=xi[:, gp, :], in0=dd[2][:], in1=dd[3][:], op=ALU.add))(gp),
                 reads=[('dd', 2), ('dd', 3)], writes=[('xi', gp)])
        for tb in range(L // 512):
            ys = yi % 2
            yi += 1
            fs = slice(tb * 512, (tb + 1) * 512)
            for gp in range(4):
                P.op('pe', (lambda gp, fs, ys: lambda e: e.matmul(psy[ys][:], lhsT=cpad_sb[:, gp, :], rhs=xr[:, gp, fs], start=(gp == 0), stop=False))(gp, fs, ys),
                     reads=['cpad', ('xr', gp)], writes=[('psy', ys)])
                P.op('pe', (lambda gp, fs, ys: lambda e: e.matmul(psy[ys][:], lhsT=cpad_sb[:, 4 + gp, :], rhs=xi[:, gp, fs], start=False, stop=(gp == 3)))(gp, fs, ys),
                     reads=['cpad', ('xi', gp)], writes=[('psy', ys)])
            P.op('dve', (lambda us, fs, ys: lambda e: e.scalar_tensor_tensor(out=ysb[ys][:], in0=ub[us][:, fs], scalar=dsk_sb[:, 0:1], in1=psy[ys][:],
                                                                            op0=ALU.mult, op1=ALU.add))(us, fs, ys),
                 reads=[('ub', us), 'dsk', ('psy', ys)], writes=[('ysb', ys)])
            P.op('pool', (lambda ys: lambda e: e.tensor_tensor(out=gt[ys][:], in0=ysb[ys][:], in1=ysb[ys][:], op=ALU.mult))(ys),
                 reads=[('ysb', ys)], writes=[('gt', ys)])
            P.op('pool', (lambda ys: lambda e: e.tensor_scalar(out=gt[ys][:], in0=gt[ys][:], scalar1=0.044715, scalar2=1.0, op0=ALU.mult, op1=ALU.add))(ys),
                 reads=[('gt', ys)], writes=[('gt', ys)])
            P.op('pool', (lambda ys: lambda e: e.tensor_tensor(out=gt[ys][:], in0=gt[ys][:], in1=ysb[ys][:], op=ALU.mult))(ys),
                 reads=[('gt', ys), ('ysb', ys)], writes=[('gt', ys)])
            P.op('act', (lambda ys: lambda e: e.activation(out=gs[ys][:], in_=gt[ys][:], func=AF.Sigmoid, scale=2.0 * math.sqrt(2.0 / PI)))(ys),
                 reads=[('gt', ys)], writes=[('gs', ys)])
            P.op('dve', (lambda ys: lambda e: e.tensor_tensor(out=yo[ys][:], in0=ysb[ys][:], in1=gs[ys][:], op=ALU.mult))(ys),
                 reads=[('ysb', ys), ('gs', ys)], writes=[('yo', ys)])
            osl = slice(seg * L + tb * 512, seg * L + (tb + 1) * 512)
            P.op('sp', (lambda ys, osl: lambda e: e.dma_start(out=yT[:, osl], in_=yo[ys][:]))(ys, osl), reads=[('yo', ys)], dma=f'yo{ys}')
    if own:
        P.end_phase(final=True)
        return nc
    return {}


def layout_B1(inp, l, r, uT_full):
    b, gb = r // 4, r % 4
    f = lambda a: np.ascontiguousarray(a, dtype=np.float32)
    g0 = gb * 8
    prm = np.zeros((128, 12), np.float32)
    bpad = np.zeros((128, 8, 128), np.float32)
    cpad = np.zeros((128, 8, 128), np.float32)
    for gp in range(4):
        for gi in range(2):
            gl = 2 * gp + gi
            g = g0 + gl
            ps = slice(gi * 64, (gi + 1) * 64)
            prm[ps, gp] = inp['ssm_log_dt'][l][g]
            prm[ps, 4 + gp] = inp['ssm_a_re'][l][g]
            prm[ps, 8 + gp] = inp['ssm_a_im'][l][g]
            cs = slice(gl * 16, (gl + 1) * 16)
            bpad[ps, gp, cs] = inp['ssm_b_re'][l][g]
            bpad[ps, 4 + gp, cs] = inp['ssm_b_im'][l][g]
            cpad[ps, gp, cs] = inp['ssm_c_re'][l][g].T
            cpad[ps, 4 + gp, cs] = inp['ssm_c_im'][l][g].T
    return {
        "uT": f(uT_full[b][gb * 128:(gb + 1) * 128, :]),
        "prm": prm, "bpad": bpad, "cpad": cpad,
        "dsk": f(inp['ssm_d'][l].reshape(512)[gb * 128:(gb + 1) * 128].reshape(128, 1)),
    }


S = 16384


def build_B2(l, nqb=32, P=None, pre="", ext=None):
    lam_init = 0.8 - 0.6 * math.exp(-0.3 * l)
    own = P is None
    if own:
        P = Prog(bass.Bass("TRN2", target_bir_lowering=False))
    nc = P.nc
    D = mkD(nc, pre, ext)
    qT = D("qT", [128, S], BF16, "ExternalInput")
    kT = D("kT", [128, S], BF16, "ExternalInput")
    V = D("V", [128, 128, 128], BF16, "ExternalInput")
    lqk = D("lqk", [64, 4], F32, "ExternalInput")
    subw = D("subw", [128, 1], F32, "ExternalInput")
    oT = D("oT", [128, S], BF16, "ExternalOutput")

    kT_sb = P.sb("kT_sb", [128, S], BF16)
    V_sb = P.sb("V_sb", [128, 128, 128], BF16)
    qb_sb = [P.sb(f"qb{i}", [128, 512], BF16) for i in range(2)]
    lqk_sb = P.sb("lqk_sb", [64, 4], F32)
    subw_sb = P.sb("subw_sb", [128, 1], F32)
    prods = P.sb("prods", [64, 2], F32)
    E = P.sb("E", [128, 2], F32)
    nlam = P.sb("nlam", [128, 1], F32)
    onesf = P.sb("onesf", [128, 128], F32)
    epsb = P.sb("epsb", [128, 1], F32)
    iot = P.sb("iot", [128, 512], F32)
    masks = [P.sb(f"mask{j}", [128, 512], BF16) for j in range(4)]
    NPT = 6
    NPS = 3
    LA = 2
    pT = [P.sb(f"pT{i}", [128, 512], BF16) for i in range(NPT)]
    acc = [P.sb(f"acc{c}", [128, 512], F32) for c in range(2)]
    rl = [P.sb(f"rl{c}", [128, 512], F32) for c in range(2)]
    t01 = [P.sb(f"t01_{c}", [128, 512], F32) for c in range(2)]
    o_sb = P.sb("o_sb", [128, 512], F32)
    osq = P.sb("osq", [128, 512], F32)
    rs = P.sb("rs", [128, 512], F32)
    on = [P.sb(f"on{i}", [128, 512], BF16) for i in range(2)]
    ps_s = [P.ps(f"ps_s{i}", [128, 512]) for i in range(NPS)]
    ps_o = [[P.ps(f"ps_o{c}_{i}", [128, 512]) for i in range(2)] for c in range(2)]
    ps_l = P.ps("ps_l", [128, 512])

    P.op('pool', lambda e: e.memset(onesf[:], 1.0), writes=['onesf'])
    P.op('pool', lambda e: e.memset(epsb[:], 1e-6), writes=['epsb'])
    P.op('pool', lambda e: e.iota(iot[:], pattern=[[1, 512]], base=0, channel_multiplier=-1,
                                  allow_small_or_imprecise_dtypes=True), writes=['iot'])
    for j in range(4):
        P.op('dve', (lambda j: lambda e: e.tensor_single_scalar(out=masks[j][:], in_=iot[:], scalar=float(128 * j), op=ALU.is_ge))(j),
             reads=['iot'], writes=[('mask', j)])
    P.op('sp', lambda e: e.dma_start(out=lqk_sb[:], in_=lqk), writes=['lqk'], dma='small')
    P.op('sp', lambda e: e.dma_start(out=subw_sb[:], in_=subw), writes=['subw'], dma='small')
    for i in range(4):
        P.op('sp', (lambda i: lambda e: e.dma_start(out=kT_sb[:, i * 4096:(i + 1) * 4096], in_=kT[:, i * 4096:(i + 1) * 4096]))(i),
             writes=[('kT', i)], dma='kT')
        P.op('sp', (lambda i: lambda e: e.dma_start(out=V_sb[:, i * 32:(i + 1) * 32, :], in_=V[:, i * 32:(i + 1) * 32, :]))(i),
             writes=[('V', i)], dma='V')
    P.op('dve', lambda e: e.tensor_tensor(out=prods[:, 0:1], in0=lqk_sb[:, 0:1], in1=lqk_sb[:, 1:2], op=ALU.mult),
         reads=['lqk'], writes=['prods'])
    P.op('dve', lambda e: e.tensor_tensor(out=prods[:, 1:2], in0=lqk_sb[:, 2:3], in1=lqk_sb[:, 3:4], op=ALU.mult),
         reads=['lqk'], writes=['prods'])
    P.op('pe', lambda e: e.matmul(ps_l[:, 0:2], lhsT=onesf[0:64, :], rhs=prods[:], start=True, stop=True),
         reads=['prods', 'onesf'], writes=['ps_l'])
    P.op('act', lambda e: e.activation(out=E[:], in_=ps_l[:, 0:2], func=AF.Exp), reads=['ps_l'], writes=['E'])
    P.op('dve', lambda e: e.tensor_tensor(out=nlam[:], in0=E[:, 1:2], in1=E[:, 0:1], op=ALU.subtract), reads=['E'], writes=['nlam'])
    P.op('dve', lambda e: e.tensor_scalar_add(out=nlam[:], in0=nlam[:], scalar1=-lam_init), reads=['nlam'], writes=['nlam'])

    steps = []
    for qb in range(nqb):
        nkt = 4 * (qb + 1)
        for kt in range(nkt):
            for c in range(2):
                steps.append((qb, kt, c, nkt))
    n = len(steps)

    def front(si):
        qb, kt, c, nkt = steps[si]
        qs = qb % 2
        if kt == 0 and c == 0:
            qsl = slice(qb * 512, (qb + 1) * 512)
            P.op('sp', (lambda qs, qsl: lambda e: e.dma_start(out=qb_sb[qs][:], in_=qT[:, qsl]))(qs, qsl),
                 writes=[('qb', qs)], dma=f'qb{qs}')
        jd = kt - 4 * qb
        sb_i = si % NPS
        pi = si % NPT
        csl = slice(c * 64, (c + 1) * 64)
        P.op('pe', (lambda sb_i, kt, qs, csl: lambda e: e.matmul(ps_s[sb_i][:], lhsT=kT_sb[csl, kt * 128:(kt + 1) * 128],
                                                                rhs=qb_sb[qs][csl, :], start=True, stop=True))(sb_i, kt, qs, csl),
             reads=[('kT', kt // 32), ('qb', qs)], writes=[('ps_s', sb_i)])
        P.op('act', (lambda sb_i, pi: lambda e: e.activation(out=pT[pi][:], in_=ps_s[sb_i][:], func=AF.Exp, scale=0.125))(sb_i, pi),
             reads=[('ps_s', sb_i)], writes=[('pT', pi)])
        if jd >= 0:
            P.op('pool', (lambda pi, jd: lambda e: e.tensor_tensor(out=pT[pi][:], in0=pT[pi][:], in1=masks[jd][:], op=ALU.mult))(pi, jd),
                 reads=[('pT', pi), ('mask', jd)], writes=[('pT', pi)])

    def back(si):
        qb, kt, c, nkt = steps[si]
        pi = si % NPT
        ob_ = qb % 2
        P.op('pe', (lambda c, ob_, pi, kt, nkt: lambda e: e.matmul(ps_o[c][ob_][:], lhsT=V_sb[:, kt, :], rhs=pT[pi][:],
                                                                  start=(kt == 0), stop=(kt == nkt - 1)))(c, ob_, pi, kt, nkt),
             reads=[('pT', pi), ('V', kt // 32)], writes=[('ps_o', c, ob_)])
        ae = 'dve' if c == 0 else 'pool'
        if kt == 0:
            P.op(ae, (lambda c, pi: lambda e: e.tensor_copy(out=acc[c][:], in_=pT[pi][:]))(c, pi),
                 reads=[('pT', pi)], writes=[('acc', c)])
        else:
            P.op(ae, (lambda c, pi: lambda e: e.tensor_tensor(out=acc[c][:], in0=acc[c][:], in1=pT[pi][:], op=ALU.add))(c, pi),
                 reads=[('pT', pi), ('acc', c)], writes=[('acc', c)])
        if kt == nkt - 1 and c == 1:
            epilogue(qb)

    def epilogue(qb):
        ob_ = qb % 2
        qsl = slice(qb * 512, (qb + 1) * 512)
        for c in range(2):
            P.op('pe', (lambda c: lambda e: e.matmul(ps_l[:], lhsT=onesf[:], rhs=acc[c][:], start=True, stop=True))(c),
                 reads=[('acc', c), 'onesf'], writes=['ps_l'])
            P.op('dve', (lambda c: lambda e: e.reciprocal(out=rl[c][:], in_=ps_l[:]))(c), reads=['ps_l'], writes=[('rl', c)])
            P.op('dve', (lambda c, ob_: lambda e: e.tensor_tensor(out=t01[c][:], in0=ps_o[c][ob_][:], in1=rl[c][:], op=ALU.mult))(c, ob_),
                 reads=[('ps_o', c, ob_), ('rl', c)], writes=[('t01', c)])
        P.op('dve', lambda e: e.scalar_tensor_tensor(out=o_sb[:], in0=t01[1][:], scalar=nlam[:, 0:1], in1=t01[0][:], op0=ALU.mult, op1=ALU.add),
             reads=[('t01', 0), ('t01', 1), 'nlam'], writes=['o_sb'])
        P.op('act', lambda e: e.activation(out=osq[:], in_=o_sb[:], func=AF.Square), reads=['o_sb'], writes=['osq'])
        P.op('pe', lambda e: e.matmul(ps_l[:], lhsT=onesf[:], rhs=osq[:], start=True, stop=True),
             reads=['osq', 'onesf'], writes=['ps_l'])
        P.op('act', lambda e: e.activation(out=rs[:], in_=ps_l[:], func=AF.Sqrt, bias=epsb[:], scale=1.0 / 128.0),
             reads=['ps_l', 'epsb'], writes=['rs'])
        P.op('dve', lambda e: e.reciprocal(out=rs[:], in_=rs[:]), reads=['rs'], writes=['rs'])
        P.op('dve', lambda e: e.tensor_scalar_mul(out=rs[:], in0=rs[:], scalar1=1.0 - lam_init), reads=['rs'], writes=['rs'])
        P.op('dve', (lambda ob_: lambda e: e.scalar_tensor_tensor(out=on[ob_][:], in0=o_sb[:], scalar=subw_sb[:, 0:1], in1=rs[:], op0=ALU.mult, op1=ALU.mult))(ob_),
             reads=['o_sb', 'rs', 'subw'], writes=[('on', ob_)])
        P.op('sp', (lambda ob_, qsl: lambda e: e.dma_start(out=oT[:, qsl], in_=on[ob_][:]))(ob_, qsl),
             reads=[('on', ob_)], dma=f'on{ob_}')

    for si in range(n + LA):
        if si < n:
            front(si)
        if si - LA >= 0:
            back(si - LA)
    if own:
        P.end_phase(final=True)
        return nc
    return {}


def layout_B2(inp, l, r, qT_full, kT_full, vT_full):
    b, h = r // 4, r % 4
    hs = slice(h * 128, (h + 1) * 128)
    Vh = np.ascontiguousarray(vT_full[b][hs, :].T)
    Vl = np.ascontiguousarray(Vh.reshape(128, 128, 128).transpose(1, 0, 2))
    f = lambda a: np.ascontiguousarray(a, dtype=np.float32)
    return {
        "qT": np.ascontiguousarray(qT_full[b][hs, :]),
        "kT": np.ascontiguousarray(kT_full[b][hs, :]),
        "V": Vl,
        "lqk": f(np.stack([inp['lambda_q1'][l], inp['lambda_k1'][l], inp['lambda_q2'][l], inp['lambda_k2'][l]], axis=1)),
        "subw": f(inp['subln_w'][l].reshape(128, 1)),
    }


T = 4096
NB = T // 512


def build_C1(P=None, pre="", ext=None):
    own = P is None
    if own:
        P = Prog(bass.Bass("TRN2", target_bir_lowering=False))
    nc = P.nc
    D = mkD(nc, pre, ext)
    xT = D("xT", [1024, T], F32, "ExternalInput")
    ysT = D("ysT", [512, T], BF16, "ExternalInput")
    oT = D("oT", [512, T], BF16, "ExternalInput")
    gT = D("gT", [2048, T], F32, "ExternalInput")
    cb = D("cb", [128, 8], F32, "ExternalInput")
    adaw = D("adaw", [1024, 1024], F32, "ExternalInput")
    adab = D("adab", [128, 8], F32, "ExternalInput")
    gluw = D("gluw", [512, 2048], F32, "ExternalInput")
    upw = D("upw", [512, 1024], F32, "ExternalInput")
    wout = D("wout", [1024, 1024], F32, "ExternalInput")
    x1T = D("x1T", [1024, T], F32, "ExternalOutput")

    glu_bf = P.sb("glu_bf", [128, 4, 2048], BF16)
    up_bf = P.sb("up_bf", [128, 4, 1024], BF16)
    wo_bf = P.sb("wo_bf", [128, 8, 1024], BF16)
    stg = [P.sb(f"stg{i}", [128, 2048], F32) for i in range(2)]
    c_sb = P.sb("c_sb", [128, 8], F32)
    ca_sb = P.sb("ca_sb", [128, 8], F32)
    adab_sb = P.sb("adab_sb", [128, 8], F32)
    g1 = P.sb("g1", [128, 8], F32)
    xb = [P.sb(f"xb{i}", [128, 8, 512], F32) for i in range(2)]
    ysb = [P.sb(f"ysb{i}", [128, 4, 512], BF16) for i in range(2)]
    ob = [P.sb(f"ob{i}", [128, 4, 512], BF16) for i in range(2)]
    gb = [P.sb(f"gb{i}", [128, 2, 512], F32) for i in range(2)]
    sg = [P.sb(f"sg{i}", [128, 512], F32) for i in range(2)]
    t1 = [P.sb(f"t1_{i}", [128, 512], F32) for i in range(2)]
    t2 = [P.sb(f"t2_{i}", [128, 512], F32) for i in range(2)]
    mg = P.sb("mg", [128, 8, 512], BF16)
    xo = [P.sb(f"xo{i}", [128, 512], F32) for i in range(2)]
    pb = [P.ps(f"pb{i}", [128, 512]) for i in range(7)]
    pm = P.ps("pm", [128, 8])
    bank = [0]

    def nb():
        bank[0] = (bank[0] + 1) % 7
        return bank[0]

    for nm, dst, src in (('c', c_sb, cb), ('adab', adab_sb, adab)):
        P.op('sp', (lambda dst, src: lambda e: e.dma_start(out=dst[:], in_=src))(dst, src), writes=[nm], dma='small')
    P.op('act', lambda e: e.activation(out=ca_sb[:], in_=c_sb[:], func=AF.Silu), reads=['c'], writes=['ca'])
    for q2 in range(2):
        xs = q2 % 2
        P.op('sp', (lambda xs, q2: lambda e: e.dma_start(out=xb[xs][:], in_=adaw[:, q2 * 512:(q2 + 1) * 512].rearrange("(kc p) t -> p kc t", p=128)))(xs, q2),
             writes=[('xb', xs)], dma=f'xb{xs}')
        for mm in range(4):
            m = q2 * 4 + mm
            for kc in range(8):
                P.op('pe', (lambda xs, kc, m, mm: lambda e: e.matmul(pm[:, m:m + 1], lhsT=xb[xs][:, kc, mm * 128:(mm + 1) * 128],
                                                                  rhs=ca_sb[:, kc:kc + 1], start=(kc == 0), stop=(kc == 7)))(xs, kc, m, mm),
                     reads=[('xb', xs), 'ca'], writes=['pm'])
    P.op('dve', lambda e: e.tensor_tensor(out=g1[:], in0=pm[:], in1=adab_sb[:], op=ALU.add), reads=['pm', 'adab'], writes=['g1'])

    si = 0
    def load_w(src, rows_kc, ncols, dst, tag):
        nonlocal si
        for kc in range(rows_kc):
            for c0 in range(0, ncols, 2048):
                cw = min(2048, ncols - c0)
                s = si % 2
                si += 1
                P.op('sp', (lambda s, kc, c0, cw: lambda e: e.dma_start(out=stg[s][:, 0:cw], in_=src[kc * 128:(kc + 1) * 128, c0:c0 + cw]))(s, kc, c0, cw),
                     writes=[('stg', s)], dma=f'stg{s}')
                P.op('pool', (lambda s, kc, c0, cw: lambda e: e.tensor_copy(out=dst[:, kc, c0:c0 + cw], in_=stg[s][:, 0:cw]))(s, kc, c0, cw),
                     reads=[('stg', s)], writes=[tag])
    load_w(gluw, 4, 2048, glu_bf, 'glu')
    load_w(upw, 4, 1024, up_bf, 'up')
    load_w(wout, 8, 1024, wo_bf, 'wo')

    gi = 0
    oi = 0
    for tb in range(NB):
        xs = tb % 2
        tsl = slice(tb * 512, (tb + 1) * 512)
        P.op('sp', (lambda xs, tsl: lambda e: e.dma_start(out=xb[xs][:], in_=xT[:, tsl].rearrange("(kc p) t -> p kc t", p=128)))(xs, tsl),
             writes=[('xb', xs)], dma=f'xb{xs}')
        P.op('sp', (lambda xs, tsl: lambda e: e.dma_start(out=ysb[xs][:], in_=ysT[:, tsl].rearrange("(kc p) t -> p kc t", p=128)))(xs, tsl),
             writes=[('ysb', xs)], dma=f'ysb{xs}')
        P.op('sp', (lambda xs, tsl: lambda e: e.dma_start(out=ob[xs][:], in_=oT[:, tsl].rearrange("(kc p) t -> p kc t", p=128)))(xs, tsl),
             writes=[('ob', xs)], dma=f'ob{xs}')
        for m in range(8):
            gs_ = gi % 2
            gi += 1
            P.op('sp', (lambda gs_, m, tsl: lambda e: e.dma_start(out=gb[gs_][:], in_=gT[:, tsl].rearrange("(two r) t -> r two t", two=2)[m * 128:(m + 1) * 128]))(gs_, m, tsl),
                 writes=[('gb', gs_)], dma=f'gb{gs_}')
            bv, bg, ba = nb(), nb(), nb()
            for kc in range(4):
                P.op('pe', (lambda bv, kc, m, xs: lambda e: e.matmul(pb[bv][:], lhsT=glu_bf[:, kc, m * 128:(m + 1) * 128], rhs=ysb[xs][:, kc, :],
                                                                    start=(kc == 0), stop=(kc == 3)))(bv, kc, m, xs),
                     reads=['glu', ('ysb', xs)], writes=[('pb', bv)])
            for kc in range(4):
                P.op('pe', (lambda bg, kc, m, xs: lambda e: e.matmul(pb[bg][:], lhsT=glu_bf[:, kc, 1024 + m * 128:1024 + (m + 1) * 128], rhs=ysb[xs][:, kc, :],
                                                                    start=(kc == 0), stop=(kc == 3)))(bg, kc, m, xs),
                     reads=['glu', ('ysb', xs)], writes=[('pb', bg)])
            for kc in range(4):
                P.op('pe', (lambda ba, kc, m, xs: lambda e: e.matmul(pb[ba][:], lhsT=up_bf[:, kc, m * 128:(m + 1) * 128], rhs=ob[xs][:, kc, :],
                                                                    start=(kc == 0), stop=(kc == 3)))(ba, kc, m, xs),
                     reads=['up', ('ob', xs)], writes=[('pb', ba)])
            s = m % 2
            P.op('act', (lambda s, bg: lambda e: e.activation(out=sg[s][:], in_=pb[bg][:], func=AF.Sigmoid))(s, bg),
                 reads=[('pb', bg)], writes=[('sg', s)])
            P.op('dve', (lambda s, bv: lambda e: e.tensor_tensor(out=t1[s][:], in0=pb[bv][:], in1=sg[s][:], op=ALU.mult))(s, bv),
                 reads=[('pb', bv), ('sg', s)], writes=[('t1', s)])
            P.op('pool', (lambda s, gs_: lambda e: e.tensor_tensor(out=t1[s][:], in0=t1[s][:], in1=gb[gs_][:, 0, :], op=ALU.mult))(s, gs_),
                 reads=[('t1', s), ('gb', gs_)], writes=[('t1', s)])
            P.op('dve', (lambda s, ba, gs_: lambda e: e.tensor_tensor(out=t2[s][:], in0=pb[ba][:], in1=gb[gs_][:, 1, :], op=ALU.mult))(s, ba, gs_),
                 reads=[('pb', ba), ('gb', gs_)], writes=[('t2', s)])
            P.op('pool', (lambda s, m: lambda e: e.tensor_tensor(out=mg[:, m, :], in0=t1[s][:], in1=t2[s][:], op=ALU.add))(s, m),
                 reads=[('t1', s), ('t2', s)], writes=[('mg', m)])
        for m in range(8):
            bo = nb()
            for kc in range(8):
                P.op('pe', (lambda bo, kc, m: lambda e: e.matmul(pb[bo][:], lhsT=wo_bf[:, kc, m * 128:(m + 1) * 128], rhs=mg[:, kc, :],
                                                                start=(kc == 0), stop=(kc == 7)))(bo, kc, m),
                     reads=['wo', ('mg', kc)], writes=[('pb', bo)])
            o = oi % 2
            oi += 1
            P.op('dve', (lambda o, bo, m, xs: lambda e: e.scalar_tensor_tensor(out=xo[o][:], in0=pb[bo][:], scalar=g1[:, m:m + 1], in1=xb[xs][:, m, :],
                                                                              op0=ALU.mult, op1=ALU.add))(o, bo, m, xs),
                 reads=[('pb', bo), 'g1', ('xb', xs)], writes=[('xo', o)])
            P.op('sp', (lambda o, m, tsl: lambda e: e.dma_start(out=x1T[m * 128:(m + 1) * 128, tsl], in_=xo[o][:]))(o, m, tsl),
                 reads=[('xo', o)], dma=f'xo{o}')
    if own:
        P.end_phase(final=True)
        return nc
    return {"x1T": x1T}


def layout_C1(inp, l, r, xT_full, ysT_full, oT_full, gT_core):
    b, j = r // 4, r % 4
    f = lambda a: np.ascontiguousarray(a, dtype=np.float32)
    sl = slice(j * T, (j + 1) * T)
    return {
        "xT": f(xT_full[b][:, sl]),
        "ysT": np.ascontiguousarray(ysT_full[b][:, sl]),
        "oT": np.ascontiguousarray(oT_full[b][:, sl]),
        "gT": gT_core,
        "cb": f(inp['c'][b].reshape(8, 128).T),
        "adaw": f(inp['ada_w'][l][:, 2048:3072]),
        "adab": f(inp['ada_b'][l][2048:3072].reshape(8, 128).T),
        "gluw": f(inp['ssm_glu_w'][l]),
        "upw": f(inp['attn_up_w'][l]),
        "wout": f(inp['w_out'][l]),
    }


T = 4096
NE = 16384
NSL = 16
GS = 4
NEG = -1.0e30
GELU_C = 2.0 * math.sqrt(2.0 / math.pi)


def build_C2(ntile=32, P=None, pre="", ext=None):
    own = P is None
    if own:
        P = Prog(bass.Bass("TRN2", target_bir_lowering=False))
    nc = P.nc
    D = mkD(nc, pre, ext)
    x1T = D("x1T", [1024, T], F32, "ExternalInput")
    cb = D("cb", [128, 8], F32, "ExternalInput")
    adaw = D("adaw", [1024, 3072], F32, "ExternalInput")
    adab = D("adab", [128, 16], F32, "ExternalInput")
    g2b = D("g2b", [1, 1024], F32, "ExternalInput")
    n2w = D("n2w", [128, 8], F32, "ExternalInput")
    wq = D("wq", [1024, 2048], F32, "ExternalInput")
    kTd = D("kT", [128, 2, 128], F32, "ExternalInput")
    utab = D("utab", [NE, 1024], F32, "ExternalInput")
    vtab = D("vtab", [NE, 1024], F32, "ExternalInput")
    x2 = D("x2", [T, 1024], F32, "ExternalOutput")
    x2T = D("x2T", [1024, T], F32, "ExternalOutput")
    uvbf = D("uvbf", [NE, 2048], BF16, "Internal")

    wq_bf = P.sb("wq_bf", [128, 8, 2048], BF16)
    c_sb = P.sb("c_sb", [128, 8], F32)
    ca_sb = P.sb("ca_sb", [128, 8], F32)
    ca_bc = P.sb("ca_bc", [128, 8, 128], F32)
    adab_sb = P.sb("adab_sb", [128, 16], F32)
    n2w_sb = P.sb("n2w_sb", [128, 8], F32)
    kT_sb = P.sb("kT_sb", [128, 2, 128], F32)
    modT = P.sb("modT", [128, 16], F32)
    wmod = P.sb("wmod", [128, 8], F32)
    g2bc = P.sb("g2bc", [128, 1024], F32)
    ones = P.sb("ones", [128, 128], F32)
    epsb = P.sb("epsb", [128, 1], F32)
    ident = P.sb("ident", [128, 128], F32)
    ident_bf = P.sb("ident_bf", [128, 128], BF16)
    iot = P.sb("iot", [128, 128], F32)
    big = P.sb("big", [128, 8, 512], F32)
    xb = [P.sb(f"xb{i}", [128, 8, 128], F32) for i in range(2)]
    sq = [P.sb(f"sq{i}", [128, 128], F32) for i in range(2)]
    rstd = P.sb("rstd", [128, 128], F32)
    tmp = [P.sb(f"tmp{i}", [128, 128], F32) for i in range(2)]
    h2T = P.sb("h2T", [128, 8, 128], F32)
    h2Tbf = P.sb("h2Tbf", [128, 8, 128], BF16)
    h2_tok = [P.sb(f"h2_tok{i}", [128, 1024], F32) for i in range(2)]
    x1_tok = [P.sb(f"x1_tok{i}", [128, 1024], F32) for i in range(2)]
    qc = [P.sb(f"qc{i}", [128, 128], F32) for i in range(3)]
    s_sb = P.sb("s_sb", [128, 16, 128], F32)
    s2 = P.sb("s2", [128, 128], F32)
    v16 = P.sb("v16", [128, 16, 16], F32)
    i16 = P.sb("i16", [128, 16, 16], U32)
    if32 = P.sb("if32", [128, 16, 16], F32)
    i1s = P.sb("i1s", [128, 8, 16], F32)
    cand = [P.sb(f"cand{i}", [128, 16, 16], F32) for i in range(2)]
    cand2 = P.sb("cand2", [128, 256], F32)
    sc = P.sb("sc", [128, 8, 16], F32)
    cpos = P.sb("cpos", [128, 8, 16], U32)
    cia = P.sb("cia", [128, 8, 16], U32)
    cib = P.sb("cib", [128, 8, 16], U32)
    caf = P.sb("caf", [128, 8, 16], F32)
    cbf = P.sb("cbf", [128, 8, 16], F32)
    oh = P.sb("oh", [128, 8, 16, 16], F32)
    e1f = P.sb("e1f", [128, 8, 16], F32)
    e2f = P.sb("e2f", [128, 8, 16], F32)
    c4u = P.sb("c4u", [128, 1], U32)
    c15u = P.sb("c15u", [128, 1], U32)
    iot16 = P.sb("iot16", [128, 16], F32)
    ef = P.sb("ef", [128, 128], F32)
    idx = [P.sb(f"idx{i}", [128, 128], I32) for i in range(2)]
    gex = P.sb("gex", [128, 8, 16], F32)
    gsum = P.sb("gsum", [128, 8], F32)
    gw = [P.sb(f"gw{i}", [128, 128], F32) for i in range(2)]
    act = P.sb("act", [128, 128], F32)
    gt = P.sb("gt", [128, 128], F32)
    gsg = P.sb("gsg", [128, 128], F32)
    wts = P.sb("wts", [128, 128], F32)
    uv = [P.sb(f"uv{i}", [128, 2048], BF16) for i in range(NSL)]
    dg = [P.sb(f"dg{i}", [128, 128], BF16) for i in range(NSL)]
    junk = P.sb("junk", [128, 1024], BF16)
    xo = P.sb("xo", [128, 1024], F32)
    xoT = P.sb("xoT", [128, 8, 128], F32)
    pb = [P.ps(f"pb{i}", [128, 512]) for i in range(4)]
    po = [[P.ps(f"po{i}_{h}", [128, 512]) for h in range(2)] for i in range(2)]
    pm = pb[0]
    bank = [0]

    def nb():
        bank[0] = (bank[0] + 1) % 4
        return bank[0]

    NCH = 8
    RCH = NE // NCH
    for ci in range(NCH):
        rs_ = slice(ci * RCH, (ci + 1) * RCH)
        P.op('pool', (lambda rs_: lambda e: e.dma_start(out=uvbf[rs_, 0:1024], in_=utab[rs_, :]))(rs_), writes=[('uvbf', ci, 0)], dma='conv')
        P.op('pool', (lambda rs_: lambda e: e.dma_start(out=uvbf[rs_, 1024:2048], in_=vtab[rs_, :]))(rs_), writes=[('uvbf', ci, 1)], dma='conv')
    alluv = [('uvbf', ci, k) for ci in range(NCH) for k in range(2)]

    P.op('pool', lambda e: e.memset(ones[:], 1.0), writes=['ones'])
    P.op('pool', lambda e: e.memset(c4u[:], 4), writes=['c4u'])
    P.op('pool', lambda e: e.memset(c15u[:], 15), writes=['c15u'])
    P.op('pool', lambda e: e.iota(iot16[:], pattern=[[1, 16]], base=0, channel_multiplier=0, allow_small_or_imprecise_dtypes=True), writes=['iot16'])
    P.op('pool', lambda e: e.memset(epsb[:], 1e-6), writes=['epsb'])
    P.op('pool', lambda e: e.iota(iot[:], pattern=[[1, 128]], base=0, channel_multiplier=-1, allow_small_or_imprecise_dtypes=True), writes=['iot'])
    P.op('dve', lambda e: e.tensor_single_scalar(out=ident[:], in_=iot[:], scalar=0.0, op=ALU.is_equal), reads=['iot'], writes=['ident'])
    P.op('dve', lambda e: e.tensor_copy(out=ident_bf[:], in_=ident[:]), reads=['ident'], writes=['ident_bf'])
    for nm, dst, src in (('c', c_sb, cb), ('adab', adab_sb, adab), ('n2w', n2w_sb, n2w), ('kT', kT_sb, kTd)):
        P.op('sp', (lambda dst, src: lambda e: e.dma_start(out=dst[:], in_=src))(dst, src), writes=[nm], dma='small')
    P.op('act', lambda e: e.activation(out=ca_sb[:], in_=c_sb[:], func=AF.Silu), reads=['c'], writes=['ca'])
    for kc in range(8):
        P.op('dve', (lambda kc: lambda e: e.tensor_copy(out=ca_bc[:, kc, :], in_=ca_sb[:, kc:kc + 1].to_broadcast([128, 128])))(kc),
             reads=['ca'], writes=['ca_bc'])
    for q4 in range(4):
        P.op('sp', (lambda q4: lambda e: e.dma_start(out=big[:], in_=adaw[:, q4 * 512:(q4 + 1) * 512].rearrange("(kc p) t -> p kc t", p=128)))(q4),
             writes=['big'], dma='big')
        for mm in range(4):
            m = q4 * 4 + mm
            for kc in range(8):
                P.op('pe', (lambda kc, m, mm: lambda e: e.matmul(pm[:, m:m + 1], lhsT=big[:, kc, mm * 128:(mm + 1) * 128],
                                                              rhs=ca_sb[:, kc:kc + 1], start=(kc == 0), stop=(kc == 7)))(kc, m, mm),
                     reads=['big', 'ca'], writes=[('pb', 0)])
    P.op('dve', lambda e: e.tensor_tensor(out=modT[:], in0=pm[:, 0:16], in1=adab_sb[:], op=ALU.add), reads=[('pb', 0), 'adab'], writes=['modT'])
    P.op('dve', lambda e: e.scalar_tensor_tensor(out=wmod[:], in0=modT[:, 8:16], scalar=1.0, in1=n2w_sb[:], op0=ALU.add, op1=ALU.mult),
         reads=['modT', 'n2w'], writes=['wmod'])
    P.op('sp', lambda e: e.dma_start(out=xo[:], in_=g2b.partition_broadcast(128)), writes=['xo'], dma='xo')
    for q2 in range(2):
        bq = 1 + q2
        P.op('sp', (lambda q2: lambda e: e.dma_start(out=big[:], in_=adaw[:, 2048 + q2 * 512:2048 + (q2 + 1) * 512].rearrange("(kc p) t -> p kc t", p=128)))(q2),
             writes=['big'], dma='big')
        for kc in range(8):
            P.op('pe', (lambda kc, bq: lambda e: e.matmul(pb[bq][:], lhsT=ca_bc[:, kc, :], rhs=big[:, kc, :], start=(kc == 0), stop=(kc == 7)))(kc, bq),
                 reads=['big', 'ca_bc'], writes=[('pb', bq)])
        P.op('dve', (lambda q2, bq: lambda e: e.tensor_tensor(out=g2bc[:, q2 * 512:(q2 + 1) * 512], in0=pb[bq][:], in1=xo[:, q2 * 512:(q2 + 1) * 512], op=ALU.add))(q2, bq),
             reads=[('pb', bq), 'xo'], writes=['g2bc'])
    for kc in range(8):
        for hh in range(2):
            P.op('pool', (lambda kc, hh: lambda e: e.dma_start(out=wq_bf[:, kc, hh * 1024:(hh + 1) * 1024], in_=wq[kc * 128:(kc + 1) * 128, hh * 1024:(hh + 1) * 1024]))(kc, hh),
                 writes=['wq'], dma='wqc')

    def stage_S(t):
        th = []
        A = th.append
        xs = t % 2
        tsl = slice(t * 128, (t + 1) * 128)
        h2t, x1t, idxt, gwt = h2_tok[xs], x1_tok[xs], idx[xs], gw[xs]
        A(lambda: P.op('sp', lambda e: e.dma_start(out=xb[xs][:], in_=x1T[:, tsl].rearrange("(kc p) t -> p kc t", p=128)),
                       writes=[('xb', xs)], dma=f'xb{xs}'))
        bn = nb()
        for kc in range(8):
            s = kc % 2
            A((lambda kc, s: lambda: P.op('act', lambda e: e.activation(out=sq[s][:], in_=xb[xs][:, kc, :], func=AF.Square),
                                          reads=[('xb', xs)], writes=[('sq', s)]))(kc, s))
            A((lambda kc, s: lambda: P.op('pe', lambda e: e.matmul(pb[bn][:, 0:128], lhsT=ones[:], rhs=sq[s][:], start=(kc == 0), stop=(kc == 7)),
                                          reads=[('sq', s), 'ones'], writes=[('pb', bn)]))(kc, s))
        A(lambda: P.op('act', lambda e: e.activation(out=rstd[:], in_=pb[bn][:, 0:128], func=AF.Sqrt, bias=epsb[:], scale=1.0 / 1024.0),
                       reads=[('pb', bn), 'epsb'], writes=['rstd']))
        A(lambda: P.op('dve', lambda e: e.reciprocal(out=rstd[:], in_=rstd[:]), reads=['rstd'], writes=['rstd']))
        for kc in range(8):
            s = kc % 2
            A((lambda kc, s: lambda: P.op('dve', lambda e: e.tensor_tensor(out=tmp[s][:], in0=xb[xs][:, kc, :], in1=rstd[:], op=ALU.mult),
                                          reads=[('xb', xs), 'rstd'], writes=[('tmp', s)]))(kc, s))
            A((lambda kc, s: lambda: P.op('act', lambda e: e.activation(out=h2T[:, kc, :], in_=tmp[s][:], func=AF.Identity,
                                                                       bias=modT[:, kc:kc + 1], scale=wmod[:, kc:kc + 1]),
                                          reads=[('tmp', s), 'modT', 'wmod'], writes=[('h2T', kc)]))(kc, s))
            A((lambda kc: lambda: P.op('pool', lambda e: e.tensor_copy(out=h2Tbf[:, kc, :], in_=h2T[:, kc, :]), reads=[('h2T', kc)], writes=[('h2Tbf', kc)]))(kc))
        for src, srcn, dst, dstn in ((h2T, 'h2T', h2t, ('h2_tok', xs)), (xb[xs], ('xb', xs), x1t, ('x1_tok', xs))):
            for half in range(2):
                bt = nb()
                for k4 in range(4):
                    kc = half * 4 + k4
                    rd = [(srcn, kc)] if srcn == 'h2T' else [srcn]
                    A((lambda src, kc, k4, bt, rd: lambda: P.op('pe', lambda e: e.transpose(out=pb[bt][:, k4 * 128:(k4 + 1) * 128], in_=src[:, kc, :], identity=ident[:]),
                                                               reads=rd + ['ident'], writes=[('pb', bt)]))(src, kc, k4, bt, rd))
                A((lambda dst, dstn, half, bt: lambda: P.op('act', lambda e: e.copy(out=dst[:, half * 512:(half + 1) * 512], in_=pb[bt][:]),
                                                           reads=[('pb', bt)], writes=[dstn]))(dst, dstn, half, bt))
        bs = [None]
        for hs in range(16):
            side = hs % 2
            bq = nb()
            for kc in range(8):
                A((lambda bq, kc, hs: lambda: P.op('pe', lambda e: e.matmul(pb[bq][:, 0:128], lhsT=wq_bf[:, kc, hs * 128:(hs + 1) * 128], rhs=h2Tbf[:, kc, :],
                                                                          start=(kc == 0), stop=(kc == 7)),
                                                  reads=['wq', ('h2Tbf', kc)], writes=[('pb', bq)]))(bq, kc, hs))
            q_ = hs % 3
            A((lambda q_, bq: lambda: P.op('act', lambda e: e.copy(out=qc[q_][:], in_=pb[bq][:, 0:128]), reads=[('pb', bq)], writes=[('qc', q_)]))(q_, bq))
            if hs % 4 == 0:
                bs[0] = nb()
            bsv = bs[0]
            A((lambda bsv, q_, hs, side: lambda: P.op('pe', lambda e: e.matmul(pb[bsv][:, (hs % 4) * 128:(hs % 4 + 1) * 128], lhsT=qc[q_][:], rhs=kT_sb[:, side, :],
                                                                             start=True, stop=True),
                                                     reads=[('qc', q_), 'kT'], writes=[('pb', bsv)]))(bsv, q_, hs, side))
            if hs % 4 == 3:
                g4 = hs // 4
                A((lambda bsv, g4: lambda: P.op('act', lambda e: e.copy(out=s_sb[:, g4 * 4:(g4 + 1) * 4, :], in_=pb[bsv][:].rearrange("p (a b) -> p a b", a=4)),
                                               reads=[('pb', bsv)], writes=[('s_sb', g4)]))(bsv, g4))
        for hs in range(16):
            rs_ = [('s_sb', hs // 4)]
            A((lambda hs, rs_: lambda: P.op('dve', lambda e: e.max(out=v16[:, hs, 0:8], in_=s_sb[:, hs, :]), reads=rs_, writes=[('v16', hs)]))(hs, rs_))
            A((lambda hs, rs_: lambda: P.op('dve', lambda e: e.max_index(out=i16[:, hs, 0:8], in_max=v16[:, hs, 0:8], in_values=s_sb[:, hs, :]),
                                           reads=rs_ + [('v16', hs)], writes=[('i16', hs)]))(hs, rs_))
            A((lambda hs, rs_: lambda: P.op('dve', lambda e: e.match_replace(out=s2[:], in_to_replace=v16[:, hs, 0:8], in_values=s_sb[:, hs, :], imm_value=NEG),
                                           reads=rs_ + [('v16', hs)], writes=['s2']))(hs, rs_))
            A((lambda hs: lambda: P.op('dve', lambda e: e.max(out=v16[:, hs, 8:16], in_=s2[:]), reads=['s2'], writes=[('v16', hs)]))(hs))
            A((lambda hs: lambda: P.op('dve', lambda e: e.max_index(out=i16[:, hs, 8:16], in_max=v16[:, hs, 8:16], in_values=s2[:]),
                                      reads=['s2', ('v16', hs)], writes=[('i16', hs)]))(hs))
        allv = [('v16', hs) for hs in range(16)]
        alli = [('i16', hs) for hs in range(16)]
        A(lambda: P.op('dve', lambda e: e.tensor_copy(out=if32[:], in_=i16[:]), reads=alli, writes=['if32']))
        A(lambda: P.op('dve', lambda e: e.tensor_scalar_mul(out=i1s[:], in0=if32[:].rearrange("p (h s) k -> p h s k", s=2)[:, :, 0, :], scalar1=128.0),
                       reads=['if32'], writes=['i1s']))
        for h in range(8):
            cs = h % 2
            cf = cand[cs][:].rearrange("p a b -> p (a b)")
            A((lambda h, cs: lambda: P.op('dve', lambda e: e.tensor_tensor(out=cand[cs][:], in0=v16[:, 2 * h, :].unsqueeze(2).to_broadcast([128, 16, 16]),
                                                                          in1=v16[:, 2 * h + 1, :].unsqueeze(1).to_broadcast([128, 16, 16]), op=ALU.add),
                                         reads=allv, writes=[('cand', cs)]))(h, cs))
            A((lambda h, cs, cf: lambda: P.op('dve', lambda e: e.max(out=sc[:, h, 0:8], in_=cf), reads=[('cand', cs)], writes=[('sc', h)]))(h, cs, cf))
            A((lambda h, cs, cf: lambda: P.op('dve', lambda e: e.max_index(out=cpos[:, h, 0:8], in_max=sc[:, h, 0:8], in_values=cf),
                                             reads=[('cand', cs), ('sc', h)], writes=[('ci', h)]))(h, cs, cf))
            A((lambda h, cs, cf: lambda: P.op('dve', lambda e: e.match_replace(out=cand2[:], in_to_replace=sc[:, h, 0:8], in_values=cf, imm_value=NEG),
                                             reads=[('cand', cs), ('sc', h)], writes=['cand2']))(h, cs, cf))
            A((lambda h: lambda: P.op('dve', lambda e: e.max(out=sc[:, h, 8:16], in_=cand2[:]), reads=['cand2'], writes=[('sc', h)]))(h))
            A((lambda h: lambda: P.op('dve', lambda e: e.max_index(out=cpos[:, h, 8:16], in_max=sc[:, h, 8:16], in_values=cand2[:]),
                                     reads=['cand2', ('sc', h)], writes=[('ci', h)]))(h))
        allsc = [('sc', h) for h in range(8)]
        allci = [('ci', h) for h in range(8)]
        A(lambda: P.op('dve', lambda e: e.tensor_single_scalar(out=cia[:], in_=cpos[:], scalar=c4u[:, 0:1], op=ALU.logical_shift_right), reads=allci + ['c4u'], writes=['cia']))
        A(lambda: P.op('dve', lambda e: e.tensor_single_scalar(out=cib[:], in_=cpos[:], scalar=c15u[:, 0:1], op=ALU.bitwise_and), reads=allci + ['c15u'], writes=['cib']))
        A(lambda: P.op('dve', lambda e: e.tensor_copy(out=caf[:], in_=cia[:]), reads=['cia'], writes=['caf']))
        A(lambda: P.op('dve', lambda e: e.tensor_copy(out=cbf[:], in_=cib[:]), reads=['cib'], writes=['cbf']))
        if3 = if32[:].rearrange("p (h s) k -> p h s k", s=2)
        for nm, src, tab in (('a', caf, i1s[:]), ('b', cbf, if3[:, :, 1, :])):
            A((lambda nm, src: lambda: P.op('dve', lambda e: e.tensor_tensor(out=oh[:], in0=src[:].unsqueeze(3).to_broadcast([128, 8, 16, 16]),
                                                                          in1=iot16[:].unsqueeze(1).unsqueeze(1).to_broadcast([128, 8, 16, 16]), op=ALU.is_equal),
                                           reads=['caf', 'cbf', 'iot16'], writes=['oh']))(nm, src))
            A((lambda nm, tab: lambda: P.op('dve', lambda e: e.tensor_tensor(out=oh[:], in0=oh[:], in1=tab.unsqueeze(2).to_broadcast([128, 8, 16, 16]), op=ALU.mult),
                                           reads=['oh', 'i1s', 'if32'], writes=['oh']))(nm, tab))
            A((lambda nm: lambda: P.op('dve', lambda e: e.tensor_reduce(out=(e1f if nm == 'a' else e2f)[:], in_=oh[:], axis=AX.X, op=ALU.add),
                                      reads=['oh'], writes=['e' + nm]))(nm))
        A(lambda: P.op('dve', lambda e: e.tensor_tensor(out=ef[:].rearrange("p (h k) -> p h k", h=8), in0=e1f[:], in1=e2f[:], op=ALU.add), reads=['ea', 'eb'], writes=['ef']))
        A(lambda: P.op('dve', lambda e: e.tensor_scalar(out=ef[:], in0=ef[:], scalar1=float(NE - 1), scalar2=0.0, op0=ALU.min, op1=ALU.max), reads=['ef'], writes=['ef']))
        A(lambda: P.op('dve', lambda e: e.tensor_copy(out=idxt[:], in_=ef[:]), reads=['ef'], writes=[('idx', xs)]))
        A(lambda: P.op('dve', lambda e: e.tensor_tensor(out=gex[:], in0=sc[:], in1=sc[:, :, 0:1].to_broadcast([128, 8, 16]), op=ALU.subtract),
                       reads=allsc, writes=['gex']))
        A(lambda: P.op('act', lambda e: e.activation(out=gex[:], in_=gex[:], func=AF.Exp), reads=['gex'], writes=['gex']))
        A(lambda: P.op('dve', lambda e: e.tensor_reduce(out=gsum[:], in_=gex[:], axis=AX.X, op=ALU.add), reads=['gex'], writes=['gsum']))
        A(lambda: P.op('dve', lambda e: e.reciprocal(out=gsum[:], in_=gsum[:]), reads=['gsum'], writes=['gsum']))
        A(lambda: P.op('dve', lambda e: e.tensor_tensor(out=gwt[:].rearrange("p (h k) -> p h k", h=8), in0=gex[:], in1=gsum[:].unsqueeze(2).to_broadcast([128, 8, 16]), op=ALU.mult),
                       reads=['gex', 'gsum'], writes=[('gw', xs)]))
        return th

    gctr = [0]

    def stage_UW(t, pending):
        xs = t % 2
        h2t, x1t, idxt, gwt = h2_tok[xs], x1_tok[xs], idx[xs], gw[xs]
        pot = po[xs]
        NG = 128 // GS
        per = (len(pending) + NG - 1) // NG if pending else 0
        prev_post = [None]
        for g in range(NG):
            js = list(range(g * GS, (g + 1) * GS))
            gsl = slice(g * GS, (g + 1) * GS)
            slots = {}
            for j in js:
                sl_ = gctr[0] % NSL
                gctr[0] += 1
                slots[j] = sl_
                P.op('pool', (lambda sl_, j: lambda e: e.indirect_dma_start(out=uv[sl_][:], out_offset=None, in_=uvbf,
                                                                          in_offset=bass.IndirectOffsetOnAxis(ap=idxt[:, j:j + 1], axis=0)))(sl_, j),
                     reads=[('idx', xs)] + alluv, writes=[('uv', sl_)], dma=f'uv{sl_}')
            for j in js:
                sl_ = slots[j]
                P.op('dve', (lambda sl_, j: lambda e: e.scalar_tensor_tensor(out=junk[:], in0=uv[sl_][:, 0:1024], scalar=1.0, in1=h2t[:], op0=ALU.mult, op1=ALU.mult,
                                                                           accum_out=act[:, j:j + 1]))(sl_, j),
                     reads=[('uv', sl_), ('h2_tok', xs)], writes=[('act', g)])
            P.op('dve', (lambda gsl: lambda e: e.tensor_tensor(out=gt[:, gsl], in0=act[:, gsl], in1=act[:, gsl], op=ALU.mult))(gsl), reads=[('act', g)], writes=[('gt', g)])
            P.op('dve', (lambda gsl: lambda e: e.tensor_scalar(out=gt[:, gsl], in0=gt[:, gsl], scalar1=0.044715, scalar2=1.0, op0=ALU.mult, op1=ALU.add))(gsl),
                 reads=[('gt', g)], writes=[('gt', g)])
            P.op('dve', (lambda gsl: lambda e: e.tensor_tensor(out=gt[:, gsl], in0=gt[:, gsl], in1=act[:, gsl], op=ALU.mult))(gsl), reads=[('gt', g), ('act', g)], writes=[('gt', g)])
            P.op('act', (lambda gsl: lambda e: e.activation(out=gsg[:, gsl], in_=gt[:, gsl], func=AF.Sigmoid, scale=GELU_C))(gsl), reads=[('gt', g)], writes=[('gsg', g)])

            def post(g=g, gsl=gsl, js=js, slots=slots):
                P.op('dve', lambda e: e.tensor_tensor(out=gt[:, gsl], in0=gsg[:, gsl], in1=act[:, gsl], op=ALU.mult), reads=[('gsg', g), ('act', g)], writes=[('gt', g)])
                P.op('dve', lambda e: e.tensor_tensor(out=wts[:, gsl], in0=gt[:, gsl], in1=gwt[:, gsl], op=ALU.mult), reads=[('gt', g), ('gw', xs)], writes=[('wts', g)])
                for j in js:
                    sl_ = slots[j]
                    P.op('act', (lambda sl_, j: lambda e: e.activation(out=dg[sl_][:], in_=ident_bf[:], func=AF.Identity, scale=wts[:, j:j + 1]))(sl_, j),
                         reads=[('wts', g), 'ident_bf'], writes=[('dg', sl_)])
                    for half in range(2):
                        P.op('pe', (lambda sl_, j, half: lambda e: e.matmul(pot[half][:], lhsT=dg[sl_][:], rhs=uv[sl_][:, 1024 + half * 512:1024 + (half + 1) * 512],
                                                                          start=(j == 0), stop=(j == 127)))(sl_, j, half),
                             reads=[('dg', sl_), ('uv', sl_)], writes=[('po', xs, half)])
            if prev_post[0] is not None:
                prev_post[0]()
            prev_post[0] = post
            if pending:
                for _ in range(per):
                    if pending:
                        pending.pop(0)()
        prev_post[0]()
        while pending:
            pending.pop(0)()
        for half in range(2):
            hs_ = slice(half * 512, (half + 1) * 512)
            P.op('dve', (lambda half, hs_: lambda e: e.tensor_tensor(out=xo[:, hs_], in0=pot[half][:], in1=g2bc[:, hs_], op=ALU.mult))(half, hs_),
                 reads=[('po', xs, half), 'g2bc'], writes=['xo'])
            P.op('pool', (lambda hs_: lambda e: e.tensor_tensor(out=xo[:, hs_], in0=xo[:, hs_], in1=x1t[:, hs_], op=ALU.add))(hs_),
                 reads=['xo', ('x1_tok', xs)], writes=['xo'])
        P.op('sp', lambda e: e.dma_start(out=x2[t * 128:(t + 1) * 128, :], in_=xo[:]), reads=['xo'], dma='xo')
        for half in range(2):
            bt = nb()
            for k4 in range(4):
                kc = half * 4 + k4
                P.op('pe', (lambda kc, k4, bt: lambda e: e.transpose(out=pb[bt][:, k4 * 128:(k4 + 1) * 128], in_=xo[:, kc * 128:(kc + 1) * 128], identity=ident[:]))(kc, k4, bt),
                     reads=['xo', 'ident'], writes=[('pb', bt)])
            P.op('act', (lambda half, bt: lambda e: e.copy(out=xoT[:, half * 4:(half + 1) * 4, :], in_=pb[bt][:].rearrange("p (a b) -> p a b", a=4)))(half, bt),
                 reads=[('pb', bt)], writes=['xoT'])
        P.op('sp', lambda e: e.dma_start(out=x2T[:, t * 128:(t + 1) * 128].rearrange("(kc p) t -> p kc t", p=128), in_=xoT[:]), reads=['xoT'], dma='xoT')

    for thunk in stage_S(0):
        thunk()
    for t in range(ntile):
        pending = stage_S(t + 1) if t + 1 < ntile else []
        stage_UW(t, pending)
    if own:
        P.end_phase(final=True)
        return nc
    return {"x2T": x2T}


def layout_C2(inp, l, r, x1T_core):
    b, j = r // 4, r % 4
    f = lambda a: np.ascontiguousarray(a, dtype=np.float32)
    return {
        "x1T": None if x1T_core is None else f(x1T_core),
        "cb": f(inp['c'][b].reshape(8, 128).T),
        "adaw": f(inp['ada_w'][l][:, 3072:6144]),
        "adab": f(inp['ada_b'][l][3072:5120].reshape(16, 128).T),
        "g2b": f(inp['ada_b'][l][5120:6144].reshape(1, 1024)),
        "n2w": f(inp['norm2_w'][l].reshape(8, 128).T),
        "wq": f(inp['peer_wq'][l]),
        "kT": f(np.stack([inp['peer_k1'][l].T, inp['peer_k2'][l].T], axis=1)),
        "utab": f(inp['peer_u'][l]),
        "vtab": f(inp['peer_v'][l]),
    }


_CACHE = {}


def _prog(key, fn):
    if key not in _CACHE:
        _CACHE[key] = fn()
    return _CACHE[key]


def _run(nc, in_maps):
    res = run_bass_kernel_spmd(nc, in_maps, core_ids=list(range(8)))
    return res.results


def _pfx(d, pre):
    return {pre + k: v for k, v in d.items()}


def _build_LB(l):
    P = Prog(bass.Bass("TRN2", target_bir_lowering=False))
    build_B1(P=P, pre="b1_")
    P.end_phase()
    build_B2(l, P=P, pre="b2_")
    P.end_phase(final=True)
    return P.nc


def _build_LC(with_a):
    P = Prog(bass.Bass("TRN2", target_bir_lowering=False))
    o1 = build_C1(P=P, pre="c1_")
    P.end_phase()
    o2 = build_C2(P=P, pre="c2_", ext={"x1T": o1["x1T"]})
    if with_a:
        P.end_phase()
        build_A(P=P, pre="a_", ext={"xT": o2["x2T"]})
    P.end_phase(final=True)
    return P.nc


def kernel(**inp):
    inp = {k: np.asarray(v) for k, v in inp.items()}
    x = inp['x']
    xT_full = [np.ascontiguousarray(x[b].T) for b in range(2)]
    ra = _run(_prog('A', build_A), [layout_A(inp, 0, r, xT_full) for r in range(8)])
    apre = ''
    x2 = None
    for l in range(2):
        cat = lambda key, b: np.concatenate([ra[b * 4 + j][apre + key] for j in range(4)], axis=1)
        uT_full = [cat('uT', b) for b in range(2)]
        qT_full = [cat('qT', b) for b in range(2)]
        kT_full = [cat('kT', b) for b in range(2)]
        vT_full = [cat('vT', b) for b in range(2)]
        rb = _run(_prog(('LB', l), lambda: _build_LB(l)),
                  [{**_pfx(layout_B1(inp, l, r, uT_full), 'b1_'), **_pfx(layout_B2(inp, l, r, qT_full, kT_full, vT_full), 'b2_')} for r in range(8)])
        ysT_full = [np.concatenate([rb[b * 4 + j]['b1_yT'] for j in range(4)], axis=0) for b in range(2)]
        oT_full = [np.concatenate([rb[b * 4 + j]['b2_oT'] for j in range(4)], axis=0) for b in range(2)]
        with_a = (l == 0)
        maps = []
        for r in range(8):
            m = _pfx(layout_C1(inp, l, r, xT_full, ysT_full, oT_full, ra[r][apre + 'gT']), 'c1_')
            c2 = layout_C2(inp, l, r, None)
            del c2['x1T']
            m.update(_pfx(c2, 'c2_'))
            if with_a:
                a = layout_A(inp, l + 1, r, None)
                del a['xT']
                m.update(_pfx(a, 'a_'))
            maps.append(m)
        rc = _run(_prog(('LC', with_a), lambda: _build_LC(with_a)), maps)
        x2 = [np.concatenate([rc[b * 4 + j]['c2_x2'] for j in range(4)], axis=0) for b in range(2)]
        xT_full = [np.concatenate([rc[b * 4 + j]['c2_x2T'] for j in range(4)], axis=1) for b in range(2)]
        ra, apre = rc, 'a_'
    return np.stack(x2, axis=0).astype(np.float32)
```

```python
import math
import numpy as np
import ml_dtypes
from contextlib import ExitStack
import concourse.bass as bass
import concourse.mybir as mybir
from concourse.bass_utils import run_bass_kernel_spmd

F32 = mybir.dt.float32
BF16 = mybir.dt.bfloat16
U32 = mybir.dt.uint32
I32 = mybir.dt.int32
AF = mybir.ActivationFunctionType
ALU = mybir.AluOpType
AX = mybir.AxisListType
NPBF = ml_dtypes.bfloat16


class Prog:
    ENG = ('pe', 'act', 'dve', 'pool', 'sp')

    def __init__(self, nc):
        self.nc = nc
        self.es = ExitStack()
        self.ops = {e: [] for e in self.ENG}
        self.cnt = {e: 0 for e in self.ENG}
        self.sem = {}
        self.dcnt = {}
        self.lastw = {}
        self.rd = {}
        self.waited = {e: {} for e in self.ENG}
        self.ph = ExitStack()
        self.phase_id = 0
        self.barrier = {}

    def _sem(self, name):
        if name not in self.sem:
            self.sem[name] = self.es.enter_context(self.nc.semaphore(name))
        return self.sem[name]

    def sb(self, name, shape, dt):
        return self.ph.enter_context(self.nc.sbuf_tensor(f"p{self.phase_id}_{name}", shape, dt))

    def ps(self, name, shape, dt=F32):
        return self.ph.enter_context(self.nc.psum_tensor(f"p{self.phase_id}_{name}", shape, dt))

    def op(self, eng, fn, reads=(), writes=(), dma=None):
        waits = {}

        def need(tok, hazard):
            s, v, e, isdma = tok
            if not isdma and dma is None and e == eng:
                if eng == 'pe' or hazard == 'war':
                    return
            if waits.get(s, 0) < v:
                waits[s] = v

        for r in reads:
            if r in self.lastw:
                need(self.lastw[r], 'raw')
        for w in writes:
            if w in self.lastw:
                need(self.lastw[w], 'waw')
            for s, tok in self.rd.get(w, {}).items():
                need(tok, 'war')
        wl = []
        for s, v in waits.items():
            if self.waited[eng].get(s, 0) < v:
                self.waited[eng][s] = v
                wl.append((s, v))
        if dma is None:
            sname = 'e_' + eng
            self._sem(sname)
            self.cnt[eng] += 1
            tok = (sname, self.cnt[eng], eng, False)
            inc = (sname, 1)
        else:
            sname = 'd_' + dma
            self._sem(sname)
            self.dcnt[sname] = self.dcnt.get(sname, 0) + 16
            tok = (sname, self.dcnt[sname], eng, True)
            inc = (sname, 16)
        for w in writes:
            self.lastw[w] = tok
            self.rd[w] = {}
        for r in reads:
            d = self.rd.setdefault(r, {})
            d[tok[0]] = tok
        self.ops[eng].append((wl, fn, inc))

    def finish(self):
        wl = [(s, v) for s, v in self.dcnt.items()]
        self.ops['sp'].append((wl, None, None))

    def end_phase(self, final=False):
        if final:
            self.finish()
        nc = self.nc
        P = self
        barrier = dict(self.barrier)

        def run(name):
            def f(e):
                for s_, v in barrier.items():
                    e.wait_ge(P.sem[s_], v)
                for wl, fn, inc in P.ops[name]:
                    for s_, v in wl:
                        e.wait_ge(P.sem[s_], v)
                    if fn is not None:
                        ins = fn(e)
                        ins.then_inc(P.sem[inc[0]], inc[1])
            return f

        with nc.Block() as block:
            block.tensor(run('pe'))
            block.scalar(run('act'))
            block.vector(run('dve'))
            block.gpsimd(run('pool'))
            block.sync(run('sp'))
        self.ph.close()
        self.ph = ExitStack()
        self.phase_id += 1
        self.ops = {e: [] for e in self.ENG}
        self.barrier = {('e_' + e): self.cnt[e] for e in self.ENG if self.cnt[e] > 0}
        self.barrier.update(self.dcnt)
        if final:
            self.es.close()

    def emit(self):
        self.end_phase(final=False)
        self.es.close()


def mkD(nc, pre, ext):
    def D(n, s, dt, k):
        if ext and n in ext:
            return ext[n]
        return nc.dram_tensor(pre + n, s, dt, kind=k).ap()
    return D


T = 4096
NB = T // 512


def build_A(P=None, pre="", ext=None):
    own = P is None
    if own:
        P = Prog(bass.Bass("TRN2", target_bir_lowering=False))
    nc = P.nc
    D = mkD(nc, pre, ext)
    xT = D("xT", [1024, T], F32, "ExternalInput")
    cb = D("cb", [128, 8], F32, "ExternalInput")
    adaw = D("adaw", [1024, 2048], F32, "ExternalInput")
    adab = D("adab", [128, 16], F32, "ExternalInput")
    n1w = D("n1w", [128, 8], F32, "ExternalInput")
    win = D("win", [1024, 4096], F32, "ExternalInput")
    qkw = D("qkw", [128, 2], F32, "ExternalInput")
    uT = D("uT", [512, T], F32, "ExternalOutput")
    qT = D("qT", [512, T], BF16, "ExternalOutput")
    kT = D("kT", [512, T], BF16, "ExternalOutput")
    vT = D("vT", [512, T], BF16, "ExternalOutput")
    gT = D("gT", [2048, T], F32, "ExternalOutput")

    wbf = P.sb("wbf", [128, 8, 4096], BF16)
    stg = [P.sb(f"stg{i}", [128, 2048], F32) for i in range(2)]
    c_sb = P.sb("c_sb", [128, 8], F32)
    ca_sb = P.sb("ca_sb", [128, 8], F32)
    adab_sb = P.sb("adab_sb", [128, 16], F32)
    n1w_sb = P.sb("n1w_sb", [128, 8], F32)
    qkw_sb = P.sb("qkw_sb", [128, 2], F32)
    modT = P.sb("modT", [128, 16], F32)
    wmod = P.sb("wmod", [128, 8], F32)
    ones = P.sb("ones", [128, 128], F32)
    blk = P.sb("blk", [128, 128], F32)
    epsb = P.sb("epsb", [128, 1], F32)
    xb = [P.sb(f"xb{i}", [128, 8, 512], F32) for i in range(2)]
    sq = [P.sb(f"sq{i}", [128, 512], F32) for i in range(2)]
    rstd = P.sb("rstd", [128, 512], F32)
    tmp = [P.sb(f"tmp{i}", [128, 512], F32) for i in range(2)]
    hT = P.sb("hT", [128, 8, 512], BF16)
    of32 = [P.sb(f"of32_{i}", [128, 512], F32) for i in range(4)]
    obf = [P.sb(f"obf_{i}", [128, 512], BF16) for i in range(4)]
    sq2 = [P.sb(f"sq2_{i}", [128, 512], F32) for i in range(2)]
    r2 = [P.sb(f"r2_{i}", [128, 512], F32) for i in range(2)]
    pz = [P.ps(f"pz{i}", [128, 512]) for i in range(4)]
    pss = [P.ps(f"pss{i}", [128, 512]) for i in range(2)]
    pn = P.ps("pn", [128, 512])
    pm = P.ps("pm", [128, 16])

    P.op('pool', lambda e: e.memset(ones[:], 1.0), writes=['ones'])
    P.op('pool', lambda e: e.memset(blk[:], 0.0), writes=['blk'])
    P.op('pool', lambda e: e.memset(blk[0:64, 0:64], 1.0), writes=['blk'])
    P.op('pool', lambda e: e.memset(blk[64:128, 64:128], 1.0), writes=['blk'])
    P.op('pool', lambda e: e.memset(epsb[:], 1e-6), writes=['epsb'])
    for nm, dst, src in (('c', c_sb, cb), ('adab', adab_sb, adab), ('n1w', n1w_sb, n1w), ('qkw', qkw_sb, qkw)):
        P.op('sp', (lambda dst, src: lambda e: e.dma_start(out=dst[:], in_=src))(dst, src), writes=[nm], dma='sm_' + nm)
    P.op('act', lambda e: e.activation(out=ca_sb[:], in_=c_sb[:], func=AF.Silu), reads=['c'], writes=['ca'])

    for q4 in range(4):
        xs = q4 % 2
        P.op('sp', (lambda xs, q4: lambda e: e.dma_start(out=xb[xs][:], in_=adaw[:, q4 * 512:(q4 + 1) * 512].rearrange("(kc p) t -> p kc t", p=128)))(xs, q4),
             writes=[('xb', xs)], dma=f'xb{xs}')
        for mm in range(4):
            m = q4 * 4 + mm
            for kc in range(8):
                P.op('pe', (lambda xs, kc, m, mm: lambda e: e.matmul(pm[:, m:m + 1], lhsT=xb[xs][:, kc, mm * 128:(mm + 1) * 128],
                                                                  rhs=ca_sb[:, kc:kc + 1], start=(kc == 0), stop=(kc == 7)))(xs, kc, m, mm),
                     reads=[('xb', xs), 'ca'], writes=['pm'])
    P.op('dve', lambda e: e.tensor_tensor(out=modT[:], in0=pm[:], in1=adab_sb[:], op=ALU.add),
         reads=['pm', 'adab'], writes=['modT'])
    P.op('dve', lambda e: e.scalar_tensor_tensor(out=wmod[:], in0=modT[:, 8:16], scalar=1.0, in1=n1w_sb[:],
                                                op0=ALU.add, op1=ALU.mult),
         reads=['modT', 'n1w'], writes=['wmod'])

    for kc in range(8):
        for hh in range(2):
            s = (kc * 2 + hh) % 2
            P.op('sp', (lambda s, kc, hh: lambda e: e.dma_start(out=stg[s][:], in_=win[kc * 128:(kc + 1) * 128, hh * 2048:(hh + 1) * 2048]))(s, kc, hh),
                 writes=[('stg', s)], dma=f'stg{s}')
            P.op('pool', (lambda s, kc, hh: lambda e: e.tensor_copy(out=wbf[:, kc, hh * 2048:(hh + 1) * 2048], in_=stg[s][:]))(s, kc, hh),
                 reads=[('stg', s)], writes=[('wbf', kc, hh)])

    oi = 0
    for tb in range(NB):
        xs = tb % 2
        tsl = slice(tb * 512, (tb + 1) * 512)
        P.op('sp', (lambda xs, tsl: lambda e: e.dma_start(out=xb[xs][:], in_=xT[:, tsl].rearrange("(kc p) t -> p kc t", p=128)))(xs, tsl),
             writes=[('xb', xs)], dma=f'xb{xs}')
        for kc in range(8):
            s = kc % 2
            P.op('act', (lambda xs, kc, s: lambda e: e.activation(out=sq[s][:], in_=xb[xs][:, kc, :], func=AF.Square))(xs, kc, s),
                 reads=[('xb', xs)], writes=[('sq', s)])
            P.op('pe', (lambda kc, s: lambda e: e.matmul(pn[:], lhsT=ones[:], rhs=sq[s][:], start=(kc == 0), stop=(kc == 7)))(kc, s),
                 reads=[('sq', s), 'ones'], writes=['pn'])
        P.op('act', lambda e: e.activation(out=rstd[:], in_=pn[:], func=AF.Sqrt, bias=epsb[:], scale=1.0 / 1024.0),
             reads=['pn', 'epsb'], writes=['rstd'])
        P.op('dve', lambda e: e.reciprocal(out=rstd[:], in_=rstd[:]), reads=['rstd'], writes=['rstd'])
        for kc in range(8):
            s = kc % 2
            P.op('dve', (lambda xs, kc, s: lambda e: e.tensor_tensor(out=tmp[s][:], in0=xb[xs][:, kc, :], in1=rstd[:], op=ALU.mult))(xs, kc, s),
                 reads=[('xb', xs), 'rstd'], writes=[('tmp', s)])
            P.op('act', (lambda kc, s: lambda e: e.activation(out=hT[:, kc, :], in_=tmp[s][:], func=AF.Identity,
                                                             bias=modT[:, kc:kc + 1], scale=wmod[:, kc:kc + 1]))(kc, s),
                 reads=[('tmp', s), 'modT', 'wmod'], writes=[('hT', kc)])
        for m in range(32):
            zs = m % 4
            for kc in range(8):
                P.op('pe', (lambda zs, kc, m: lambda e: e.matmul(pz[zs][:], lhsT=wbf[:, kc, m * 128:(m + 1) * 128], rhs=hT[:, kc, :],
                                                                start=(kc == 0), stop=(kc == 7)))(zs, kc, m),
                     reads=[('hT', kc), ('wbf', kc, m // 16)], writes=[('pz', zs)])
            o = oi % 4
            oi += 1
            msl = slice((m % 4) * 128, (m % 4 + 1) * 128)
            if m < 4:
                P.op('dve', (lambda zs, o: lambda e: e.tensor_copy(out=of32[o][:], in_=pz[zs][:]))(zs, o),
                     reads=[('pz', zs)], writes=[('of32', o)])
                P.op('sp', (lambda o, msl, tsl: lambda e: e.dma_start(out=uT[msl, tsl], in_=of32[o][:]))(o, msl, tsl),
                     reads=[('of32', o)], dma=f'of32_{o}')
            elif m < 12:
                s = m % 2
                wcol = 0 if m < 8 else 1
                dst = qT if m < 8 else kT
                P.op('act', (lambda zs, s: lambda e: e.activation(out=sq2[s][:], in_=pz[zs][:], func=AF.Square))(zs, s),
                     reads=[('pz', zs)], writes=[('sq2', s)])
                P.op('pe', (lambda s: lambda e: e.matmul(pss[s][:], lhsT=blk[:], rhs=sq2[s][:], start=True, stop=True))(s),
                     reads=[('sq2', s), 'blk'], writes=[('pss', s)])
                P.op('act', (lambda s: lambda e: e.activation(out=r2[s][:], in_=pss[s][:], func=AF.Sqrt, bias=epsb[:], scale=1.0 / 64.0))(s),
                     reads=[('pss', s), 'epsb'], writes=[('r2', s)])
                P.op('dve', (lambda s: lambda e: e.reciprocal(out=r2[s][:], in_=r2[s][:]))(s), reads=[('r2', s)], writes=[('r2', s)])
                P.op('dve', (lambda zs, s, o, wcol: lambda e: e.scalar_tensor_tensor(out=obf[o][:], in0=pz[zs][:], scalar=qkw_sb[:, wcol:wcol + 1],
                                                                                    in1=r2[s][:], op0=ALU.mult, op1=ALU.mult))(zs, s, o, wcol),
                     reads=[('pz', zs), ('r2', s), 'qkw'], writes=[('obf', o)])
                P.op('sp', (lambda o, msl, tsl, dst: lambda e: e.dma_start(out=dst[msl, tsl], in_=obf[o][:]))(o, msl, tsl, dst),
                     reads=[('obf', o)], dma=f'obf_{o}')
            elif m < 16:
                P.op('act', (lambda zs, o: lambda e: e.activation(out=obf[o][:], in_=pz[zs][:], func=AF.Identity))(zs, o),
                     reads=[('pz', zs)], writes=[('obf', o)])
                P.op('sp', (lambda o, msl, tsl: lambda e: e.dma_start(out=vT[msl, tsl], in_=obf[o][:]))(o, msl, tsl),
                     reads=[('obf', o)], dma=f'obf_{o}')
            else:
                gsl = slice((m - 16) * 128, (m - 15) * 128)
                P.op('act', (lambda zs, o: lambda e: e.activation(out=of32[o][:], in_=pz[zs][:], func=AF.Sigmoid))(zs, o),
                     reads=[('pz', zs)], writes=[('of32', o)])
                P.op('sp', (lambda o, gsl, tsl: lambda e: e.dma_start(out=gT[gsl, tsl], in_=of32[o][:]))(o, gsl, tsl),
                     reads=[('of32', o)], dma=f'of32_{o}')
    if own:
        P.end_phase(final=True)
        return nc
    return {}


def layout_A(inp, l, r, xT_full):
    b, j = r // 4, r % 4
    f = lambda a: np.ascontiguousarray(a, dtype=np.float32)
    qn = np.tile(inp['q_norm_w'][l], 2)
    kn = np.tile(inp['k_norm_w'][l], 2)
    return {
        "xT": None if xT_full is None else f(xT_full[b][:, j * T:(j + 1) * T]),
        "cb": f(inp['c'][b].reshape(8, 128).T),
        "adaw": f(inp['ada_w'][l][:, 0:2048]),
        "adab": f(inp['ada_b'][l][0:2048].reshape(16, 128).T),
        "n1w": f(inp['norm1_w'][l].reshape(8, 128).T),
        "win": f(inp['w_in'][l]),
        "qkw": f(np.stack([qn, kn], axis=1)),
    }


S = 16384
L = 1024
NSEG = S // L
PI = math.pi


def build_B1(nseg=NSEG, P=None, pre="", ext=None):
    own = P is None
    if own:
        P = Prog(bass.Bass("TRN2", target_bir_lowering=False))
    nc = P.nc
    D = mkD(nc, pre, ext)
    uT = D("uT", [128, S], F32, "ExternalInput")
    prm = D("prm", [128, 12], F32, "ExternalInput")
    bpad = D("bpad", [128, 8, 128], F32, "ExternalInput")
    cpad = D("cpad", [128, 8, 128], F32, "ExternalInput")
    dsk = D("dsk", [128, 1], F32, "ExternalInput")
    yT = D("yT", [128, S], BF16, "ExternalOutput")

    prm_sb = P.sb("prm_sb", [128, 12], F32)
    bpad_sb = P.sb("bpad_sb", [128, 8, 128], F32)
    cpad_sb = P.sb("cpad_sb", [128, 8, 128], F32)
    dsk_sb = P.sb("dsk_sb", [128, 1], F32)
    sm = P.sb("sm", [128, 64], F32)
    npi = P.sb("npi", [128, 1], F32)
    qi_sb = P.sb("qi_sb", [128, 4], I32)
    ident = P.sb("ident", [128, 128], F32)
    iot = P.sb("iot", [128, 128], F32)
    bbp = P.sb("bbp", [128, 8, 128], F32)
    btmp = P.sb("btmp", [128, 128], F32)
    W = P.sb("W", [128, 8, 128], F32)
    Ec = P.sb("Ec", [128, 4, L], F32)
    Es = P.sb("Es", [128, 4, L], F32)
    etmp = P.sb("etmp", [128, L], F32)
    rot = P.sb("rot", [128, 8], F32)
    carry = P.sb("carry", [128, 8], F32)
    ctmp = P.sb("ctmp", [128, 4], F32)
    ub = [P.sb(f"ub{i}", [128, L], F32) for i in range(2)]
    m = [[P.sb(f"m{k}_{i}", [128, 512], F32) for i in range(2)] for k in range(4)]
    win_r = P.sb("win_r", [128, L], F32)
    win_i = P.sb("win_i", [128, L], F32)
    w_r = P.sb("w_r", [128, L], F32)
    w_i = P.sb("w_i", [128, L], F32)
    dd = [P.sb(f"dd{k}", [128, L], F32) for k in range(4)]
    xr = P.sb("xr", [128, 4, L], F32)
    xi = P.sb("xi", [128, 4, L], F32)
    ysb = [P.sb(f"ysb{i}", [128, 512], F32) for i in range(2)]
    gt = [P.sb(f"gt{i}", [128, 512], F32) for i in range(2)]
    gs = [P.sb(f"gs{i}", [128, 512], F32) for i in range(2)]
    yo = [P.sb(f"yo{i}", [128, 512], BF16) for i in range(2)]
    psr = [P.ps(f"psr{i}", [128, 512]) for i in range(2)]
    psi = [P.ps(f"psi{i}", [128, 512]) for i in range(2)]
    psy = [P.ps(f"psy{i}", [128, 512]) for i in range(2)]
    pst = P.ps("pst", [128, 128])

    col = lambda k: sm[:, k:k + 1]
    for nm, dst, src in (('prm', prm_sb, prm), ('bpad', bpad_sb, bpad), ('cpad', cpad_sb, cpad), ('dsk', dsk_sb, dsk)):
        P.op('sp', (lambda dst, src: lambda e: e.dma_start(out=dst[:], in_=src))(dst, src), writes=[nm], dma='sm_' + nm)
    P.op('pool', lambda e: e.memset(npi[:], -PI), writes=['npi'])
    P.op('pool', lambda e: e.memset(carry[:], 0.0), writes=['carry'])
    P.op('pool', lambda e: e.iota(iot[:], pattern=[[1, 128]], base=0, channel_multiplier=-1, allow_small_or_imprecise_dtypes=True), writes=['iot'])
    P.op('dve', lambda e: e.tensor_single_scalar(out=ident[:], in_=iot[:], scalar=0.0, op=ALU.is_equal), reads=['iot'], writes=['ident'])
    P.op('act', lambda e: e.mul(out=cpad_sb[:, 4:8, :], in_=cpad_sb[:, 4:8, :], mul=-1.0), reads=['cpad'], writes=['cpad'])

    c4 = lambda k: sm[:, k:k + 4]
    ldt, are, aim = prm_sb[:, 0:4], prm_sb[:, 4:8], prm_sb[:, 8:12]
    seq = []
    def dv(fn, reads=('sm',), writes=('sm',)):
        P.op('dve', fn, reads=list(reads) + ['prm'], writes=list(writes))
    def ac(fn):
        P.op('act', fn, reads=['sm', 'prm', 'npi'], writes=['sm'])
    ac(lambda e: e.activation(out=c4(0), in_=ldt, func=AF.Exp))
    dv(lambda e: e.tensor_tensor(out=c4(44), in0=are, in1=c4(0), op=ALU.mult))
    ac(lambda e: e.activation(out=c4(4), in_=c4(44), func=AF.Exp))
    dv(lambda e: e.tensor_tensor(out=c4(8), in0=aim, in1=c4(0), op=ALU.mult))
    def red_sin(dst, shift):
        dv(lambda e: e.tensor_scalar_add(out=c4(52), in0=c4(8), scalar1=shift))
        dv(lambda e: e.tensor_scalar_mul(out=c4(44), in0=c4(52), scalar1=1.0 / (2 * PI)))
        P.op('dve', lambda e: e.tensor_copy(out=qi_sb[:], in_=c4(44)), reads=['sm'], writes=['qi'])
        P.op('dve', lambda e: e.tensor_copy(out=c4(44), in_=qi_sb[:]), reads=['qi'], writes=['sm'])
        dv(lambda e: e.scalar_tensor_tensor(out=c4(48), in0=c4(44), scalar=-2 * PI, in1=c4(52), op0=ALU.mult, op1=ALU.add))
        dv(lambda e: e.tensor_single_scalar(out=c4(44), in_=c4(48), scalar=PI, op=ALU.is_gt))
        dv(lambda e: e.scalar_tensor_tensor(out=c4(48), in0=c4(44), scalar=-2 * PI, in1=c4(48), op0=ALU.mult, op1=ALU.add))
        dv(lambda e: e.tensor_single_scalar(out=c4(44), in_=c4(48), scalar=-PI, op=ALU.is_lt))
        dv(lambda e: e.scalar_tensor_tensor(out=c4(48), in0=c4(44), scalar=2 * PI, in1=c4(48), op0=ALU.mult, op1=ALU.add))
        ac(lambda e: e.activation(out=c4(dst), in_=c4(48), func=AF.Sin))
    red_sin(16, 0.0)
    red_sin(12, 0.5 * PI)
    dv(lambda e: e.tensor_tensor(out=c4(20), in0=c4(4), in1=c4(12), op=ALU.mult))
    dv(lambda e: e.tensor_tensor(out=c4(24), in0=c4(4), in1=c4(16), op=ALU.mult))
    dv(lambda e: e.tensor_tensor(out=c4(44), in0=are, in1=are, op=ALU.mult))
    dv(lambda e: e.tensor_tensor(out=c4(48), in0=aim, in1=aim, op=ALU.mult))
    dv(lambda e: e.tensor_tensor(out=c4(28), in0=c4(44), in1=c4(48), op=ALU.add))
    dv(lambda e: e.reciprocal(out=c4(28), in_=c4(28)))
    dv(lambda e: e.tensor_scalar_add(out=c4(32), in0=c4(20), scalar1=-1.0))
    dv(lambda e: e.tensor_tensor(out=c4(44), in0=c4(32), in1=are, op=ALU.mult))
    dv(lambda e: e.tensor_tensor(out=c4(48), in0=c4(24), in1=aim, op=ALU.mult))
    dv(lambda e: e.tensor_tensor(out=c4(44), in0=c4(44), in1=c4(48), op=ALU.add))
    dv(lambda e: e.tensor_tensor(out=c4(36), in0=c4(44), in1=c4(28), op=ALU.mult))
    dv(lambda e: e.tensor_tensor(out=c4(44), in0=c4(24), in1=are, op=ALU.mult))
    dv(lambda e: e.tensor_tensor(out=c4(48), in0=c4(32), in1=aim, op=ALU.mult))
    dv(lambda e: e.tensor_tensor(out=c4(44), in0=c4(44), in1=c4(48), op=ALU.subtract))
    dv(lambda e: e.tensor_tensor(out=c4(40), in0=c4(44), in1=c4(28), op=ALU.mult))

    for gp in range(4):
        fre, fim = col(36 + gp), col(40 + gp)
        P.op('dve', (lambda gp, fim: lambda e: e.tensor_scalar_mul(out=btmp[:], in0=bpad_sb[:, 4 + gp, :], scalar1=fim))(gp, fim),
             reads=['sm', 'bpad'], writes=['btmp'])
        P.op('dve', (lambda gp, fre: lambda e: e.scalar_tensor_tensor(out=bbp[:, gp, :], in0=bpad_sb[:, gp, :], scalar=fre, in1=btmp[:],
                                                                      op0=ALU.mult, op1=ALU.subtract))(gp, fre),
             reads=['sm', 'bpad', 'btmp'], writes=[('bbp', gp)])
        P.op('dve', (lambda gp, fim: lambda e: e.tensor_scalar_mul(out=btmp[:], in0=bpad_sb[:, gp, :], scalar1=fim))(gp, fim),
             reads=['sm', 'bpad'], writes=['btmp'])
        P.op('dve', (lambda gp, fre: lambda e: e.scalar_tensor_tensor(out=bbp[:, 4 + gp, :], in0=bpad_sb[:, 4 + gp, :], scalar=fre, in1=btmp[:],
                                                                      op0=ALU.mult, op1=ALU.add))(gp, fre),
             reads=['sm', 'bpad', 'btmp'], writes=[('bbp', 4 + gp)])
        for k in (gp, 4 + gp):
            P.op('pe', (lambda k: lambda e: e.transpose(out=pst[:], in_=bbp[:, k, :], identity=ident[:]))(k),
                 reads=[('bbp', k), 'ident'], writes=['pst'])
            P.op('act', (lambda k: lambda e: e.copy(out=W[:, k, :], in_=pst[:]))(k), reads=['pst'], writes=[('W', k)])

    for gp in range(4):
        c1, s1 = col(12 + gp), col(16 + gp)
        ec, es = Ec[:, gp, :], Es[:, gp, :]
        tg = ('E', gp)
        P.op('pool', (lambda ec: lambda e: e.memset(ec[:, 0:1], 1.0))(ec), writes=[tg])
        P.op('pool', (lambda es: lambda e: e.memset(es[:, 0:1], 0.0))(es), writes=[tg])
        P.op('dve', (lambda ec, c1: lambda e: e.tensor_copy(out=ec[:, 1:2], in_=c1))(ec, c1), reads=['sm', tg], writes=[tg])
        P.op('dve', (lambda es, s1: lambda e: e.tensor_copy(out=es[:, 1:2], in_=s1))(es, s1), reads=['sm', tg], writes=[tg])
        n = 2
        while n <= L:
            P.op('dve', (lambda es, n, s1: lambda e: e.tensor_tensor(out=ctmp[:, 2:3], in0=es[:, n - 1:n], in1=s1, op=ALU.mult))(es, n, s1),
                 reads=[tg, 'sm'], writes=['ctmp'])
            P.op('dve', (lambda ec, n, c1: lambda e: e.scalar_tensor_tensor(out=ctmp[:, 0:1], in0=ec[:, n - 1:n], scalar=c1, in1=ctmp[:, 2:3],
                                                                           op0=ALU.mult, op1=ALU.subtract))(ec, n, c1),
                 reads=[tg, 'sm', 'ctmp'], writes=['ctmp'])
            P.op('dve', (lambda es, n, c1: lambda e: e.tensor_tensor(out=ctmp[:, 2:3], in0=es[:, n - 1:n], in1=c1, op=ALU.mult))(es, n, c1),
                 reads=[tg, 'sm', 'ctmp'], writes=['ctmp'])
            P.op('dve', (lambda ec, n, s1: lambda e: e.scalar_tensor_tensor(out=ctmp[:, 1:2], in0=ec[:, n - 1:n], scalar=s1, in1=ctmp[:, 2:3],
                                                                           op0=ALU.mult, op1=ALU.add))(ec, n, s1),
                 reads=[tg, 'sm', 'ctmp'], writes=['ctmp'])
            if n == L:
                P.op('dve', (lambda gp: lambda e: e.tensor_copy(out=rot[:, gp:gp + 1], in_=ctmp[:, 0:1]))(gp), reads=['ctmp'], writes=['rot'])
                P.op('dve', (lambda gp: lambda e: e.tensor_copy(out=rot[:, 4 + gp:5 + gp], in_=ctmp[:, 1:2]))(gp), reads=['ctmp'], writes=['rot'])
                break
            P.op('dve', (lambda es, n: lambda e: e.tensor_scalar_mul(out=etmp[:, 0:n], in0=es[:, 0:n], scalar1=ctmp[:, 1:2]))(es, n),
                 reads=[tg, 'ctmp'], writes=['etmp'])
            P.op('dve', (lambda ec, n: lambda e: e.scalar_tensor_tensor(out=ec[:, n:2 * n], in0=ec[:, 0:n], scalar=ctmp[:, 0:1], in1=etmp[:, 0:n],
                                                                       op0=ALU.mult, op1=ALU.subtract))(ec, n),
                 reads=[tg, 'ctmp', 'etmp'], writes=[tg])
            P.op('dve', (lambda ec, n: lambda e: e.tensor_scalar_mul(out=etmp[:, 0:n], in0=ec[:, 0:n], scalar1=ctmp[:, 1:2]))(ec, n),
                 reads=[tg, 'ctmp'], writes=['etmp'])
            P.op('dve', (lambda es, n: lambda e: e.scalar_tensor_tensor(out=es[:, n:2 * n], in0=es[:, 0:n], scalar=ctmp[:, 0:1], in1=etmp[:, 0:n],
                                                                       op0=ALU.mult, op1=ALU.add))(es, n),
                 reads=[tg, 'ctmp', 'etmp'], writes=[tg])
            n *= 2

    mi = 0
    yi = 0
    for seg in range(nseg):
        us = seg % 2
        ssl = slice(seg * L, (seg + 1) * L)
        P.op('sp', (lambda us, ssl: lambda e: e.dma_start(out=ub[us][:], in_=uT[:, ssl]))(us, ssl), writes=[('ub', us)], dma=f'ub{us}')
        for gp in range(4):
            tg = ('E', gp)
            for tb in range(L // 512):
                bs = mi % 2
                mi += 1
                fs = slice(tb * 512, (tb + 1) * 512)
                P.op('pe', (lambda gp, us, fs, bs: lambda e: e.matmul(psr[bs][:], lhsT=W[:, gp, :], rhs=ub[us][:, fs], start=True, stop=True))(gp, us, fs, bs),
                     reads=[('W', gp), ('ub', us)], writes=[('psr', bs)])
                P.op('pe', (lambda gp, us, fs, bs: lambda e: e.matmul(psi[bs][:], lhsT=W[:, 4 + gp, :], rhs=ub[us][:, fs], start=True, stop=True))(gp, us, fs, bs),
                     reads=[('W', 4 + gp), ('ub', us)], writes=[('psi', bs)])
                ecs, ess = Ec[:, gp, fs], Es[:, gp, fs]
                for k, (src, srcn, tab) in enumerate(((psr, 'psr', ecs), (psi, 'psi', ess), (psi, 'psi', ecs), (psr, 'psr', ess))):
                    P.op('dve', (lambda k, src, tab, bs: lambda e: e.tensor_tensor(out=m[k][bs][:], in0=src[bs][:], in1=tab, op=ALU.mult))(k, src, tab, bs),
                         reads=[(srcn, bs), tg], writes=[('m', k, bs)])
                P.op('pool', (lambda bs, fs: lambda e: e.tensor_tensor(out=win_r[:, fs], in0=m[0][bs][:], in1=m[1][bs][:], op=ALU.add))(bs, fs),
                     reads=[('m', 0, bs), ('m', 1, bs)], writes=[('win_r', tb)])
                P.op('pool', (lambda bs, fs: lambda e: e.tensor_tensor(out=win_i[:, fs], in0=m[2][bs][:], in1=m[3][bs][:], op=ALU.subtract))(bs, fs),
                     reads=[('m', 2, bs), ('m', 3, bs)], writes=[('win_i', tb)])
            rho = sm[:, 4 + gp:5 + gp].to_broadcast([128, L])
            P.op('dve', (lambda gp, rho: lambda e: e.tensor_tensor_scan(out=w_r[:], data0=rho, data1=win_r[:], initial=carry[:, gp:gp + 1],
                                                                       op0=ALU.mult, op1=ALU.add))(gp, rho),
                 reads=[('win_r', 0), ('win_r', 1), 'sm', 'carry'], writes=['w_r'])
            P.op('dve', (lambda gp, rho: lambda e: e.tensor_tensor_scan(out=w_i[:], data0=rho, data1=win_i[:], initial=carry[:, 4 + gp:5 + gp],
                                                                       op0=ALU.mult, op1=ALU.add))(gp, rho),
                 reads=[('win_i', 0), ('win_i', 1), 'sm', 'carry'], writes=['w_i'])
            rc, rs_ = rot[:, gp:gp + 1], rot[:, 4 + gp:5 + gp]
            P.op('dve', (lambda rs_: lambda e: e.tensor_tensor(out=ctmp[:, 0:1], in0=w_i[:, L - 1:L], in1=rs_, op=ALU.mult))(rs_),
                 reads=['w_i', 'rot'], writes=['ctmp'])
            P.op('dve', (lambda rc: lambda e: e.tensor_tensor(out=ctmp[:, 1:2], in0=w_i[:, L - 1:L], in1=rc, op=ALU.mult))(rc),
                 reads=['w_i', 'rot'], writes=['ctmp'])
            P.op('dve', (lambda gp, rc: lambda e: e.scalar_tensor_tensor(out=carry[:, gp:gp + 1], in0=w_r[:, L - 1:L], scalar=rc, in1=ctmp[:, 0:1],
                                                                        op0=ALU.mult, op1=ALU.subtract))(gp, rc),
                 reads=['w_r', 'rot', 'ctmp'], writes=['carry'])
            P.op('dve', (lambda gp, rs_: lambda e: e.scalar_tensor_tensor(out=carry[:, 4 + gp:5 + gp], in0=w_r[:, L - 1:L], scalar=rs_, in1=ctmp[:, 1:2],
                                                                         op0=ALU.mult, op1=ALU.add))(gp, rs_),
                 reads=['w_r', 'rot', 'ctmp'], writes=['carry'])
            ecl, esl = Ec[:, gp, :], Es[:, gp, :]
            P.op('pool', (lambda ecl: lambda e: e.tensor_tensor(out=dd[0][:], in0=w_r[:], in1=ecl, op=ALU.mult))(ecl), reads=['w_r', tg], writes=[('dd', 0)])
            P.op('pool', (lambda esl: lambda e: e.tensor_tensor(out=dd[1][:], in0=w_i[:], in1=esl, op=ALU.mult))(esl), reads=['w_i', tg], writes=[('dd', 1)])
            P.op('dve', (lambda gp: lambda e: e.tensor_tensor(out=xr[:, gp, :], in0=dd[0][:], in1=dd[1][:], op=ALU.subtract))(gp),
                 reads=[('dd', 0), ('dd', 1)], writes=[('xr', gp)])
            P.op('pool', (lambda esl: lambda e: e.tensor_tensor(out=dd[2][:], in0=w_r[:], in1=esl, op=ALU.mult))(esl), reads=['w_r', tg], writes=[('dd', 2)])
            P.op('dve', (lambda ecl: lambda e: e.tensor_tensor(out=dd[3][:], in0=w_i[:], in1=ecl, op=ALU.mult))(ecl), reads=['w_i', tg], writes=[('dd', 3)])
            P.op('dve', (lambda gp: lambda e: e.tensor_tensor(out=xi[:, gp, :], in0=dd[2][:], in1=dd[3][:], op=ALU.add))(gp),
                 reads=[('dd', 2), ('dd', 3)], writes=[('xi', gp)])
        for tb in range(L // 512):
            ys = yi % 2
            yi += 1
            fs = slice(tb * 512, (tb + 1) * 512)
            for gp in range(4):
                P.op('pe', (lambda gp, fs, ys: lambda e: e.matmul(psy[ys][:], lhsT=cpad_sb[:, gp, :], rhs=xr[:, gp, fs], start=(gp == 0), stop=False))(gp, fs, ys),
                     reads=['cpad', ('xr', gp)], writes=[('psy', ys)])
                P.op('pe', (lambda gp, fs, ys: lambda e: e.matmul(psy[ys][:], lhsT=cpad_sb[:, 4 + gp, :], rhs=xi[:, gp, fs], start=False, stop=(gp == 3)))(gp, fs, ys),
                     reads=['cpad', ('xi', gp)], writes=[('psy', ys)])
            P.op('dve', (lambda us, fs, ys: lambda e: e.scalar_tensor_tensor(out=ysb[ys][:], in0=ub[us][:, fs], scalar=dsk_sb[:, 0:1], in1=psy[ys][:],
                                                                            op0=ALU.mult, op1=ALU.add))(us, fs, ys),
                 reads=[('ub', us), 'dsk', ('psy', ys)], writes=[('ysb', ys)])
            P.op('pool', (lambda ys: lambda e: e.tensor_tensor(out=gt[ys][:], in0=ysb[ys][:], in1=ysb[ys][:], op=ALU.mult))(ys),
                 reads=[('ysb', ys)], writes=[('gt', ys)])
            P.op('pool', (lambda ys: lambda e: e.tensor_scalar(out=gt[ys][:], in0=gt[ys][:], scalar1=0.044715, scalar2=1.0, op0=ALU.mult, op1=ALU.add))(ys),
                 reads=[('gt', ys)], writes=[('gt', ys)])
            P.op('pool', (lambda ys: lambda e: e.tensor_tensor(out=gt[ys][:], in0=gt[ys][:], in1=ysb[ys][:], op=ALU.mult))(ys),
                 reads=[('gt', ys), ('ysb', ys)], writes=[('gt', ys)])
            P.op('act', (lambda ys: lambda e: e.activation(out=gs[ys][:], in_=gt[ys][:], func=AF.Sigmoid, scale=2.0 * math.sqrt(2.0 / PI)))(ys),
                 reads=[('gt', ys)], writes=[('gs', ys)])
            P.op('dve', (lambda ys: lambda e: e.tensor_tensor(out=yo[ys][:], in0=ysb[ys][:], in1=gs[ys][:], op=ALU.mult))(ys),
                 reads=[('ysb', ys), ('gs', ys)], writes=[('yo', ys)])
            osl = slice(seg * L + tb * 512, seg * L + (tb + 1) * 512)
            P.op('sp', (lambda ys, osl: lambda e: e.dma_start(out=yT[:, osl], in_=yo[ys][:]))(ys, osl), reads=[('yo', ys)], dma=f'yo{ys}')
    if own:
        P.end_phase(final=True)
        return nc
    return {}


def layout_B1(inp, l, r, uT_full):
    b, gb = r // 4, r % 4
    f = lambda a: np.ascontiguousarray(a, dtype=np.float32)
    g0 = gb * 8
    prm = np.zeros((128, 12), np.float32)
    bpad = np.zeros((128, 8, 128), np.float32)
    cpad = np.zeros((128, 8, 128), np.float32)
    for gp in range(4):
        for gi in range(2):
            gl = 2 * gp + gi
            g = g0 + gl
            ps = slice(gi * 64, (gi + 1) * 64)
            prm[ps, gp] = inp['ssm_log_dt'][l][g]
            prm[ps, 4 + gp] = inp['ssm_a_re'][l][g]
            prm[ps, 8 + gp] = inp['ssm_a_im'][l][g]
            cs = slice(gl * 16, (gl + 1) * 16)
            bpad[ps, gp, cs] = inp['ssm_b_re'][l][g]
            bpad[ps, 4 + gp, cs] = inp['ssm_b_im'][l][g]
            cpad[ps, gp, cs] = inp['ssm_c_re'][l][g].T
            cpad[ps, 4 + gp, cs] = inp['ssm_c_im'][l][g].T
    return {
        "uT": f(uT_full[b][gb * 128:(gb + 1) * 128, :]),
        "prm": prm, "bpad": bpad, "cpad": cpad,
        "dsk": f(inp['ssm_d'][l].reshape(512)[gb * 128:(gb + 1) * 128].reshape(128, 1)),
    }


S = 16384


def build_B2(l, nqb=32, P=None, pre="", ext=None):
    lam_init = 0.8 - 0.6 * math.exp(-0.3 * l)
    own = P is None
    if own:
        P = Prog(bass.Bass("TRN2", target_bir_lowering=False))
    nc = P.nc
    D = mkD(nc, pre, ext)
    qT = D("qT", [128, S], BF16, "ExternalInput")
    kT = D("kT", [128, S], BF16, "ExternalInput")
    V = D("V", [128, 128, 128], BF16, "ExternalInput")
    lqk = D("lqk", [64, 4], F32, "ExternalInput")
    subw = D("subw", [128, 1], F32, "ExternalInput")
    oT = D("oT", [128, S], BF16, "ExternalOutput")

    kT_sb = P.sb("kT_sb", [128, S], BF16)
    V_sb = P.sb("V_sb", [128, 128, 128], BF16)
    qb_sb = [P.sb(f"qb{i}", [128, 512], BF16) for i in range(2)]
    lqk_sb = P.sb("lqk_sb", [64, 4], F32)
    subw_sb = P.sb("subw_sb", [128, 1], F32)
    prods = P.sb("prods", [64, 2], F32)
    E = P.sb("E", [128, 2], F32)
    nlam = P.sb("nlam", [128, 1], F32)
    onesf = P.sb("onesf", [128, 128], F32)
    epsb = P.sb("epsb", [128, 1], F32)
    iot = P.sb("iot", [128, 512], F32)
    masks = [P.sb(f"mask{j}", [128, 512], BF16) for j in range(4)]
    NPT = 6
    NPS = 3
    LA = 2
    pT = [P.sb(f"pT{i}", [128, 512], BF16) for i in range(NPT)]
    acc = [P.sb(f"acc{c}", [128, 512], F32) for c in range(2)]
    rl = [P.sb(f"rl{c}", [128, 512], F32) for c in range(2)]
    t01 = [P.sb(f"t01_{c}", [128, 512], F32) for c in range(2)]
    o_sb = P.sb("o_sb", [128, 512], F32)
    osq = P.sb("osq", [128, 512], F32)
    rs = P.sb("rs", [128, 512], F32)
    on = [P.sb(f"on{i}", [128, 512], BF16) for i in range(2)]
    ps_s = [P.ps(f"ps_s{i}", [128, 512]) for i in range(NPS)]
    ps_o = [[P.ps(f"ps_o{c}_{i}", [128, 512]) for i in range(2)] for c in range(2)]
    ps_l = P.ps("ps_l", [128, 512])

    P.op('pool', lambda e: e.memset(onesf[:], 1.0), writes=['onesf'])
    P.op('pool', lambda e: e.memset(epsb[:], 1e-6), writes=['epsb'])
    P.op('pool', lambda e: e.iota(iot[:], pattern=[[1, 512]], base=0, channel_multiplier=-1,
                                  allow_small_or_imprecise_dtypes=True), writes=['iot'])
    for j in range(4):
        P.op('dve', (lambda j: lambda e: e.tensor_single_scalar(out=masks[j][:], in_=iot[:], scalar=float(128 * j), op=ALU.is_ge))(j),
             reads=['iot'], writes=[('mask', j)])
    P.op('sp', lambda e: e.dma_start(out=lqk_sb[:], in_=lqk), writes=['lqk'], dma='sm_lqk')
    P.op('sp', lambda e: e.dma_start(out=subw_sb[:], in_=subw), writes=['subw'], dma='sm_subw')
    for i in range(4):
        P.op('sp', (lambda i: lambda e: e.dma_start(out=kT_sb[:, i * 4096:(i + 1) * 4096], in_=kT[:, i * 4096:(i + 1) * 4096]))(i),
             writes=[('kT', i)], dma=f'kT{i}')
        P.op('sp', (lambda i: lambda e: e.dma_start(out=V_sb[:, i * 32:(i + 1) * 32, :], in_=V[:, i * 32:(i + 1) * 32, :]))(i),
             writes=[('V', i)], dma=f'V{i}')
    P.op('dve', lambda e: e.tensor_tensor(out=prods[:, 0:1], in0=lqk_sb[:, 0:1], in1=lqk_sb[:, 1:2], op=ALU.mult),
         reads=['lqk'], writes=['prods'])
    P.op('dve', lambda e: e.tensor_tensor(out=prods[:, 1:2], in0=lqk_sb[:, 2:3], in1=lqk_sb[:, 3:4], op=ALU.mult),
         reads=['lqk'], writes=['prods'])
    P.op('pe', lambda e: e.matmul(ps_l[:, 0:2], lhsT=onesf[0:64, :], rhs=prods[:], start=True, stop=True),
         reads=['prods', 'onesf'], writes=['ps_l'])
    P.op('act', lambda e: e.activation(out=E[:], in_=ps_l[:, 0:2], func=AF.Exp), reads=['ps_l'], writes=['E'])
    P.op('dve', lambda e: e.tensor_tensor(out=nlam[:], in0=E[:, 1:2], in1=E[:, 0:1], op=ALU.subtract), reads=['E'], writes=['nlam'])
    P.op('dve', lambda e: e.tensor_scalar_add(out=nlam[:], in0=nlam[:], scalar1=-lam_init), reads=['nlam'], writes=['nlam'])

    steps = []
    for qb in range(nqb):
        nkt = 4 * (qb + 1)
        for kt in range(nkt):
            for c in range(2):
                steps.append((qb, kt, c, nkt))
    n = len(steps)

    def front(si):
        qb, kt, c, nkt = steps[si]
        qs = qb % 2
        if kt == 0 and c == 0:
            qsl = slice(qb * 512, (qb + 1) * 512)
            P.op('sp', (lambda qs, qsl: lambda e: e.dma_start(out=qb_sb[qs][:], in_=qT[:, qsl]))(qs, qsl),
                 writes=[('qb', qs)], dma=f'qb{qs}')
        jd = kt - 4 * qb
        sb_i = si % NPS
        pi = si % NPT
        csl = slice(c * 64, (c + 1) * 64)
        P.op('pe', (lambda sb_i, kt, qs, csl: lambda e: e.matmul(ps_s[sb_i][:], lhsT=kT_sb[csl, kt * 128:(kt + 1) * 128],
                                                                rhs=qb_sb[qs][csl, :], start=True, stop=True))(sb_i, kt, qs, csl),
             reads=[('kT', kt // 32), ('qb', qs)], writes=[('ps_s', sb_i)])
        P.op('act', (lambda sb_i, pi: lambda e: e.activation(out=pT[pi][:], in_=ps_s[sb_i][:], func=AF.Exp, scale=0.125))(sb_i, pi),
             reads=[('ps_s', sb_i)], writes=[('pT', pi)])
        if jd >= 0:
            P.op('pool', (lambda pi, jd: lambda e: e.tensor_tensor(out=pT[pi][:], in0=pT[pi][:], in1=masks[jd][:], op=ALU.mult))(pi, jd),
                 reads=[('pT', pi), ('mask', jd)], writes=[('pT', pi)])

    def back(si):
        qb, kt, c, nkt = steps[si]
        pi = si % NPT
        ob_ = qb % 2
        P.op('pe', (lambda c, ob_, pi, kt, nkt: lambda e: e.matmul(ps_o[c][ob_][:], lhsT=V_sb[:, kt, :], rhs=pT[pi][:],
                                                                  start=(kt == 0), stop=(kt == nkt - 1)))(c, ob_, pi, kt, nkt),
             reads=[('pT', pi), ('V', kt // 32)], writes=[('ps_o', c, ob_)])
        ae = 'dve' if c == 0 else 'pool'
        if kt == 0:
            P.op(ae, (lambda c, pi: lambda e: e.tensor_copy(out=acc[c][:], in_=pT[pi][:]))(c, pi),
                 reads=[('pT', pi)], writes=[('acc', c)])
        else:
            P.op(ae, (lambda c, pi: lambda e: e.tensor_tensor(out=acc[c][:], in0=acc[c][:], in1=pT[pi][:], op=ALU.add))(c, pi),
                 reads=[('pT', pi), ('acc', c)], writes=[('acc', c)])
        if kt == nkt - 1 and c == 1:
            epilogue(qb)

    def epilogue(qb):
        ob_ = qb % 2
        qsl = slice(qb * 512, (qb + 1) * 512)
        for c in range(2):
            P.op('pe', (lambda c: lambda e: e.matmul(ps_l[:], lhsT=onesf[:], rhs=acc[c][:], start=True, stop=True))(c),
                 reads=[('acc', c), 'onesf'], writes=['ps_l'])
            P.op('dve', (lambda c: lambda e: e.reciprocal(out=rl[c][:], in_=ps_l[:]))(c), reads=['ps_l'], writes=[('rl', c)])
            P.op('dve', (lambda c, ob_: lambda e: e.tensor_tensor(out=t01[c][:], in0=ps_o[c][ob_][:], in1=rl[c][:], op=ALU.mult))(c, ob_),
                 reads=[('ps_o', c, ob_), ('rl', c)], writes=[('t01', c)])
        P.op('dve', lambda e: e.scalar_tensor_tensor(out=o_sb[:], in0=t01[1][:], scalar=nlam[:, 0:1], in1=t01[0][:], op0=ALU.mult, op1=ALU.add),
             reads=[('t01', 0), ('t01', 1), 'nlam'], writes=['o_sb'])
        P.op('act', lambda e: e.activation(out=osq[:], in_=o_sb[:], func=AF.Square), reads=['o_sb'], writes=['osq'])
        P.op('pe', lambda e: e.matmul(ps_l[:], lhsT=onesf[:], rhs=osq[:], start=True, stop=True),
             reads=['osq', 'onesf'], writes=['ps_l'])
        P.op('act', lambda e: e.activation(out=rs[:], in_=ps_l[:], func=AF.Sqrt, bias=epsb[:], scale=1.0 / 128.0),
             reads=['ps_l', 'epsb'], writes=['rs'])
        P.op('dve', lambda e: e.reciprocal(out=rs[:], in_=rs[:]), reads=['rs'], writes=['rs'])
        P.op('dve', lambda e: e.tensor_scalar_mul(out=rs[:], in0=rs[:], scalar1=1.0 - lam_init), reads=['rs'], writes=['rs'])
        P.op('dve', (lambda ob_: lambda e: e.scalar_tensor_tensor(out=on[ob_][:], in0=o_sb[:], scalar=subw_sb[:, 0:1], in1=rs[:], op0=ALU.mult, op1=ALU.mult))(ob_),
             reads=['o_sb', 'rs', 'subw'], writes=[('on', ob_)])
        P.op('sp', (lambda ob_, qsl: lambda e: e.dma_start(out=oT[:, qsl], in_=on[ob_][:]))(ob_, qsl),
             reads=[('on', ob_)], dma=f'on{ob_}')

    for si in range(n + LA):
        if si < n:
            front(si)
        if si - LA >= 0:
            back(si - LA)
    if own:
        P.end_phase(final=True)
        return nc
    return {}


def layout_B2(inp, l, r, qT_full, kT_full, vT_full):
    b, h = r // 4, r % 4
    hs = slice(h * 128, (h + 1) * 128)
    Vh = np.ascontiguousarray(vT_full[b][hs, :].T)
    Vl = np.ascontiguousarray(Vh.reshape(128, 128, 128).transpose(1, 0, 2))
    f = lambda a: np.ascontiguousarray(a, dtype=np.float32)
    return {
        "qT": np.ascontiguousarray(qT_full[b][hs, :]),
        "kT": np.ascontiguousarray(kT_full[b][hs, :]),
        "V": Vl,
        "lqk": f(np.stack([inp['lambda_q1'][l], inp['lambda_k1'][l], inp['lambda_q2'][l], inp['lambda_k2'][l]], axis=1)),
        "subw": f(inp['subln_w'][l].reshape(128, 1)),
    }


T = 4096
NB = T // 512


def build_C1(P=None, pre="", ext=None):
    own = P is None
    if own:
        P = Prog(bass.Bass("TRN2", target_bir_lowering=False))
    nc = P.nc
    D = mkD(nc, pre, ext)
    xT = D("xT", [1024, T], F32, "ExternalInput")
    ysT = D("ysT", [512, T], BF16, "ExternalInput")
    oT = D("oT", [512, T], BF16, "ExternalInput")
    gT = D("gT", [2048, T], F32, "ExternalInput")
    cb = D("cb", [128, 8], F32, "ExternalInput")
    adaw = D("adaw", [1024, 1024], F32, "ExternalInput")
    adab = D("adab", [128, 8], F32, "ExternalInput")
    gluw = D("gluw", [512, 2048], F32, "ExternalInput")
    upw = D("upw", [512, 1024], F32, "ExternalInput")
    wout = D("wout", [1024, 1024], F32, "ExternalInput")
    x1T = D("x1T", [1024, T], F32, "ExternalOutput")

    glu_bf = P.sb("glu_bf", [128, 4, 2048], BF16)
    up_bf = P.sb("up_bf", [128, 4, 1024], BF16)
    wo_bf = P.sb("wo_bf", [128, 8, 1024], BF16)
    stg = [P.sb(f"stg{i}", [128, 2048], F32) for i in range(2)]
    c_sb = P.sb("c_sb", [128, 8], F32)
    ca_sb = P.sb("ca_sb", [128, 8], F32)
    adab_sb = P.sb("adab_sb", [128, 8], F32)
    g1 = P.sb("g1", [128, 8], F32)
    xb = [P.sb(f"xb{i}", [128, 8, 512], F32) for i in range(2)]
    ysb = [P.sb(f"ysb{i}", [128, 4, 512], BF16) for i in range(2)]
    ob = [P.sb(f"ob{i}", [128, 4, 512], BF16) for i in range(2)]
    gb = [P.sb(f"gb{i}", [128, 2, 512], F32) for i in range(2)]
    sg = [P.sb(f"sg{i}", [128, 512], F32) for i in range(2)]
    t1 = [P.sb(f"t1_{i}", [128, 512], F32) for i in range(2)]
    t2 = [P.sb(f"t2_{i}", [128, 512], F32) for i in range(2)]
    mg = P.sb("mg", [128, 8, 512], BF16)
    xo = [P.sb(f"xo{i}", [128, 512], F32) for i in range(2)]
    pb = [P.ps(f"pb{i}", [128, 512]) for i in range(7)]
    pm = P.ps("pm", [128, 8])
    bank = [0]

    def nb():
        bank[0] = (bank[0] + 1) % 7
        return bank[0]

    for nm, dst, src in (('c', c_sb, cb), ('adab', adab_sb, adab)):
        P.op('sp', (lambda dst, src: lambda e: e.dma_start(out=dst[:], in_=src))(dst, src), writes=[nm], dma='sm_' + nm)
    P.op('act', lambda e: e.activation(out=ca_sb[:], in_=c_sb[:], func=AF.Silu), reads=['c'], writes=['ca'])
    for q2 in range(2):
        xs = q2 % 2
        P.op('sp', (lambda xs, q2: lambda e: e.dma_start(out=xb[xs][:], in_=adaw[:, q2 * 512:(q2 + 1) * 512].rearrange("(kc p) t -> p kc t", p=128)))(xs, q2),
             writes=[('xb', xs)], dma=f'xb{xs}')
        for mm in range(4):
            m = q2 * 4 + mm
            for kc in range(8):
                P.op('pe', (lambda xs, kc, m, mm: lambda e: e.matmul(pm[:, m:m + 1], lhsT=xb[xs][:, kc, mm * 128:(mm + 1) * 128],
                                                                  rhs=ca_sb[:, kc:kc + 1], start=(kc == 0), stop=(kc == 7)))(xs, kc, m, mm),
                     reads=[('xb', xs), 'ca'], writes=['pm'])
    P.op('dve', lambda e: e.tensor_tensor(out=g1[:], in0=pm[:], in1=adab_sb[:], op=ALU.add), reads=['pm', 'adab'], writes=['g1'])

    si = 0
    def load_w(src, rows_kc, ncols, dst, tag):
        nonlocal si
        for kc in range(rows_kc):
            for c0 in range(0, ncols, 2048):
                cw = min(2048, ncols - c0)
                s = si % 2
                si += 1
                P.op('sp', (lambda s, kc, c0, cw: lambda e: e.dma_start(out=stg[s][:, 0:cw], in_=src[kc * 128:(kc + 1) * 128, c0:c0 + cw]))(s, kc, c0, cw),
                     writes=[('stg', s)], dma=f'stg{s}')
                P.op('pool', (lambda s, kc, c0, cw: lambda e: e.tensor_copy(out=dst[:, kc, c0:c0 + cw], in_=stg[s][:, 0:cw]))(s, kc, c0, cw),
                     reads=[('stg', s)], writes=[tag])
    load_w(gluw, 4, 2048, glu_bf, 'glu')
    load_w(upw, 4, 1024, up_bf, 'up')
    load_w(wout, 8, 1024, wo_bf, 'wo')

    gi = 0
    oi = 0
    for tb in range(NB):
        xs = tb % 2
        tsl = slice(tb * 512, (tb + 1) * 512)
        P.op('sp', (lambda xs, tsl: lambda e: e.dma_start(out=xb[xs][:], in_=xT[:, tsl].rearrange("(kc p) t -> p kc t", p=128)))(xs, tsl),
             writes=[('xb', xs)], dma=f'xb{xs}')
        P.op('sp', (lambda xs, tsl: lambda e: e.dma_start(out=ysb[xs][:], in_=ysT[:, tsl].rearrange("(kc p) t -> p kc t", p=128)))(xs, tsl),
             writes=[('ysb', xs)], dma=f'ysb{xs}')
        P.op('sp', (lambda xs, tsl: lambda e: e.dma_start(out=ob[xs][:], in_=oT[:, tsl].rearrange("(kc p) t -> p kc t", p=128)))(xs, tsl),
             writes=[('ob', xs)], dma=f'ob{xs}')
        for m in range(8):
            gs_ = gi % 2
            gi += 1
            P.op('sp', (lambda gs_, m, tsl: lambda e: e.dma_start(out=gb[gs_][:], in_=gT[:, tsl].rearrange("(two r) t -> r two t", two=2)[m * 128:(m + 1) * 128]))(gs_, m, tsl),
                 writes=[('gb', gs_)], dma=f'gb{gs_}')
            bv, bg, ba = nb(), nb(), nb()
            for kc in range(4):
                P.op('pe', (lambda bv, kc, m, xs: lambda e: e.matmul(pb[bv][:], lhsT=glu_bf[:, kc, m * 128:(m + 1) * 128], rhs=ysb[xs][:, kc, :],
                                                                    start=(kc == 0), stop=(kc == 3)))(bv, kc, m, xs),
                     reads=['glu', ('ysb', xs)], writes=[('pb', bv)])
            for kc in range(4):
                P.op('pe', (lambda bg, kc, m, xs: lambda e: e.matmul(pb[bg][:], lhsT=glu_bf[:, kc, 1024 + m * 128:1024 + (m + 1) * 128], rhs=ysb[xs][:, kc, :],
                                                                    start=(kc == 0), stop=(kc == 3)))(bg, kc, m, xs),
                     reads=['glu', ('ysb', xs)], writes=[('pb', bg)])
            for kc in range(4):
                P.op('pe', (lambda ba, kc, m, xs: lambda e: e.matmul(pb[ba][:], lhsT=up_bf[:, kc, m * 128:(m + 1) * 128], rhs=ob[xs][:, kc, :],
                                                                    start=(kc == 0), stop=(kc == 3)))(ba, kc, m, xs),
                     reads=['up', ('ob', xs)], writes=[('pb', ba)])
            s = m % 2
            P.op('act', (lambda s, bg: lambda e: e.activation(out=sg[s][:], in_=pb[bg][:], func=AF.Sigmoid))(s, bg),
                 reads=[('pb', bg)], writes=[('sg', s)])
            P.op('dve', (lambda s, bv: lambda e: e.tensor_tensor(out=t1[s][:], in0=pb[bv][:], in1=sg[s][:], op=ALU.mult))(s, bv),
                 reads=[('pb', bv), ('sg', s)], writes=[('t1', s)])
            P.op('pool', (lambda s, gs_: lambda e: e.tensor_tensor(out=t1[s][:], in0=t1[s][:], in1=gb[gs_][:, 0, :], op=ALU.mult))(s, gs_),
                 reads=[('t1', s), ('gb', gs_)], writes=[('t1', s)])
            P.op('dve', (lambda s, ba, gs_: lambda e: e.tensor_tensor(out=t2[s][:], in0=pb[ba][:], in1=gb[gs_][:, 1, :], op=ALU.mult))(s, ba, gs_),
                 reads=[('pb', ba), ('gb', gs_)], writes=[('t2', s)])
            P.op('pool', (lambda s, m: lambda e: e.tensor_tensor(out=mg[:, m, :], in0=t1[s][:], in1=t2[s][:], op=ALU.add))(s, m),
                 reads=[('t1', s), ('t2', s)], writes=[('mg', m)])
        for m in range(8):
            bo = nb()
            for kc in range(8):
                P.op('pe', (lambda bo, kc, m: lambda e: e.matmul(pb[bo][:], lhsT=wo_bf[:, kc, m * 128:(m + 1) * 128], rhs=mg[:, kc, :],
                                                                start=(kc == 0), stop=(kc == 7)))(bo, kc, m),
                     reads=['wo', ('mg', kc)], writes=[('pb', bo)])
            o = oi % 2
            oi += 1
            P.op('dve', (lambda o, bo, m, xs: lambda e: e.scalar_tensor_tensor(out=xo[o][:], in0=pb[bo][:], scalar=g1[:, m:m + 1], in1=xb[xs][:, m, :],
                                                                              op0=ALU.mult, op1=ALU.add))(o, bo, m, xs),
                 reads=[('pb', bo), 'g1', ('xb', xs)], writes=[('xo', o)])
            P.op('sp', (lambda o, m, tsl: lambda e: e.dma_start(out=x1T[m * 128:(m + 1) * 128, tsl], in_=xo[o][:]))(o, m, tsl),
                 reads=[('xo', o)], dma=f'xo{o}')
    if own:
        P.end_phase(final=True)
        return nc
    return {"x1T": x1T}


def layout_C1(inp, l, r, xT_full, ysT_full, oT_full, gT_core):
    b, j = r // 4, r % 4
    f = lambda a: np.ascontiguousarray(a, dtype=np.float32)
    sl = slice(j * T, (j + 1) * T)
    return {
        "xT": f(xT_full[b][:, sl]),
        "ysT": np.ascontiguousarray(ysT_full[b][:, sl]),
        "oT": np.ascontiguousarray(oT_full[b][:, sl]),
        "gT": gT_core,
        "cb": f(inp['c'][b].reshape(8, 128).T),
        "adaw": f(inp['ada_w'][l][:, 2048:3072]),
        "adab": f(inp['ada_b'][l][2048:3072].reshape(8, 128).T),
        "gluw": f(inp['ssm_glu_w'][l]),
        "upw": f(inp['attn_up_w'][l]),
        "wout": f(inp['w_out'][l]),
    }


T = 4096
NE = 16384
NSL = 16
GS = 4
NEG = -1.0e30
GELU_C = 2.0 * math.sqrt(2.0 / math.pi)


def emit_conv(P, utab, vtab, uvbf):
    NCH = 8
    RCH = NE // NCH
    for ci in range(NCH):
        rs_ = slice(ci * RCH, (ci + 1) * RCH)
        P.op('pool', (lambda rs_: lambda e: e.dma_start(out=uvbf[rs_, 0:1024], in_=utab[rs_, :]))(rs_), writes=[('uvbf', ci, 0)], dma='conv')
        P.op('pool', (lambda rs_: lambda e: e.dma_start(out=uvbf[rs_, 1024:2048], in_=vtab[rs_, :]))(rs_), writes=[('uvbf', ci, 1)], dma='conv')


def conv_tensors(P, pre):
    D = mkD(P.nc, pre, None)
    return {"utab": D("utab", [NE, 1024], F32, "ExternalInput"), "vtab": D("vtab", [NE, 1024], F32, "ExternalInput"),
            "uvbf": D("uvbf", [NE, 2048], BF16, "Internal")}


def build_C2(ntile=32, P=None, pre="", ext=None, conv_done=False):
    own = P is None
    if own:
        P = Prog(bass.Bass("TRN2", target_bir_lowering=False))
    nc = P.nc
    D = mkD(nc, pre, ext)
    x1T = D("x1T", [1024, T], F32, "ExternalInput")
    cb = D("cb", [128, 8], F32, "ExternalInput")
    adaw = D("adaw", [1024, 3072], F32, "ExternalInput")
    adab = D("adab", [128, 16], F32, "ExternalInput")
    g2b = D("g2b", [1, 1024], F32, "ExternalInput")
    n2w = D("n2w", [128, 8], F32, "ExternalInput")
    wq = D("wq", [1024, 2048], F32, "ExternalInput")
    kTd = D("kT", [128, 2, 128], F32, "ExternalInput")
    utab = D("utab", [NE, 1024], F32, "ExternalInput")
    vtab = D("vtab", [NE, 1024], F32, "ExternalInput")
    x2 = D("x2", [T, 1024], F32, "ExternalOutput")
    x2T = D("x2T", [1024, T], F32, "ExternalOutput")
    uvbf = D("uvbf", [NE, 2048], BF16, "Internal")

    wq_bf = P.sb("wq_bf", [128, 8, 2048], BF16)
    c_sb = P.sb("c_sb", [128, 8], F32)
    ca_sb = P.sb("ca_sb", [128, 8], F32)
    ca_bc = P.sb("ca_bc", [128, 8, 128], F32)
    adab_sb = P.sb("adab_sb", [128, 16], F32)
    n2w_sb = P.sb("n2w_sb", [128, 8], F32)
    kT_sb = P.sb("kT_sb", [128, 2, 128], F32)
    modT = P.sb("modT", [128, 16], F32)
    wmod = P.sb("wmod", [128, 8], F32)
    g2bc = P.sb("g2bc", [128, 1024], F32)
    ones = P.sb("ones", [128, 128], F32)
    epsb = P.sb("epsb", [128, 1], F32)
    ident = P.sb("ident", [128, 128], F32)
    ident_bf = P.sb("ident_bf", [128, 128], BF16)
    iot = P.sb("iot", [128, 128], F32)
    big = P.sb("big", [128, 8, 512], F32)
    xb = [P.sb(f"xb{i}", [128, 8, 128], F32) for i in range(2)]
    sq = [P.sb(f"sq{i}", [128, 128], F32) for i in range(2)]
    rstd = P.sb("rstd", [128, 128], F32)
    tmp = [P.sb(f"tmp{i}", [128, 128], F32) for i in range(2)]
    h2T = P.sb("h2T", [128, 8, 128], F32)
    h2Tbf = P.sb("h2Tbf", [128, 8, 128], BF16)
    h2_tok = [P.sb(f"h2_tok{i}", [128, 1024], F32) for i in range(2)]
    x1_tok = [P.sb(f"x1_tok{i}", [128, 1024], F32) for i in range(2)]
    qc = [P.sb(f"qc{i}", [128, 128], F32) for i in range(3)]
    s_sb = P.sb("s_sb", [128, 16, 128], F32)
    s2 = P.sb("s2", [128, 128], F32)
    v16 = P.sb("v16", [128, 16, 16], F32)
    i16 = P.sb("i16", [128, 16, 16], U32)
    if32 = P.sb("if32", [128, 16, 16], F32)
    i1s = P.sb("i1s", [128, 8, 16], F32)
    cand = [P.sb(f"cand{i}", [128, 16, 16], F32) for i in range(2)]
    cand2 = P.sb("cand2", [128, 256], F32)
    sc = P.sb("sc", [128, 8, 16], F32)
    cpos = P.sb("cpos", [128, 8, 16], U32)
    cia = P.sb("cia", [128, 8, 16], U32)
    cib = P.sb("cib", [128, 8, 16], U32)
    caf = P.sb("caf", [128, 8, 16], F32)
    cbf = P.sb("cbf", [128, 8, 16], F32)
    oh = P.sb("oh", [128, 8, 16, 16], F32)
    e1f = P.sb("e1f", [128, 8, 16], F32)
    e2f = P.sb("e2f", [128, 8, 16], F32)
    c4u = P.sb("c4u", [128, 1], U32)
    c15u = P.sb("c15u", [128, 1], U32)
    iot16 = P.sb("iot16", [128, 16], F32)
    ef = P.sb("ef", [128, 128], F32)
    idx = [P.sb(f"idx{i}", [128, 128], I32) for i in range(2)]
    gex = P.sb("gex", [128, 8, 16], F32)
    gsum = P.sb("gsum", [128, 8], F32)
    gw = [P.sb(f"gw{i}", [128, 128], F32) for i in range(2)]
    act = P.sb("act", [128, 128], F32)
    gt = P.sb("gt", [128, 128], F32)
    gsg = P.sb("gsg", [128, 128], F32)
    wts = P.sb("wts", [128, 128], F32)
    uv = [P.sb(f"uv{i}", [128, 2048], BF16) for i in range(NSL)]
    dg = [P.sb(f"dg{i}", [128, 128], BF16) for i in range(NSL)]
    junk = P.sb("junk", [128, 1024], BF16)
    xo = P.sb("xo", [128, 1024], F32)
    xoT = P.sb("xoT", [128, 8, 128], F32)
    pb = [P.ps(f"pb{i}", [128, 512]) for i in range(4)]
    po = [[P.ps(f"po{i}_{h}", [128, 512]) for h in range(2)] for i in range(2)]
    pm = pb[0]
    bank = [0]

    def nb():
        bank[0] = (bank[0] + 1) % 4
        return bank[0]

    NCH = 8
    RCH = NE // NCH
    if not conv_done:
        emit_conv(P, utab, vtab, uvbf)
    alluv = [('uvbf', ci, k) for ci in range(NCH) for k in range(2)]

    P.op('pool', lambda e: e.memset(ones[:], 1.0), writes=['ones'])
    P.op('pool', lambda e: e.memset(c4u[:], 4), writes=['c4u'])
    P.op('pool', lambda e: e.memset(c15u[:], 15), writes=['c15u'])
    P.op('pool', lambda e: e.iota(iot16[:], pattern=[[1, 16]], base=0, channel_multiplier=0, allow_small_or_imprecise_dtypes=True), writes=['iot16'])
    P.op('pool', lambda e: e.memset(epsb[:], 1e-6), writes=['epsb'])
    P.op('pool', lambda e: e.iota(iot[:], pattern=[[1, 128]], base=0, channel_multiplier=-1, allow_small_or_imprecise_dtypes=True), writes=['iot'])
    P.op('dve', lambda e: e.tensor_single_scalar(out=ident[:], in_=iot[:], scalar=0.0, op=ALU.is_equal), reads=['iot'], writes=['ident'])
    P.op('dve', lambda e: e.tensor_copy(out=ident_bf[:], in_=ident[:]), reads=['ident'], writes=['ident_bf'])
    for nm, dst, src in (('c', c_sb, cb), ('adab', adab_sb, adab), ('n2w', n2w_sb, n2w), ('kT', kT_sb, kTd)):
        P.op('sp', (lambda dst, src: lambda e: e.dma_start(out=dst[:], in_=src))(dst, src), writes=[nm], dma='sm_' + nm)
    P.op('act', lambda e: e.activation(out=ca_sb[:], in_=c_sb[:], func=AF.Silu), reads=['c'], writes=['ca'])
    for kc in range(8):
        P.op('dve', (lambda kc: lambda e: e.tensor_copy(out=ca_bc[:, kc, :], in_=ca_sb[:, kc:kc + 1].to_broadcast([128, 128])))(kc),
             reads=['ca'], writes=['ca_bc'])
    for q4 in range(4):
        P.op('sp', (lambda q4: lambda e: e.dma_start(out=big[:], in_=adaw[:, q4 * 512:(q4 + 1) * 512].rearrange("(kc p) t -> p kc t", p=128)))(q4),
             writes=['big'], dma='big')
        for mm in range(4):
            m = q4 * 4 + mm
            for kc in range(8):
                P.op('pe', (lambda kc, m, mm: lambda e: e.matmul(pm[:, m:m + 1], lhsT=big[:, kc, mm * 128:(mm + 1) * 128],
                                                              rhs=ca_sb[:, kc:kc + 1], start=(kc == 0), stop=(kc == 7)))(kc, m, mm),
                     reads=['big', 'ca'], writes=[('pb', 0)])
    P.op('dve', lambda e: e.tensor_tensor(out=modT[:], in0=pm[:, 0:16], in1=adab_sb[:], op=ALU.add), reads=[('pb', 0), 'adab'], writes=['modT'])
    P.op('dve', lambda e: e.scalar_tensor_tensor(out=wmod[:], in0=modT[:, 8:16], scalar=1.0, in1=n2w_sb[:], op0=ALU.add, op1=ALU.mult),
         reads=['modT', 'n2w'], writes=['wmod'])
    P.op('sp', lambda e: e.dma_start(out=xo[:], in_=g2b.partition_broadcast(128)), writes=['xo'], dma='xo')
    for q2 in range(2):
        bq = 1 + q2
        P.op('sp', (lambda q2: lambda e: e.dma_start(out=big[:], in_=adaw[:, 2048 + q2 * 512:2048 + (q2 + 1) * 512].rearrange("(kc p) t -> p kc t", p=128)))(q2),
             writes=['big'], dma='big')
        for kc in range(8):
            P.op('pe', (lambda kc, bq: lambda e: e.matmul(pb[bq][:], lhsT=ca_bc[:, kc, :], rhs=big[:, kc, :], start=(kc == 0), stop=(kc == 7)))(kc, bq),
                 reads=['big', 'ca_bc'], writes=[('pb', bq)])
        P.op('dve', (lambda q2, bq: lambda e: e.tensor_tensor(out=g2bc[:, q2 * 512:(q2 + 1) * 512], in0=pb[bq][:], in1=xo[:, q2 * 512:(q2 + 1) * 512], op=ALU.add))(q2, bq),
             reads=[('pb', bq), 'xo'], writes=['g2bc'])
    for kc in range(8):
        for hh in range(2):
            P.op('pool', (lambda kc, hh: lambda e: e.dma_start(out=wq_bf[:, kc, hh * 1024:(hh + 1) * 1024], in_=wq[kc * 128:(kc + 1) * 128, hh * 1024:(hh + 1) * 1024]))(kc, hh),
                 writes=['wq'], dma='wqc')

    def stage_S(t):
        th = []
        A = th.append
        xs = t % 2
        tsl = slice(t * 128, (t + 1) * 128)
        h2t, x1t, idxt, gwt = h2_tok[xs], x1_tok[xs], idx[xs], gw[xs]
        A(lambda: P.op('sp', lambda e: e.dma_start(out=xb[xs][:], in_=x1T[:, tsl].rearrange("(kc p) t -> p kc t", p=128)),
                       writes=[('xb', xs)], dma=f'xb{xs}'))
        bn = nb()
        for kc in range(8):
            s = kc % 2
            A((lambda kc, s: lambda: P.op('act', lambda e: e.activation(out=sq[s][:], in_=xb[xs][:, kc, :], func=AF.Square),
                                          reads=[('xb', xs)], writes=[('sq', s)]))(kc, s))
            A((lambda kc, s: lambda: P.op('pe', lambda e: e.matmul(pb[bn][:, 0:128], lhsT=ones[:], rhs=sq[s][:], start=(kc == 0), stop=(kc == 7)),
                                          reads=[('sq', s), 'ones'], writes=[('pb', bn)]))(kc, s))
        A(lambda: P.op('act', lambda e: e.activation(out=rstd[:], in_=pb[bn][:, 0:128], func=AF.Sqrt, bias=epsb[:], scale=1.0 / 1024.0),
                       reads=[('pb', bn), 'epsb'], writes=['rstd']))
        A(lambda: P.op('dve', lambda e: e.reciprocal(out=rstd[:], in_=rstd[:]), reads=['rstd'], writes=['rstd']))
        for kc in range(8):
            s = kc % 2
            A((lambda kc, s: lambda: P.op('dve', lambda e: e.tensor_tensor(out=tmp[s][:], in0=xb[xs][:, kc, :], in1=rstd[:], op=ALU.mult),
                                          reads=[('xb', xs), 'rstd'], writes=[('tmp', s)]))(kc, s))
            A((lambda kc, s: lambda: P.op('act', lambda e: e.activation(out=h2T[:, kc, :], in_=tmp[s][:], func=AF.Identity,
                                                                       bias=modT[:, kc:kc + 1], scale=wmod[:, kc:kc + 1]),
                                          reads=[('tmp', s), 'modT', 'wmod'], writes=[('h2T', kc)]))(kc, s))
            A((lambda kc: lambda: P.op('pool', lambda e: e.tensor_copy(out=h2Tbf[:, kc, :], in_=h2T[:, kc, :]), reads=[('h2T', kc)], writes=[('h2Tbf', kc)]))(kc))
        for src, srcn, dst, dstn in ((h2T, 'h2T', h2t, ('h2_tok', xs)), (xb[xs], ('xb', xs), x1t, ('x1_tok', xs))):
            for half in range(2):
                bt = nb()
                for k4 in range(4):
                    kc = half * 4 + k4
                    rd = [(srcn, kc)] if srcn == 'h2T' else [srcn]
                    A((lambda src, kc, k4, bt, rd: lambda: P.op('pe', lambda e: e.transpose(out=pb[bt][:, k4 * 128:(k4 + 1) * 128], in_=src[:, kc, :], identity=ident[:]),
                                                               reads=rd + ['ident'], writes=[('pb', bt)]))(src, kc, k4, bt, rd))
                A((lambda dst, dstn, half, bt: lambda: P.op('act', lambda e: e.copy(out=dst[:, half * 512:(half + 1) * 512], in_=pb[bt][:]),
                                                           reads=[('pb', bt)], writes=[dstn]))(dst, dstn, half, bt))
        bs = [None]
        for hs in range(16):
            side = hs % 2
            bq = nb()
            for kc in range(8):
                A((lambda bq, kc, hs: lambda: P.op('pe', lambda e: e.matmul(pb[bq][:, 0:128], lhsT=wq_bf[:, kc, hs * 128:(hs + 1) * 128], rhs=h2Tbf[:, kc, :],
                                                                          start=(kc == 0), stop=(kc == 7)),
                                                  reads=['wq', ('h2Tbf', kc)], writes=[('pb', bq)]))(bq, kc, hs))
            q_ = hs % 3
            A((lambda q_, bq: lambda: P.op('act', lambda e: e.copy(out=qc[q_][:], in_=pb[bq][:, 0:128]), reads=[('pb', bq)], writes=[('qc', q_)]))(q_, bq))
            if hs % 4 == 0:
                bs[0] = nb()
            bsv = bs[0]
            A((lambda bsv, q_, hs, side: lambda: P.op('pe', lambda e: e.matmul(pb[bsv][:, (hs % 4) * 128:(hs % 4 + 1) * 128], lhsT=qc[q_][:], rhs=kT_sb[:, side, :],
                                                                             start=True, stop=True),
                                                     reads=[('qc', q_), 'kT'], writes=[('pb', bsv)]))(bsv, q_, hs, side))
            if hs % 4 == 3:
                g4 = hs // 4
                A((lambda bsv, g4: lambda: P.op('act', lambda e: e.copy(out=s_sb[:, g4 * 4:(g4 + 1) * 4, :], in_=pb[bsv][:].rearrange("p (a b) -> p a b", a=4)),
                                               reads=[('pb', bsv)], writes=[('s_sb', g4)]))(bsv, g4))
        for hs in range(16):
            rs_ = [('s_sb', hs // 4)]
            A((lambda hs, rs_: lambda: P.op('dve', lambda e: e.max(out=v16[:, hs, 0:8], in_=s_sb[:, hs, :]), reads=rs_, writes=[('v16', hs)]))(hs, rs_))
            A((lambda hs, rs_: lambda: P.op('dve', lambda e: e.max_index(out=i16[:, hs, 0:8], in_max=v16[:, hs, 0:8], in_values=s_sb[:, hs, :]),
                                           reads=rs_ + [('v16', hs)], writes=[('i16', hs)]))(hs, rs_))
            A((lambda hs, rs_: lambda: P.op('dve', lambda e: e.match_replace(out=s2[:], in_to_replace=v16[:, hs, 0:8], in_values=s_sb[:, hs, :], imm_value=NEG),
                                           reads=rs_ + [('v16', hs)], writes=['s2']))(hs, rs_))
            A((lambda hs: lambda: P.op('dve', lambda e: e.max(out=v16[:, hs, 8:16], in_=s2[:]), reads=['s2'], writes=[('v16', hs)]))(hs))
            A((lambda hs: lambda: P.op('dve', lambda e: e.max_index(out=i16[:, hs, 8:16], in_max=v16[:, hs, 8:16], in_values=s2[:]),
                                      reads=['s2', ('v16', hs)], writes=[('i16', hs)]))(hs))
        allv = [('v16', hs) for hs in range(16)]
        alli = [('i16', hs) for hs in range(16)]
        A(lambda: P.op('dve', lambda e: e.tensor_copy(out=if32[:], in_=i16[:]), reads=alli, writes=['if32']))
        A(lambda: P.op('dve', lambda e: e.tensor_scalar_mul(out=i1s[:], in0=if32[:].rearrange("p (h s) k -> p h s k", s=2)[:, :, 0, :], scalar1=128.0),
                       reads=['if32'], writes=['i1s']))
        for h in range(8):
            cs = h % 2
            cf = cand[cs][:].rearrange("p a b -> p (a b)")
            A((lambda h, cs: lambda: P.op('dve', lambda e: e.tensor_tensor(out=cand[cs][:], in0=v16[:, 2 * h, :].unsqueeze(2).to_broadcast([128, 16, 16]),
                                                                          in1=v16[:, 2 * h + 1, :].unsqueeze(1).to_broadcast([128, 16, 16]), op=ALU.add),
                                         reads=allv, writes=[('cand', cs)]))(h, cs))
            A((lambda h, cs, cf: lambda: P.op('dve', lambda e: e.max(out=sc[:, h, 0:8], in_=cf), reads=[('cand', cs)], writes=[('sc', h)]))(h, cs, cf))
            A((lambda h, cs, cf: lambda: P.op('dve', lambda e: e.max_index(out=cpos[:, h, 0:8], in_max=sc[:, h, 0:8], in_values=cf),
                                             reads=[('cand', cs), ('sc', h)], writes=[('ci', h)]))(h, cs, cf))
            A((lambda h, cs, cf: lambda: P.op('dve', lambda e: e.match_replace(out=cand2[:], in_to_replace=sc[:, h, 0:8], in_values=cf, imm_value=NEG),
                                             reads=[('cand', cs), ('sc', h)], writes=['cand2']))(h, cs, cf))
            A((lambda h: lambda: P.op('dve', lambda e: e.max(out=sc[:, h, 8:16], in_=cand2[:]), reads=['cand2'], writes=[('sc', h)]))(h))
            A((lambda h: lambda: P.op('dve', lambda e: e.max_index(out=cpos[:, h, 8:16], in_max=sc[:, h, 8:16], in_values=cand2[:]),
                                     reads=['cand2', ('sc', h)], writes=[('ci', h)]))(h))
        allsc = [('sc', h) for h in range(8)]
        allci = [('ci', h) for h in range(8)]
        A(lambda: P.op('dve', lambda e: e.tensor_single_scalar(out=cia[:], in_=cpos[:], scalar=c4u[:, 0:1], op=ALU.logical_shift_right), reads=allci + ['c4u'], writes=['cia']))
        A(lambda: P.op('dve', lambda e: e.tensor_single_scalar(out=cib[:], in_=cpos[:], scalar=c15u[:, 0:1], op=ALU.bitwise_and), reads=allci + ['c15u'], writes=['cib']))
        A(lambda: P.op('dve', lambda e: e.tensor_copy(out=caf[:], in_=cia[:]), reads=['cia'], writes=['caf']))
        A(lambda: P.op('dve', lambda e: e.tensor_copy(out=cbf[:], in_=cib[:]), reads=['cib'], writes=['cbf']))
        if3 = if32[:].rearrange("p (h s) k -> p h s k", s=2)
        for nm, src, tab in (('a', caf, i1s[:]), ('b', cbf, if3[:, :, 1, :])):
            A((lambda nm, src: lambda: P.op('dve', lambda e: e.tensor_tensor(out=oh[:], in0=src[:].unsqueeze(3).to_broadcast([128, 8, 16, 16]),
                                                                          in1=iot16[:].unsqueeze(1).unsqueeze(1).to_broadcast([128, 8, 16, 16]), op=ALU.is_equal),
                                           reads=['caf', 'cbf', 'iot16'], writes=['oh']))(nm, src))
            A((lambda nm, tab: lambda: P.op('dve', lambda e: e.tensor_tensor(out=oh[:], in0=oh[:], in1=tab.unsqueeze(2).to_broadcast([128, 8, 16, 16]), op=ALU.mult),
                                           reads=['oh', 'i1s', 'if32'], writes=['oh']))(nm, tab))
            A((lambda nm: lambda: P.op('dve', lambda e: e.tensor_reduce(out=(e1f if nm == 'a' else e2f)[:], in_=oh[:], axis=AX.X, op=ALU.add),
                                      reads=['oh'], writes=['e' + nm]))(nm))
        A(lambda: P.op('dve', lambda e: e.tensor_tensor(out=ef[:].rearrange("p (h k) -> p h k", h=8), in0=e1f[:], in1=e2f[:], op=ALU.add), reads=['ea', 'eb'], writes=['ef']))
        A(lambda: P.op('dve', lambda e: e.tensor_scalar(out=ef[:], in0=ef[:], scalar1=float(NE - 1), scalar2=0.0, op0=ALU.min, op1=ALU.max), reads=['ef'], writes=['ef']))
        A(lambda: P.op('dve', lambda e: e.tensor_copy(out=idxt[:], in_=ef[:]), reads=['ef'], writes=[('idx', xs)]))
        A(lambda: P.op('dve', lambda e: e.tensor_tensor(out=gex[:], in0=sc[:], in1=sc[:, :, 0:1].to_broadcast([128, 8, 16]), op=ALU.subtract),
                       reads=allsc, writes=['gex']))
        A(lambda: P.op('act', lambda e: e.activation(out=gex[:], in_=gex[:], func=AF.Exp), reads=['gex'], writes=['gex']))
        A(lambda: P.op('dve', lambda e: e.tensor_reduce(out=gsum[:], in_=gex[:], axis=AX.X, op=ALU.add), reads=['gex'], writes=['gsum']))
        A(lambda: P.op('dve', lambda e: e.reciprocal(out=gsum[:], in_=gsum[:]), reads=['gsum'], writes=['gsum']))
        A(lambda: P.op('dve', lambda e: e.tensor_tensor(out=gwt[:].rearrange("p (h k) -> p h k", h=8), in0=gex[:], in1=gsum[:].unsqueeze(2).to_broadcast([128, 8, 16]), op=ALU.mult),
                       reads=['gex', 'gsum'], writes=[('gw', xs)]))
        return th

    gctr = [0]

    def stage_UW(t, pending):
        xs = t % 2
        h2t, x1t, idxt, gwt = h2_tok[xs], x1_tok[xs], idx[xs], gw[xs]
        pot = po[xs]
        NG = 128 // GS
        per = (len(pending) + NG - 1) // NG if pending else 0
        prev_post = [None]
        for g in range(NG):
            js = list(range(g * GS, (g + 1) * GS))
            gsl = slice(g * GS, (g + 1) * GS)
            slots = {}
            for j in js:
                sl_ = gctr[0] % NSL
                gctr[0] += 1
                slots[j] = sl_
                P.op('pool', (lambda sl_, j: lambda e: e.indirect_dma_start(out=uv[sl_][:], out_offset=None, in_=uvbf,
                                                                          in_offset=bass.IndirectOffsetOnAxis(ap=idxt[:, j:j + 1], axis=0)))(sl_, j),
                     reads=[('idx', xs)] + alluv, writes=[('uv', sl_)], dma=f'uv{sl_}')
            for j in js:
                sl_ = slots[j]
                P.op('dve', (lambda sl_, j: lambda e: e.scalar_tensor_tensor(out=junk[:], in0=uv[sl_][:, 0:1024], scalar=1.0, in1=h2t[:], op0=ALU.mult, op1=ALU.mult,
                                                                           accum_out=act[:, j:j + 1]))(sl_, j),
                     reads=[('uv', sl_), ('h2_tok', xs)], writes=[('act', g)])
            P.op('dve', (lambda gsl: lambda e: e.tensor_tensor(out=gt[:, gsl], in0=act[:, gsl], in1=act[:, gsl], op=ALU.mult))(gsl), reads=[('act', g)], writes=[('gt', g)])
            P.op('dve', (lambda gsl: lambda e: e.tensor_scalar(out=gt[:, gsl], in0=gt[:, gsl], scalar1=0.044715, scalar2=1.0, op0=ALU.mult, op1=ALU.add))(gsl),
                 reads=[('gt', g)], writes=[('gt', g)])
            P.op('dve', (lambda gsl: lambda e: e.tensor_tensor(out=gt[:, gsl], in0=gt[:, gsl], in1=act[:, gsl], op=ALU.mult))(gsl), reads=[('gt', g), ('act', g)], writes=[('gt', g)])
            P.op('act', (lambda gsl: lambda e: e.activation(out=gsg[:, gsl], in_=gt[:, gsl], func=AF.Sigmoid, scale=GELU_C))(gsl), reads=[('gt', g)], writes=[('gsg', g)])

            def post(g=g, gsl=gsl, js=js, slots=slots):
                P.op('dve', lambda e: e.tensor_tensor(out=gt[:, gsl], in0=gsg[:, gsl], in1=act[:, gsl], op=ALU.mult), reads=[('gsg', g), ('act', g)], writes=[('gt', g)])
                P.op('dve', lambda e: e.tensor_tensor(out=wts[:, gsl], in0=gt[:, gsl], in1=gwt[:, gsl], op=ALU.mult), reads=[('gt', g), ('gw', xs)], writes=[('wts', g)])
                for j in js:
                    sl_ = slots[j]
                    P.op('act', (lambda sl_, j: lambda e: e.activation(out=dg[sl_][:], in_=ident_bf[:], func=AF.Identity, scale=wts[:, j:j + 1]))(sl_, j),
                         reads=[('wts', g), 'ident_bf'], writes=[('dg', sl_)])
                    for half in range(2):
                        P.op('pe', (lambda sl_, j, half: lambda e: e.matmul(pot[half][:], lhsT=dg[sl_][:], rhs=uv[sl_][:, 1024 + half * 512:1024 + (half + 1) * 512],
                                                                          start=(j == 0), stop=(j == 127)))(sl_, j, half),
                             reads=[('dg', sl_), ('uv', sl_)], writes=[('po', xs, half)])
            if prev_post[0] is not None:
                prev_post[0]()
            prev_post[0] = post
            if pending:
                for _ in range(per):
                    if pending:
                        pending.pop(0)()
        prev_post[0]()
        while pending:
            pending.pop(0)()
        for half in range(2):
            hs_ = slice(half * 512, (half + 1) * 512)
            P.op('dve', (lambda half, hs_: lambda e: e.tensor_tensor(out=xo[:, hs_], in0=pot[half][:], in1=g2bc[:, hs_], op=ALU.mult))(half, hs_),
                 reads=[('po', xs, half), 'g2bc'], writes=['xo'])
            P.op('pool', (lambda hs_: lambda e: e.tensor_tensor(out=xo[:, hs_], in0=xo[:, hs_], in1=x1t[:, hs_], op=ALU.add))(hs_),
                 reads=['xo', ('x1_tok', xs)], writes=['xo'])
        P.op('sp', lambda e: e.dma_start(out=x2[t * 128:(t + 1) * 128, :], in_=xo[:]), reads=['xo'], dma='xo')
        for half in range(2):
            bt = nb()
            for k4 in range(4):
                kc = half * 4 + k4
                P.op('pe', (lambda kc, k4, bt: lambda e: e.transpose(out=pb[bt][:, k4 * 128:(k4 + 1) * 128], in_=xo[:, kc * 128:(kc + 1) * 128], identity=ident[:]))(kc, k4, bt),
                     reads=['xo', 'ident'], writes=[('pb', bt)])
            P.op('act', (lambda half, bt: lambda e: e.copy(out=xoT[:, half * 4:(half + 1) * 4, :], in_=pb[bt][:].rearrange("p (a b) -> p a b", a=4)))(half, bt),
                 reads=[('pb', bt)], writes=['xoT'])
        P.op('sp', lambda e: e.dma_start(out=x2T[:, t * 128:(t + 1) * 128].rearrange("(kc p) t -> p kc t", p=128), in_=xoT[:]), reads=['xoT'], dma='xoT')

    for thunk in stage_S(0):
        thunk()
    for t in range(ntile):
        pending = stage_S(t + 1) if t + 1 < ntile else []
        stage_UW(t, pending)
    if own:
        P.end_phase(final=True)
        return nc
    return {"x2T": x2T}


def layout_C2(inp, l, r, x1T_core):
    b, j = r // 4, r % 4
    f = lambda a: np.ascontiguousarray(a, dtype=np.float32)
    return {
        "x1T": None if x1T_core is None else f(x1T_core),
        "cb": f(inp['c'][b].reshape(8, 128).T),
        "adaw": f(inp['ada_w'][l][:, 3072:6144]),
        "adab": f(inp['ada_b'][l][3072:5120].reshape(16, 128).T),
        "g2b": f(inp['ada_b'][l][5120:6144].reshape(1, 1024)),
        "n2w": f(inp['norm2_w'][l].reshape(8, 128).T),
        "wq": f(inp['peer_wq'][l]),
        "kT": f(np.stack([inp['peer_k1'][l].T, inp['peer_k2'][l].T], axis=1)),
        "utab": f(inp['peer_u'][l]),
        "vtab": f(inp['peer_v'][l]),
    }


_CACHE = {}


def _prog(key, fn):
    if key not in _CACHE:
        _CACHE[key] = fn()
    return _CACHE[key]


def _run(nc, in_maps):
    res = run_bass_kernel_spmd(nc, in_maps, core_ids=list(range(8)))
    return res.results


def _pfx(d, pre):
    return {pre + k: v for k, v in d.items()}


def _build_LB(l):
    P = Prog(bass.Bass("TRN2", target_bir_lowering=False))
    build_B1(P=P, pre="b1_")
    P.end_phase()
    build_B2(l, P=P, pre="b2_")
    P.end_phase(final=True)
    return P.nc


def _build_LC(with_a):
    P = Prog(bass.Bass("TRN2", target_bir_lowering=False))
    ct = conv_tensors(P, "c2_")
    emit_conv(P, ct["utab"], ct["vtab"], ct["uvbf"])
    o1 = build_C1(P=P, pre="c1_")
    P.end_phase()
    o2 = build_C2(P=P, pre="c2_", ext={"x1T": o1["x1T"], **ct}, conv_done=True)
    if with_a:
        P.end_phase()
        build_A(P=P, pre="a_", ext={"xT": o2["x2T"]})
    P.end_phase(final=True)
    return P.nc


def kernel(**inp):
    inp = {k: np.asarray(v) for k, v in inp.items()}
    x = inp['x']
    xT_full = [np.ascontiguousarray(x[b].T) for b in range(2)]
    ra = _run(_prog('A', build_A), [layout_A(inp, 0, r, xT_full) for r in range(8)])
    apre = ''
    x2 = None
    for l in range(2):
        cat = lambda key, b: np.concatenate([ra[b * 4 + j][apre + key] for j in range(4)], axis=1)
        uT_full = [cat('uT', b) for b in range(2)]
        qT_full = [cat('qT', b) for b in range(2)]
        kT_full = [cat('kT', b) for b in range(2)]
        vT_full = [cat('vT', b) for b in range(2)]
        rb = _run(_prog(('LB', l), lambda: _build_LB(l)),
                  [{**_pfx(layout_B1(inp, l, r, uT_full), 'b1_'), **_pfx(layout_B2(inp, l, r, qT_full, kT_full, vT_full), 'b2_')} for r in range(8)])
        ysT_full = [np.concatenate([rb[b * 4 + j]['b1_yT'] for j in range(4)], axis=0) for b in range(2)]
        oT_full = [np.concatenate([rb[b * 4 + j]['b2_oT'] for j in range(4)], axis=0) for b in range(2)]
        with_a = (l == 0)
        maps = []
        for r in range(8):
            m = _pfx(layout_C1(inp, l, r, xT_full, ysT_full, oT_full, ra[r][apre + 'gT']), 'c1_')
            c2 = layout_C2(inp, l, r, None)
            del c2['x1T']
            m.update(_pfx(c2, 'c2_'))
            if with_a:
                a = layout_A(inp, l + 1, r, None)
                del a['xT']
                m.update(_pfx(a, 'a_'))
            maps.append(m)
        rc = _run(_prog(('LC', with_a), lambda: _build_LC(with_a)), maps)
        x2 = [np.concatenate([rc[b * 4 + j]['c2_x2'] for j in range(4)], axis=0) for b in range(2)]
        xT_full = [np.concatenate([rc[b * 4 + j]['c2_x2T'] for j in range(4)], axis=1) for b in range(2)]
        ra, apre = rc, 'a_'
    return np.stack(x2, axis=0).astype(np.float32)
```

```python
import math
import numpy as np
import ml_dtypes
from contextlib import ExitStack
import concourse.bass as bass
import concourse.mybir as mybir
from concourse.bass_utils import run_bass_kernel_spmd

F32 = mybir.dt.float32
BF16 = mybir.dt.bfloat16
U32 = mybir.dt.uint32
I32 = mybir.dt.int32
AF = mybir.ActivationFunctionType
ALU = mybir.AluOpType
AX = mybir.AxisListType
NPBF = ml_dtypes.bfloat16
SYNC_WAW = False


class Prog:
    ENG = ('pe', 'act', 'dve', 'pool', 'sp')

    def __init__(self, nc):
        self.nc = nc
        self.es = ExitStack()
        self.ops = {e: [] for e in self.ENG}
        self.cnt = {e: 0 for e in self.ENG}
        self.sem = {}
        self.dcnt = {}
        self.lastw = {}
        self.rd = {}
        self.waited = {e: {} for e in self.ENG}
        self.ph = ExitStack()
        self.phase_id = 0
        self.barrier = {}

    def _sem(self, name):
        if name not in self.sem:
            self.sem[name] = self.es.enter_context(self.nc.semaphore(name))
        return self.sem[name]

    def sb(self, name, shape, dt):
        return self.ph.enter_context(self.nc.sbuf_tensor(f"p{self.phase_id}_{name}", shape, dt))

    def ps(self, name, shape, dt=F32):
        return self.ph.enter_context(self.nc.psum_tensor(f"p{self.phase_id}_{name}", shape, dt))

    def op(self, eng, fn, reads=(), writes=(), dma=None):
        waits = {}

        def need(tok, hazard):
            s, v, e, isdma = tok
            if not isdma and dma is None and e == eng:
                if eng == 'pe' or hazard == 'war' or (hazard == 'waw' and not SYNC_WAW):
                    return
            if waits.get(s, 0) < v:
                waits[s] = v

        for r in reads:
            if r in self.lastw:
                need(self.lastw[r], 'raw')
        for w in writes:
            if w in self.lastw:
                need(self.lastw[w], 'waw')
            for s, tok in self.rd.get(w, {}).items():
                need(tok, 'war')
        wl = []
        for s, v in waits.items():
            if self.waited[eng].get(s, 0) < v:
                self.waited[eng][s] = v
                wl.append((s, v))
        if dma is None:
            sname = 'e_' + eng
            self._sem(sname)
            self.cnt[eng] += 1
            tok = (sname, self.cnt[eng], eng, False)
            inc = (sname, 1)
        else:
            sname = 'd_' + dma
            self._sem(sname)
            self.dcnt[sname] = self.dcnt.get(sname, 0) + 16
            tok = (sname, self.dcnt[sname], eng, True)
            inc = (sname, 16)
        for w in writes:
            self.lastw[w] = tok
            self.rd[w] = {}
        for r in reads:
            d = self.rd.setdefault(r, {})
            d[tok[0]] = tok
        self.ops[eng].append((wl, fn, inc))

    def finish(self):
        wl = [(s, v) for s, v in self.dcnt.items()]
        self.ops['sp'].append((wl, None, None))

    def end_phase(self, final=False):
        if final:
            self.finish()
        nc = self.nc
        P = self
        barrier = dict(self.barrier)

        def run(name):
            def f(e):
                for s_, v in barrier.items():
                    e.wait_ge(P.sem[s_], v)
                for wl, fn, inc in P.ops[name]:
                    for s_, v in wl:
                        e.wait_ge(P.sem[s_], v)
                    if fn is not None:
                        ins = fn(e)
                        ins.then_inc(P.sem[inc[0]], inc[1])
            return f

        with nc.Block() as block:
            block.tensor(run('pe'))
            block.scalar(run('act'))
            block.vector(run('dve'))
            block.gpsimd(run('pool'))
            block.sync(run('sp'))
        self.ph.close()
        self.ph = ExitStack()
        self.phase_id += 1
        self.ops = {e: [] for e in self.ENG}
        self.barrier = {('e_' + e): self.cnt[e] for e in self.ENG if self.cnt[e] > 0}
        self.barrier.update(self.dcnt)
        if final:
            self.es.close()

    def emit(self):
        self.end_phase(final=False)
        self.es.close()


def mkD(nc, pre, ext):
    def D(n, s, dt, k):
        if ext and n in ext:
            return ext[n]
        return nc.dram_tensor(pre + n, s, dt, kind=k).ap()
    return D


T = 4096
NB = T // 512


def build_A(P=None, pre="", ext=None):
    own = P is None
    if own:
        P = Prog(bass.Bass("TRN2", target_bir_lowering=False))
    nc = P.nc
    D = mkD(nc, pre, ext)
    xT = D("xT", [1024, T], F32, "ExternalInput")
    cb = D("cb", [128, 8], F32, "ExternalInput")
    adaw = D("adaw", [1024, 2048], F32, "ExternalInput")
    adab = D("adab", [128, 16], F32, "ExternalInput")
    n1w = D("n1w", [128, 8], F32, "ExternalInput")
    win = D("win", [1024, 4096], F32, "ExternalInput")
    qkw = D("qkw", [128, 2], F32, "ExternalInput")
    uT = D("uT", [512, T], F32, "ExternalOutput")
    qT = D("qT", [512, T], BF16, "ExternalOutput")
    kT = D("kT", [512, T], BF16, "ExternalOutput")
    vT = D("vT", [512, T], BF16, "ExternalOutput")
    gT = D("gT", [2048, T], F32, "ExternalOutput")

    wbf = P.sb("wbf", [128, 8, 4096], BF16)
    stg = [P.sb(f"stg{i}", [128, 2048], F32) for i in range(2)]
    c_sb = P.sb("c_sb", [128, 8], F32)
    ca_sb = P.sb("ca_sb", [128, 8], F32)
    adab_sb = P.sb("adab_sb", [128, 16], F32)
    n1w_sb = P.sb("n1w_sb", [128, 8], F32)
    qkw_sb = P.sb("qkw_sb", [128, 2], F32)
    modT = P.sb("modT", [128, 16], F32)
    wmod = P.sb("wmod", [128, 8], F32)
    ones = P.sb("ones", [128, 128], F32)
    blk = P.sb("blk", [128, 128], F32)
    epsb = P.sb("epsb", [128, 1], F32)
    xb = [P.sb(f"xb{i}", [128, 8, 512], F32) for i in range(2)]
    sq = [P.sb(f"sq{i}", [128, 512], F32) for i in range(2)]
    rstd = P.sb("rstd", [128, 512], F32)
    tmp = [P.sb(f"tmp{i}", [128, 512], F32) for i in range(2)]
    hT = P.sb("hT", [128, 8, 512], BF16)
    of32 = [P.sb(f"of32_{i}", [128, 512], F32) for i in range(4)]
    obf = [P.sb(f"obf_{i}", [128, 512], BF16) for i in range(4)]
    sq2 = [P.sb(f"sq2_{i}", [128, 512], F32) for i in range(2)]
    r2 = [P.sb(f"r2_{i}", [128, 512], F32) for i in range(2)]
    pz = [P.ps(f"pz{i}", [128, 512]) for i in range(4)]
    pss = [P.ps(f"pss{i}", [128, 512]) for i in range(2)]
    pn = P.ps("pn", [128, 512])
    pm = P.ps("pm", [128, 16])

    P.op('pool', lambda e: e.memset(ones[:], 1.0), writes=['ones'])
    P.op('pool', lambda e: e.memset(blk[:], 0.0), writes=['blk'])
    P.op('pool', lambda e: e.memset(blk[0:64, 0:64], 1.0), writes=['blk'])
    P.op('pool', lambda e: e.memset(blk[64:128, 64:128], 1.0), writes=['blk'])
    P.op('pool', lambda e: e.memset(epsb[:], 1e-6), writes=['epsb'])
    for nm, dst, src in (('c', c_sb, cb), ('adab', adab_sb, adab), ('n1w', n1w_sb, n1w), ('qkw', qkw_sb, qkw)):
        P.op('sp', (lambda dst, src: lambda e: e.dma_start(out=dst[:], in_=src))(dst, src), writes=[nm], dma='sm_' + nm)
    P.op('act', lambda e: e.activation(out=ca_sb[:], in_=c_sb[:], func=AF.Silu), reads=['c'], writes=['ca'])

    for q4 in range(4):
        xs = q4 % 2
        P.op('sp', (lambda xs, q4: lambda e: e.dma_start(out=xb[xs][:], in_=adaw[:, q4 * 512:(q4 + 1) * 512].rearrange("(kc p) t -> p kc t", p=128)))(xs, q4),
             writes=[('xb', xs)], dma=f'xb{xs}')
        for mm in range(4):
            m = q4 * 4 + mm
            for kc in range(8):
                P.op('pe', (lambda xs, kc, m, mm: lambda e: e.matmul(pm[:, m:m + 1], lhsT=xb[xs][:, kc, mm * 128:(mm + 1) * 128],
                                                                  rhs=ca_sb[:, kc:kc + 1], start=(kc == 0), stop=(kc == 7)))(xs, kc, m, mm),
                     reads=[('xb', xs), 'ca'], writes=['pm'])
    P.op('dve', lambda e: e.tensor_tensor(out=modT[:], in0=pm[:], in1=adab_sb[:], op=ALU.add),
         reads=['pm', 'adab'], writes=['modT'])
    P.op('dve', lambda e: e.scalar_tensor_tensor(out=wmod[:], in0=modT[:, 8:16], scalar=1.0, in1=n1w_sb[:],
                                                op0=ALU.add, op1=ALU.mult),
         reads=['modT', 'n1w'], writes=['wmod'])

    for kc in range(8):
        for hh in range(2):
            s = (kc * 2 + hh) % 2
            P.op('sp', (lambda s, kc, hh: lambda e: e.dma_start(out=stg[s][:], in_=win[kc * 128:(kc + 1) * 128, hh * 2048:(hh + 1) * 2048]))(s, kc, hh),
                 writes=[('stg', s)], dma=f'stg{s}')
            P.op('pool', (lambda s, kc, hh: lambda e: e.tensor_copy(out=wbf[:, kc, hh * 2048:(hh + 1) * 2048], in_=stg[s][:]))(s, kc, hh),
                 reads=[('stg', s)], writes=[('wbf', kc, hh)])

    oi = 0
    for tb in range(NB):
        xs = tb % 2
        tsl = slice(tb * 512, (tb + 1) * 512)
        P.op('sp', (lambda xs, tsl: lambda e: e.dma_start(out=xb[xs][:], in_=xT[:, tsl].rearrange("(kc p) t -> p kc t", p=128)))(xs, tsl),
             writes=[('xb', xs)], dma=f'xb{xs}')
        for kc in range(8):
            s = kc % 2
            P.op('act', (lambda xs, kc, s: lambda e: e.activation(out=sq[s][:], in_=xb[xs][:, kc, :], func=AF.Square))(xs, kc, s),
                 reads=[('xb', xs)], writes=[('sq', s)])
            P.op('pe', (lambda kc, s: lambda e: e.matmul(pn[:], lhsT=ones[:], rhs=sq[s][:], start=(kc == 0), stop=(kc == 7)))(kc, s),
                 reads=[('sq', s), 'ones'], writes=['pn'])
        P.op('act', lambda e: e.activation(out=rstd[:], in_=pn[:], func=AF.Sqrt, bias=epsb[:], scale=1.0 / 1024.0),
             reads=['pn', 'epsb'], writes=['rstd'])
        P.op('dve', lambda e: e.reciprocal(out=rstd[:], in_=rstd[:]), reads=['rstd'], writes=['rstd'])
        for kc in range(8):
            s = kc % 2
            P.op('dve', (lambda xs, kc, s: lambda e: e.tensor_tensor(out=tmp[s][:], in0=xb[xs][:, kc, :], in1=rstd[:], op=ALU.mult))(xs, kc, s),
                 reads=[('xb', xs), 'rstd'], writes=[('tmp', s)])
            P.op('act', (lambda kc, s: lambda e: e.activation(out=hT[:, kc, :], in_=tmp[s][:], func=AF.Identity,
                                                             bias=modT[:, kc:kc + 1], scale=wmod[:, kc:kc + 1]))(kc, s),
                 reads=[('tmp', s), 'modT', 'wmod'], writes=[('hT', kc)])
        for m in range(32):
            zs = m % 4
            for kc in range(8):
                P.op('pe', (lambda zs, kc, m: lambda e: e.matmul(pz[zs][:], lhsT=wbf[:, kc, m * 128:(m + 1) * 128], rhs=hT[:, kc, :],
                                                                start=(kc == 0), stop=(kc == 7)))(zs, kc, m),
                     reads=[('hT', kc), ('wbf', kc, m // 16)], writes=[('pz', zs)])
            o = oi % 4
            oi += 1
            msl = slice((m % 4) * 128, (m % 4 + 1) * 128)
            if m < 4:
                P.op('dve', (lambda zs, o: lambda e: e.tensor_copy(out=of32[o][:], in_=pz[zs][:]))(zs, o),
                     reads=[('pz', zs)], writes=[('of32', o)])
                P.op('sp', (lambda o, msl, tsl: lambda e: e.dma_start(out=uT[msl, tsl], in_=of32[o][:]))(o, msl, tsl),
                     reads=[('of32', o)], dma=f'of32_{o}')
            elif m < 12:
                s = m % 2
                wcol = 0 if m < 8 else 1
                dst = qT if m < 8 else kT
                P.op('act', (lambda zs, s: lambda e: e.activation(out=sq2[s][:], in_=pz[zs][:], func=AF.Square))(zs, s),
                     reads=[('pz', zs)], writes=[('sq2', s)])
                P.op('pe', (lambda s: lambda e: e.matmul(pss[s][:], lhsT=blk[:], rhs=sq2[s][:], start=True, stop=True))(s),
                     reads=[('sq2', s), 'blk'], writes=[('pss', s)])
                P.op('act', (lambda s: lambda e: e.activation(out=r2[s][:], in_=pss[s][:], func=AF.Sqrt, bias=epsb[:], scale=1.0 / 64.0))(s),
                     reads=[('pss', s), 'epsb'], writes=[('r2', s)])
                P.op('dve', (lambda s: lambda e: e.reciprocal(out=r2[s][:], in_=r2[s][:]))(s), reads=[('r2', s)], writes=[('r2', s)])
                P.op('dve', (lambda zs, s, o, wcol: lambda e: e.scalar_tensor_tensor(out=obf[o][:], in0=pz[zs][:], scalar=qkw_sb[:, wcol:wcol + 1],
                                                                                    in1=r2[s][:], op0=ALU.mult, op1=ALU.mult))(zs, s, o, wcol),
                     reads=[('pz', zs), ('r2', s), 'qkw'], writes=[('obf', o)])
                P.op('sp', (lambda o, msl, tsl, dst: lambda e: e.dma_start(out=dst[msl, tsl], in_=obf[o][:]))(o, msl, tsl, dst),
                     reads=[('obf', o)], dma=f'obf_{o}')
            elif m < 16:
                P.op('act', (lambda zs, o: lambda e: e.activation(out=obf[o][:], in_=pz[zs][:], func=AF.Identity))(zs, o),
                     reads=[('pz', zs)], writes=[('obf', o)])
                P.op('sp', (lambda o, msl, tsl: lambda e: e.dma_start(out=vT[msl, tsl], in_=obf[o][:]))(o, msl, tsl),
                     reads=[('obf', o)], dma=f'obf_{o}')
            else:
                gsl = slice((m - 16) * 128, (m - 15) * 128)
                P.op('act', (lambda zs, o: lambda e: e.activation(out=of32[o][:], in_=pz[zs][:], func=AF.Sigmoid))(zs, o),
                     reads=[('pz', zs)], writes=[('of32', o)])
                P.op('sp', (lambda o, gsl, tsl: lambda e: e.dma_start(out=gT[gsl, tsl], in_=of32[o][:]))(o, gsl, tsl),
                     reads=[('of32', o)], dma=f'of32_{o}')
    if own:
        P.end_phase(final=True)
        return nc
    return {}


def layout_A(inp, l, r, xT_full):
    b, j = r // 4, r % 4
    f = lambda a: np.ascontiguousarray(a, dtype=np.float32)
    qn = np.tile(inp['q_norm_w'][l], 2)
    kn = np.tile(inp['k_norm_w'][l], 2)
    return {
        "xT": None if xT_full is None else f(xT_full[b][:, j * T:(j + 1) * T]),
        "cb": f(inp['c'][b].reshape(8, 128).T),
        "adaw": f(inp['ada_w'][l][:, 0:2048]),
        "adab": f(inp['ada_b'][l][0:2048].reshape(16, 128).T),
        "n1w": f(inp['norm1_w'][l].reshape(8, 128).T),
        "win": f(inp['w_in'][l]),
        "qkw": f(np.stack([qn, kn], axis=1)),
    }


S = 16384
L = 1024
NSEG = S // L
PI = math.pi


def build_B1(nseg=NSEG, P=None, pre="", ext=None):
    own = P is None
    if own:
        P = Prog(bass.Bass("TRN2", target_bir_lowering=False))
    nc = P.nc
    D = mkD(nc, pre, ext)
    uT = D("uT", [128, S], F32, "ExternalInput")
    prm = D("prm", [128, 12], F32, "ExternalInput")
    bpad = D("bpad", [128, 8, 128], F32, "ExternalInput")
    cpad = D("cpad", [128, 8, 128], F32, "ExternalInput")
    dsk = D("dsk", [128, 1], F32, "ExternalInput")
    yT = D("yT", [128, S], BF16, "ExternalOutput")

    prm_sb = P.sb("prm_sb", [128, 12], F32)
    bpad_sb = P.sb("bpad_sb", [128, 8, 128], F32)
    cpad_sb = P.sb("cpad_sb", [128, 8, 128], F32)
    dsk_sb = P.sb("dsk_sb", [128, 1], F32)
    sm = P.sb("sm", [128, 64], F32)
    npi = P.sb("npi", [128, 1], F32)
    qi_sb = P.sb("qi_sb", [128, 4], I32)
    ident = P.sb("ident", [128, 128], F32)
    iot = P.sb("iot", [128, 128], F32)
    bbp = P.sb("bbp", [128, 8, 128], F32)
    btmp = P.sb("btmp", [128, 128], F32)
    W = P.sb("W", [128, 8, 128], F32)
    Ec = P.sb("Ec", [128, 4, L], F32)
    Es = P.sb("Es", [128, 4, L], F32)
    etmp = P.sb("etmp", [128, L], F32)
    rot = P.sb("rot", [128, 8], F32)
    carry = P.sb("carry", [128, 8], F32)
    ctmp = P.sb("ctmp", [128, 4], F32)
    ub = [P.sb(f"ub{i}", [128, L], F32) for i in range(2)]
    m = [[P.sb(f"m{k}_{i}", [128, 512], F32) for i in range(2)] for k in range(4)]
    win_r = P.sb("win_r", [128, L], F32)
    win_i = P.sb("win_i", [128, L], F32)
    w_r = P.sb("w_r", [128, L], F32)
    w_i = P.sb("w_i", [128, L], F32)
    dd = [P.sb(f"dd{k}", [128, L], F32) for k in range(4)]
    xr = P.sb("xr", [128, 4, L], F32)
    xi = P.sb("xi", [128, 4, L], F32)
    ysb = [P.sb(f"ysb{i}", [128, 512], F32) for i in range(2)]
    gt = [P.sb(f"gt{i}", [128, 512], F32) for i in range(2)]
    gs = [P.sb(f"gs{i}", [128, 512], F32) for i in range(2)]
    yo = [P.sb(f"yo{i}", [128, 512], BF16) for i in range(2)]
    psr = [P.ps(f"psr{i}", [128, 512]) for i in range(2)]
    psi = [P.ps(f"psi{i}", [128, 512]) for i in range(2)]
    psy = [P.ps(f"psy{i}", [128, 512]) for i in range(2)]
    pst = P.ps("pst", [128, 128])

    col = lambda k: sm[:, k:k + 1]
    for nm, dst, src in (('prm', prm_sb, prm), ('bpad', bpad_sb, bpad), ('cpad', cpad_sb, cpad), ('dsk', dsk_sb, dsk)):
        P.op('sp', (lambda dst, src: lambda e: e.dma_start(out=dst[:], in_=src))(dst, src), writes=[nm], dma='sm_' + nm)
    P.op('pool', lambda e: e.memset(npi[:], -PI), writes=['npi'])
    P.op('pool', lambda e: e.memset(carry[:], 0.0), writes=['carry'])
    P.op('pool', lambda e: e.iota(iot[:], pattern=[[1, 128]], base=0, channel_multiplier=-1, allow_small_or_imprecise_dtypes=True), writes=['iot'])
    P.op('dve', lambda e: e.tensor_single_scalar(out=ident[:], in_=iot[:], scalar=0.0, op=ALU.is_equal), reads=['iot'], writes=['ident'])
    P.op('act', lambda e: e.mul(out=cpad_sb[:, 4:8, :], in_=cpad_sb[:, 4:8, :], mul=-1.0), reads=['cpad'], writes=['cpad'])

    c4 = lambda k: sm[:, k:k + 4]
    ldt, are, aim = prm_sb[:, 0:4], prm_sb[:, 4:8], prm_sb[:, 8:12]
    seq = []
    def dv(fn, reads=('sm',), writes=('sm',)):
        P.op('dve', fn, reads=list(reads) + ['prm'], writes=list(writes))
    def ac(fn):
        P.op('act', fn, reads=['sm', 'prm', 'npi'], writes=['sm'])
    ac(lambda e: e.activation(out=c4(0), in_=ldt, func=AF.Exp))
    dv(lambda e: e.tensor_tensor(out=c4(44), in0=are, in1=c4(0), op=ALU.mult))
    ac(lambda e: e.activation(out=c4(4), in_=c4(44), func=AF.Exp))
    dv(lambda e: e.tensor_tensor(out=c4(8), in0=aim, in1=c4(0), op=ALU.mult))
    def red_sin(dst, shift):
        dv(lambda e: e.tensor_scalar_add(out=c4(52), in0=c4(8), scalar1=shift))
        dv(lambda e: e.tensor_scalar_mul(out=c4(44), in0=c4(52), scalar1=1.0 / (2 * PI)))
        P.op('dve', lambda e: e.tensor_copy(out=qi_sb[:], in_=c4(44)), reads=['sm'], writes=['qi'])
        P.op('dve', lambda e: e.tensor_copy(out=c4(44), in_=qi_sb[:]), reads=['qi'], writes=['sm'])
        dv(lambda e: e.scalar_tensor_tensor(out=c4(48), in0=c4(44), scalar=-2 * PI, in1=c4(52), op0=ALU.mult, op1=ALU.add))
        dv(lambda e: e.tensor_single_scalar(out=c4(44), in_=c4(48), scalar=PI, op=ALU.is_gt))
        dv(lambda e: e.scalar_tensor_tensor(out=c4(48), in0=c4(44), scalar=-2 * PI, in1=c4(48), op0=ALU.mult, op1=ALU.add))
        dv(lambda e: e.tensor_single_scalar(out=c4(44), in_=c4(48), scalar=-PI, op=ALU.is_lt))
        dv(lambda e: e.scalar_tensor_tensor(out=c4(48), in0=c4(44), scalar=2 * PI, in1=c4(48), op0=ALU.mult, op1=ALU.add))
        ac(lambda e: e.activation(out=c4(dst), in_=c4(48), func=AF.Sin))
    red_sin(16, 0.0)
    red_sin(12, 0.5 * PI)
    dv(lambda e: e.tensor_tensor(out=c4(20), in0=c4(4), in1=c4(12), op=ALU.mult))
    dv(lambda e: e.tensor_tensor(out=c4(24), in0=c4(4), in1=c4(16), op=ALU.mult))
    dv(lambda e: e.tensor_tensor(out=c4(44), in0=are, in1=are, op=ALU.mult))
    dv(lambda e: e.tensor_tensor(out=c4(48), in0=aim, in1=aim, op=ALU.mult))
    dv(lambda e: e.tensor_tensor(out=c4(28), in0=c4(44), in1=c4(48), op=ALU.add))
    dv(lambda e: e.reciprocal(out=c4(28), in_=c4(28)))
    dv(lambda e: e.tensor_scalar_add(out=c4(32), in0=c4(20), scalar1=-1.0))
    dv(lambda e: e.tensor_tensor(out=c4(44), in0=c4(32), in1=are, op=ALU.mult))
    dv(lambda e: e.tensor_tensor(out=c4(48), in0=c4(24), in1=aim, op=ALU.mult))
    dv(lambda e: e.tensor_tensor(out=c4(44), in0=c4(44), in1=c4(48), op=ALU.add))
    dv(lambda e: e.tensor_tensor(out=c4(36), in0=c4(44), in1=c4(28), op=ALU.mult))
    dv(lambda e: e.tensor_tensor(out=c4(44), in0=c4(24), in1=are, op=ALU.mult))
    dv(lambda e: e.tensor_tensor(out=c4(48), in0=c4(32), in1=aim, op=ALU.mult))
    dv(lambda e: e.tensor_tensor(out=c4(44), in0=c4(44), in1=c4(48), op=ALU.subtract))
    dv(lambda e: e.tensor_tensor(out=c4(40), in0=c4(44), in1=c4(28), op=ALU.mult))

    for gp in range(4):
        fre, fim = col(36 + gp), col(40 + gp)
        P.op('dve', (lambda gp, fim: lambda e: e.tensor_scalar_mul(out=btmp[:], in0=bpad_sb[:, 4 + gp, :], scalar1=fim))(gp, fim),
             reads=['sm', 'bpad'], writes=['btmp'])
        P.op('dve', (lambda gp, fre: lambda e: e.scalar_tensor_tensor(out=bbp[:, gp, :], in0=bpad_sb[:, gp, :], scalar=fre, in1=btmp[:],
                                                                      op0=ALU.mult, op1=ALU.subtract))(gp, fre),
             reads=['sm', 'bpad', 'btmp'], writes=[('bbp', gp)])
        P.op('dve', (lambda gp, fim: lambda e: e.tensor_scalar_mul(out=btmp[:], in0=bpad_sb[:, gp, :], scalar1=fim))(gp, fim),
             reads=['sm', 'bpad'], writes=['btmp'])
        P.op('dve', (lambda gp, fre: lambda e: e.scalar_tensor_tensor(out=bbp[:, 4 + gp, :], in0=bpad_sb[:, 4 + gp, :], scalar=fre, in1=btmp[:],
                                                                      op0=ALU.mult, op1=ALU.add))(gp, fre),
             reads=['sm', 'bpad', 'btmp'], writes=[('bbp', 4 + gp)])
        for k in (gp, 4 + gp):
            P.op('pe', (lambda k: lambda e: e.transpose(out=pst[:], in_=bbp[:, k, :], identity=ident[:]))(k),
                 reads=[('bbp', k), 'ident'], writes=['pst'])
            P.op('act', (lambda k: lambda e: e.copy(out=W[:, k, :], in_=pst[:]))(k), reads=['pst'], writes=[('W', k)])

    for gp in range(4):
        c1, s1 = col(12 + gp), col(16 + gp)
        ec, es = Ec[:, gp, :], Es[:, gp, :]
        tg = ('E', gp)
        P.op('pool', (lambda ec: lambda e: e.memset(ec[:, 0:1], 1.0))(ec), writes=[tg])
        P.op('pool', (lambda es: lambda e: e.memset(es[:, 0:1], 0.0))(es), writes=[tg])
        P.op('dve', (lambda ec, c1: lambda e: e.tensor_copy(out=ec[:, 1:2], in_=c1))(ec, c1), reads=['sm', tg], writes=[tg])
        P.op('dve', (lambda es, s1: lambda e: e.tensor_copy(out=es[:, 1:2], in_=s1))(es, s1), reads=['sm', tg], writes=[tg])
        n = 2
        while n <= L:
            P.op('dve', (lambda es, n, s1: lambda e: e.tensor_tensor(out=ctmp[:, 2:3], in0=es[:, n - 1:n], in1=s1, op=ALU.mult))(es, n, s1),
                 reads=[tg, 'sm'], writes=['ctmp'])
            P.op('dve', (lambda ec, n, c1: lambda e: e.scalar_tensor_tensor(out=ctmp[:, 0:1], in0=ec[:, n - 1:n], scalar=c1, in1=ctmp[:, 2:3],
                                                                           op0=ALU.mult, op1=ALU.subtract))(ec, n, c1),
                 reads=[tg, 'sm', 'ctmp'], writes=['ctmp'])
            P.op('dve', (lambda es, n, c1: lambda e: e.tensor_tensor(out=ctmp[:, 2:3], in0=es[:, n - 1:n], in1=c1, op=ALU.mult))(es, n, c1),
                 reads=[tg, 'sm', 'ctmp'], writes=['ctmp'])
            P.op('dve', (lambda ec, n, s1: lambda e: e.scalar_tensor_tensor(out=ctmp[:, 1:2], in0=ec[:, n - 1:n], scalar=s1, in1=ctmp[:, 2:3],
                                                                           op0=ALU.mult, op1=ALU.add))(ec, n, s1),
                 reads=[tg, 'sm', 'ctmp'], writes=['ctmp'])
            if n == L:
                P.op('dve', (lambda gp: lambda e: e.tensor_copy(out=rot[:, gp:gp + 1], in_=ctmp[:, 0:1]))(gp), reads=['ctmp'], writes=['rot'])
                P.op('dve', (lambda gp: lambda e: e.tensor_copy(out=rot[:, 4 + gp:5 + gp], in_=ctmp[:, 1:2]))(gp), reads=['ctmp'], writes=['rot'])
                break
            P.op('dve', (lambda es, n: lambda e: e.tensor_scalar_mul(out=etmp[:, 0:n], in0=es[:, 0:n], scalar1=ctmp[:, 1:2]))(es, n),
                 reads=[tg, 'ctmp'], writes=['etmp'])
            P.op('dve', (lambda ec, n: lambda e: e.scalar_tensor_tensor(out=ec[:, n:2 * n], in0=ec[:, 0:n], scalar=ctmp[:, 0:1], in1=etmp[:, 0:n],
                                                                       op0=ALU.mult, op1=ALU.subtract))(ec, n),
                 reads=[tg, 'ctmp', 'etmp'], writes=[tg])
            P.op('dve', (lambda ec, n: lambda e: e.tensor_scalar_mul(out=etmp[:, 0:n], in0=ec[:, 0:n], scalar1=ctmp[:, 1:2]))(ec, n),
                 reads=[tg, 'ctmp'], writes=['etmp'])
            P.op('dve', (lambda es, n: lambda e: e.scalar_tensor_tensor(out=es[:, n:2 * n], in0=es[:, 0:n], scalar=ctmp[:, 0:1], in1=etmp[:, 0:n],
                                                                       op0=ALU.mult, op1=ALU.add))(es, n),
                 reads=[tg, 'ctmp', 'etmp'], writes=[tg])
            n *= 2

    mi = 0
    yi = 0
    for seg in range(nseg):
        us = seg % 2
        ssl = slice(seg * L, (seg + 1) * L)
        P.op('sp', (lambda us, ssl: lambda e: e.dma_start(out=ub[us][:], in_=uT[:, ssl]))(us, ssl), writes=[('ub', us)], dma=f'ub{us}')
        for gp in range(4):
            tg = ('E', gp)
            for tb in range(L // 512):
                bs = mi % 2
                mi += 1
                fs = slice(tb * 512, (tb + 1) * 512)
                P.op('pe', (lambda gp, us, fs, bs: lambda e: e.matmul(psr[bs][:], lhsT=W[:, gp, :], rhs=ub[us][:, fs], start=True, stop=True))(gp, us, fs, bs),
                     reads=[('W', gp), ('ub', us)], writes=[('psr', bs)])
                P.op('pe', (lambda gp, us, fs, bs: lambda e: e.matmul(psi[bs][:], lhsT=W[:, 4 + gp, :], rhs=ub[us][:, fs], start=True, stop=True))(gp, us, fs, bs),
                     reads=[('W', 4 + gp), ('ub', us)], writes=[('psi', bs)])
                ecs, ess = Ec[:, gp, fs], Es[:, gp, fs]
                for k, (src, srcn, tab) in enumerate(((psr, 'psr', ecs), (psi, 'psi', ess), (psi, 'psi', ecs), (psr, 'psr', ess))):
                    P.op('dve', (lambda k, src, tab, bs: lambda e: e.tensor_tensor(out=m[k][bs][:], in0=src[bs][:], in1=tab, op=ALU.mult))(k, src, tab, bs),
                         reads=[(srcn, bs), tg], writes=[('m', k, bs)])
                P.op('pool', (lambda bs, fs: lambda e: e.tensor_tensor(out=win_r[:, fs], in0=m[0][bs][:], in1=m[1][bs][:], op=ALU.add))(bs, fs),
                     reads=[('m', 0, bs), ('m', 1, bs)], writes=[('win_r', tb)])
                P.op('pool', (lambda bs, fs: lambda e: e.tensor_tensor(out=win_i[:, fs], in0=m[2][bs][:], in1=m[3][bs][:], op=ALU.subtract))(bs, fs),
                     reads=[('m', 2, bs), ('m', 3, bs)], writes=[('win_i', tb)])
            rho = sm[:, 4 + gp:5 + gp].to_broadcast([128, L])
            P.op('dve', (lambda gp, rho: lambda e: e.tensor_tensor_scan(out=w_r[:], data0=rho, data1=win_r[:], initial=carry[:, gp:gp + 1],
                                                                       op0=ALU.mult, op1=ALU.add))(gp, rho),
                 reads=[('win_r', 0), ('win_r', 1), 'sm', 'carry'], writes=['w_r'])
            P.op('dve', (lambda gp, rho: lambda e: e.tensor_tensor_scan(out=w_i[:], data0=rho, data1=win_i[:], initial=carry[:, 4 + gp:5 + gp],
                                                                       op0=ALU.mult, op1=ALU.add))(gp, rho),
                 reads=[('win_i', 0), ('win_i', 1), 'sm', 'carry'], writes=['w_i'])
            rc, rs_ = rot[:, gp:gp + 1], rot[:, 4 + gp:5 + gp]
            P.op('dve', (lambda rs_: lambda e: e.tensor_tensor(out=ctmp[:, 0:1], in0=w_i[:, L - 1:L], in1=rs_, op=ALU.mult))(rs_),
                 reads=['w_i', 'rot'], writes=['ctmp'])
            P.op('dve', (lambda rc: lambda e: e.tensor_tensor(out=ctmp[:, 1:2], in0=w_i[:, L - 1:L], in1=rc, op=ALU.mult))(rc),
                 reads=['w_i', 'rot'], writes=['ctmp'])
            P.op('dve', (lambda gp, rc: lambda e: e.scalar_tensor_tensor(out=carry[:, gp:gp + 1], in0=w_r[:, L - 1:L], scalar=rc, in1=ctmp[:, 0:1],
                                                                        op0=ALU.mult, op1=ALU.subtract))(gp, rc),
                 reads=['w_r', 'rot', 'ctmp'], writes=['carry'])
            P.op('dve', (lambda gp, rs_: lambda e: e.scalar_tensor_tensor(out=carry[:, 4 + gp:5 + gp], in0=w_r[:, L - 1:L], scalar=rs_, in1=ctmp[:, 1:2],
                                                                         op0=ALU.mult, op1=ALU.add))(gp, rs_),
                 reads=['w_r', 'rot', 'ctmp'], writes=['carry'])
            ecl, esl = Ec[:, gp, :], Es[:, gp, :]
            P.op('pool', (lambda ecl: lambda e: e.tensor_tensor(out=dd[0][:], in0=w_r[:], in1=ecl, op=ALU.mult))(ecl), reads=['w_r', tg], writes=[('dd', 0)])
            P.op('pool', (lambda esl: lambda e: e.tensor_tensor(out=dd[1][:], in0=w_i[:], in1=esl, op=ALU.mult))(esl), reads=['w_i', tg], writes=[('dd', 1)])
            P.op('dve', (lambda gp: lambda e: e.tensor_tensor(out=xr[:, gp, :], in0=dd[0][:], in1=dd[1][:], op=ALU.subtract))(gp),
                 reads=[('dd', 0), ('dd', 1)], writes=[('xr', gp)])
            P.op('pool', (lambda esl: lambda e: e.tensor_tensor(out=dd[2][:], in0=w_r[:], in1=esl, op=ALU.mult))(esl), reads=['w_r', tg], writes=[('dd', 2)])
            P.op('dve', (lambda ecl: lambda e: e.tensor_tensor(out=dd[3][:], in0=w_i[:], in1=ecl, op=ALU.mult))(ecl), reads=['w_i', tg], writes=[('dd', 3)])
            P.op('dve', (lambda gp: lambda e: e.tensor_tensor(out=xi[:, gp, :], in0=dd[2][:], in1=dd[3][:], op=ALU.add))(gp),
                 reads=[('dd', 2), ('dd', 3)], writes=[('xi', gp)])
        for tb in range(L // 512):
            ys = yi % 2
            yi += 1
            fs = slice(tb * 512, (tb + 1) * 512)
            for gp in range(4):
                P.op('pe', (lambda gp, fs, ys: lambda e: e.matmul(psy[ys][:], lhsT=cpad_sb[:, gp, :], rhs=xr[:, gp, fs], start=(gp == 0), stop=False))(gp, fs, ys),
                     reads=['cpad', ('xr', gp)], writes=[('psy', ys)])
                P.op('pe', (lambda gp, fs, ys: lambda e: e.matmul(psy[ys][:], lhsT=cpad_sb[:, 4 + gp, :], rhs=xi[:, gp, fs], start=False, stop=(gp == 3)))(gp, fs, ys),
                     reads=['cpad', ('xi', gp)], writes=[('psy', ys)])
            P.op('dve', (lambda us, fs, ys: lambda e: e.scalar_tensor_tensor(out=ysb[ys][:], in0=ub[us][:, fs], scalar=dsk_sb[:, 0:1], in1=psy[ys][:],
                                                                            op0=ALU.mult, op1=ALU.add))(us, fs, ys),
                 reads=[('ub', us), 'dsk', ('psy', ys)], writes=[('ysb', ys)])
            P.op('pool', (lambda ys: lambda e: e.tensor_tensor(out=gt[ys][:], in0=ysb[ys][:], in1=ysb[ys][:], op=ALU.mult))(ys),
                 reads=[('ysb', ys)], writes=[('gt', ys)])
            P.op('pool', (lambda ys: lambda e: e.tensor_scalar(out=gt[ys][:], in0=gt[ys][:], scalar1=0.044715, scalar2=1.0, op0=ALU.mult, op1=ALU.add))(ys),
                 reads=[('gt', ys)], writes=[('gt', ys)])
            P.op('pool', (lambda ys: lambda e: e.tensor_tensor(out=gt[ys][:], in0=gt[ys][:], in1=ysb[ys][:], op=ALU.mult))(ys),
                 reads=[('gt', ys), ('ysb', ys)], writes=[('gt', ys)])
            P.op('act', (lambda ys: lambda e: e.activation(out=gs[ys][:], in_=gt[ys][:], func=AF.Sigmoid, scale=2.0 * math.sqrt(2.0 / PI)))(ys),
                 reads=[('gt', ys)], writes=[('gs', ys)])
            P.op('dve', (lambda ys: lambda e: e.tensor_tensor(out=yo[ys][:], in0=ysb[ys][:], in1=gs[ys][:], op=ALU.mult))(ys),
                 reads=[('ysb', ys), ('gs', ys)], writes=[('yo', ys)])
            osl = slice(seg * L + tb * 512, seg * L + (tb + 1) * 512)
            P.op('sp', (lambda ys, osl: lambda e: e.dma_start(out=yT[:, osl], in_=yo[ys][:]))(ys, osl), reads=[('yo', ys)], dma=f'yo{ys}')
    if own:
        P.end_phase(final=True)
        return nc
    return {}


def layout_B1(inp, l, r, uT_full):
    b, gb = r // 4, r % 4
    f = lambda a: np.ascontiguousarray(a, dtype=np.float32)
    g0 = gb * 8
    prm = np.zeros((128, 12), np.float32)
    bpad = np.zeros((128, 8, 128), np.float32)
    cpad = np.zeros((128, 8, 128), np.float32)
    for gp in range(4):
        for gi in range(2):
            gl = 2 * gp + gi
            g = g0 + gl
            ps = slice(gi * 64, (gi + 1) * 64)
            prm[ps, gp] = inp['ssm_log_dt'][l][g]
            prm[ps, 4 + gp] = inp['ssm_a_re'][l][g]
            prm[ps, 8 + gp] = inp['ssm_a_im'][l][g]
            cs = slice(gl * 16, (gl + 1) * 16)
            bpad[ps, gp, cs] = inp['ssm_b_re'][l][g]
            bpad[ps, 4 + gp, cs] = inp['ssm_b_im'][l][g]
            cpad[ps, gp, cs] = inp['ssm_c_re'][l][g].T
            cpad[ps, 4 + gp, cs] = inp['ssm_c_im'][l][g].T
    return {
        "uT": f(uT_full[b][gb * 128:(gb + 1) * 128, :]),
        "prm": prm, "bpad": bpad, "cpad": cpad,
        "dsk": f(inp['ssm_d'][l].reshape(512)[gb * 128:(gb + 1) * 128].reshape(128, 1)),
    }


S = 16384


def build_B2(l, nqb=32, P=None, pre="", ext=None):
    lam_init = 0.8 - 0.6 * math.exp(-0.3 * l)
    own = P is None
    if own:
        P = Prog(bass.Bass("TRN2", target_bir_lowering=False))
    nc = P.nc
    D = mkD(nc, pre, ext)
    qT = D("qT", [128, S], BF16, "ExternalInput")
    kT = D("kT", [128, S], BF16, "ExternalInput")
    V = D("V", [128, 128, 128], BF16, "ExternalInput")
    lqk = D("lqk", [64, 4], F32, "ExternalInput")
    subw = D("subw", [128, 1], F32, "ExternalInput")
    oT = D("oT", [128, S], BF16, "ExternalOutput")

    kT_sb = P.sb("kT_sb", [128, S], BF16)
    V_sb = P.sb("V_sb", [128, 128, 128], BF16)
    qb_sb = [P.sb(f"qb{i}", [128, 512], BF16) for i in range(2)]
    lqk_sb = P.sb("lqk_sb", [64, 4], F32)
    subw_sb = P.sb("subw_sb", [128, 1], F32)
    prods = P.sb("prods", [64, 2], F32)
    E = P.sb("E", [128, 2], F32)
    nlam = P.sb("nlam", [128, 1], F32)
    onesf = P.sb("onesf", [128, 128], F32)
    epsb = P.sb("epsb", [128, 1], F32)
    iot = P.sb("iot", [128, 512], F32)
    masks = [P.sb(f"mask{j}", [128, 512], BF16) for j in range(4)]
    NPT = 6
    NPS = 4
    LA = 2
    pT = [P.sb(f"pT{i}", [128, 512], BF16) for i in range(NPT)]
    acc = [P.sb(f"acc{c}", [128, 512], F32) for c in range(2)]
    rl = [P.sb(f"rl{c}", [128, 512], F32) for c in range(2)]
    t01 = [P.sb(f"t01_{c}", [128, 512], F32) for c in range(2)]
    o_sb = P.sb("o_sb", [128, 512], F32)
    osq = P.sb("osq", [128, 512], F32)
    rs = P.sb("rs", [128, 512], F32)
    on = [P.sb(f"on{i}", [128, 512], BF16) for i in range(2)]
    ps_s = [P.ps(f"ps_s{i}", [128, 512]) for i in range(NPS)]
    ps_o = [[P.ps(f"ps_o{c}_{i}", [128, 512]) for i in range(1)] for c in range(2)]
    ps_l = P.ps("ps_l", [128, 512])
    ps_ls = P.ps("ps_ls", [128, 512])
    onesb = P.sb("onesb", [128, 128], BF16)

    P.op('pool', lambda e: e.memset(onesf[:], 1.0), writes=['onesf'])
    P.op('pool', lambda e: e.memset(onesb[:], 1.0), writes=['onesb'])
    P.op('pool', lambda e: e.memset(epsb[:], 1e-6), writes=['epsb'])
    P.op('pool', lambda e: e.iota(iot[:], pattern=[[1, 512]], base=0, channel_multiplier=-1,
                                  allow_small_or_imprecise_dtypes=True), writes=['iot'])
    for j in range(4):
        P.op('dve', (lambda j: lambda e: e.tensor_single_scalar(out=masks[j][:], in_=iot[:], scalar=float(128 * j), op=ALU.is_ge))(j),
             reads=['iot'], writes=[('mask', j)])
    P.op('sp', lambda e: e.dma_start(out=lqk_sb[:], in_=lqk), writes=['lqk'], dma='sm_lqk')
    P.op('sp', lambda e: e.dma_start(out=subw_sb[:], in_=subw), writes=['subw'], dma='sm_subw')
    for i in range(4):
        P.op('sp', (lambda i: lambda e: e.dma_start(out=kT_sb[:, i * 4096:(i + 1) * 4096], in_=kT[:, i * 4096:(i + 1) * 4096]))(i),
             writes=[('kT', i)], dma=f'kT{i}')
        P.op('sp', (lambda i: lambda e: e.dma_start(out=V_sb[:, i * 32:(i + 1) * 32, :], in_=V[:, i * 32:(i + 1) * 32, :]))(i),
             writes=[('V', i)], dma=f'V{i}')
    P.op('dve', lambda e: e.tensor_tensor(out=prods[:, 0:1], in0=lqk_sb[:, 0:1], in1=lqk_sb[:, 1:2], op=ALU.mult),
         reads=['lqk'], writes=['prods'])
    P.op('dve', lambda e: e.tensor_tensor(out=prods[:, 1:2], in0=lqk_sb[:, 2:3], in1=lqk_sb[:, 3:4], op=ALU.mult),
         reads=['lqk'], writes=['prods'])
    P.op('pe', lambda e: e.matmul(ps_l[:, 0:2], lhsT=onesf[0:64, :], rhs=prods[:], start=True, stop=True),
         reads=['prods', 'onesf'], writes=['ps_l'])
    P.op('act', lambda e: e.activation(out=E[:], in_=ps_l[:, 0:2], func=AF.Exp), reads=['ps_l'], writes=['E'])
    P.op('dve', lambda e: e.tensor_tensor(out=nlam[:], in0=E[:, 1:2], in1=E[:, 0:1], op=ALU.subtract), reads=['E'], writes=['nlam'])
    P.op('dve', lambda e: e.tensor_scalar_add(out=nlam[:], in0=nlam[:], scalar1=-lam_init), reads=['nlam'], writes=['nlam'])

    steps = []
    for qb in range(nqb):
        nkt = 4 * (qb + 1)
        for kt in range(nkt):
            for c in range(2):
                steps.append((qb, kt, c, nkt))
    n = len(steps)

    def front(si):
        qb, kt, c, nkt = steps[si]
        qs = qb % 2
        if kt == 0 and c == 0:
            qsl = slice(qb * 512, (qb + 1) * 512)
            P.op('sp', (lambda qs, qsl: lambda e: e.dma_start(out=qb_sb[qs][:], in_=qT[:, qsl]))(qs, qsl),
                 writes=[('qb', qs)], dma=f'qb{qs}')
        jd = kt - 4 * qb
        sb_i = si % NPS
        pi = si % NPT
        csl = slice(c * 64, (c + 1) * 64)
        P.op('pe', (lambda sb_i, kt, qs, csl: lambda e: e.matmul(ps_s[sb_i][:], lhsT=kT_sb[csl, kt * 128:(kt + 1) * 128],
                                                                rhs=qb_sb[qs][csl, :], start=True, stop=True))(sb_i, kt, qs, csl),
             reads=[('kT', kt // 32), ('qb', qs)], writes=[('ps_s', sb_i)])
        P.op('act', (lambda sb_i, pi: lambda e: e.activation(out=pT[pi][:], in_=ps_s[sb_i][:], func=AF.Exp, scale=0.125))(sb_i, pi),
             reads=[('ps_s', sb_i)], writes=[('pT', pi)])
        if jd >= 0:
            P.op('pool', (lambda pi, jd: lambda e: e.tensor_tensor(out=pT[pi][:], in0=pT[pi][:], in1=masks[jd][:], op=ALU.mult))(pi, jd),
                 reads=[('pT', pi), ('mask', jd)], writes=[('pT', pi)])

    def back(si):
        qb, kt, c, nkt = steps[si]
        pi = si % NPT
        ob_ = 0
        P.op('pe', (lambda c, ob_, pi, kt, nkt: lambda e: e.matmul(ps_o[c][ob_][:], lhsT=V_sb[:, kt, :], rhs=pT[pi][:],
                                                                  start=(kt == 0), stop=(kt == nkt - 1)))(c, ob_, pi, kt, nkt),
             reads=[('pT', pi), ('V', kt // 32)], writes=[('ps_o', c, ob_)])
        if c == 1:
            P.op('pe', (lambda pi, kt, nkt: lambda e: e.matmul(ps_ls[:], lhsT=onesb[:], rhs=pT[pi][:], start=(kt == 0), stop=(kt == nkt - 1)))(pi, kt, nkt),
                 reads=[('pT', pi), 'onesb'], writes=['ps_ls'])
        elif kt == 0:
            P.op('dve', (lambda c, pi: lambda e: e.tensor_copy(out=acc[c][:], in_=pT[pi][:]))(c, pi),
                 reads=[('pT', pi)], writes=[('acc', c)])
        else:
            P.op('dve', (lambda c, pi: lambda e: e.tensor_tensor(out=acc[c][:], in0=acc[c][:], in1=pT[pi][:], op=ALU.add))(c, pi),
                 reads=[('pT', pi), ('acc', c)], writes=[('acc', c)])
        if kt == nkt - 1 and c == 1:
            epilogue(qb)

    def epilogue(qb):
        ob_ = qb % 2
        qsl = slice(qb * 512, (qb + 1) * 512)
        for c in range(2):
            if c == 0:
                P.op('pe', (lambda c: lambda e: e.matmul(ps_l[:], lhsT=onesf[:], rhs=acc[c][:], start=True, stop=True))(c),
                     reads=[('acc', c), 'onesf'], writes=['ps_l'])
                P.op('dve', (lambda c: lambda e: e.reciprocal(out=rl[c][:], in_=ps_l[:]))(c), reads=['ps_l'], writes=[('rl', c)])
            else:
                P.op('dve', (lambda c: lambda e: e.reciprocal(out=rl[c][:], in_=ps_ls[:]))(c), reads=['ps_ls'], writes=[('rl', c)])
            P.op('dve', (lambda c: lambda e: e.tensor_tensor(out=t01[c][:], in0=ps_o[c][0][:], in1=rl[c][:], op=ALU.mult))(c),
                 reads=[('ps_o', c, 0), ('rl', c)], writes=[('t01', c)])
        P.op('dve', lambda e: e.scalar_tensor_tensor(out=o_sb[:], in0=t01[1][:], scalar=nlam[:, 0:1], in1=t01[0][:], op0=ALU.mult, op1=ALU.add),
             reads=[('t01', 0), ('t01', 1), 'nlam'], writes=['o_sb'])
        P.op('act', lambda e: e.activation(out=osq[:], in_=o_sb[:], func=AF.Square), reads=['o_sb'], writes=['osq'])
        P.op('pe', lambda e: e.matmul(ps_l[:], lhsT=onesf[:], rhs=osq[:], start=True, stop=True),
             reads=['osq', 'onesf'], writes=['ps_l'])
        P.op('act', lambda e: e.activation(out=rs[:], in_=ps_l[:], func=AF.Sqrt, bias=epsb[:], scale=1.0 / 128.0),
             reads=['ps_l', 'epsb'], writes=['rs'])
        P.op('dve', lambda e: e.reciprocal(out=rs[:], in_=rs[:]), reads=['rs'], writes=['rs'])
        P.op('dve', lambda e: e.tensor_scalar_mul(out=rs[:], in0=rs[:], scalar1=1.0 - lam_init), reads=['rs'], writes=['rs'])
        P.op('dve', (lambda ob_: lambda e: e.scalar_tensor_tensor(out=on[ob_][:], in0=o_sb[:], scalar=subw_sb[:, 0:1], in1=rs[:], op0=ALU.mult, op1=ALU.mult))(ob_),
             reads=['o_sb', 'rs', 'subw'], writes=[('on', ob_)])
        P.op('sp', (lambda ob_, qsl: lambda e: e.dma_start(out=oT[:, qsl], in_=on[ob_][:]))(ob_, qsl),
             reads=[('on', ob_)], dma=f'on{ob_}')

    npair = n // 2
    for p in range(npair + 1):
        if p < npair:
            front(2 * p)
            front(2 * p + 1)
        if p >= 1:
            back(2 * (p - 1))
            back(2 * (p - 1) + 1)
    if own:
        P.end_phase(final=True)
        return nc
    return {}


def layout_B2(inp, l, r, qT_full, kT_full, vT_full):
    b, h = r // 4, r % 4
    hs = slice(h * 128, (h + 1) * 128)
    Vh = np.ascontiguousarray(vT_full[b][hs, :].T)
    Vl = np.ascontiguousarray(Vh.reshape(128, 128, 128).transpose(1, 0, 2))
    f = lambda a: np.ascontiguousarray(a, dtype=np.float32)
    return {
        "qT": np.ascontiguousarray(qT_full[b][hs, :]),
        "kT": np.ascontiguousarray(kT_full[b][hs, :]),
        "V": Vl,
        "lqk": f(np.stack([inp['lambda_q1'][l], inp['lambda_k1'][l], inp['lambda_q2'][l], inp['lambda_k2'][l]], axis=1)),
        "subw": f(inp['subln_w'][l].reshape(128, 1)),
    }


T = 4096
NB = T // 512


def build_C1(P=None, pre="", ext=None):
    own = P is None
    if own:
        P = Prog(bass.Bass("TRN2", target_bir_lowering=False))
    nc = P.nc
    D = mkD(nc, pre, ext)
    xT = D("xT", [1024, T], F32, "ExternalInput")
    ysT = D("ysT", [512, T], BF16, "ExternalInput")
    oT = D("oT", [512, T], BF16, "ExternalInput")
    gT = D("gT", [2048, T], F32, "ExternalInput")
    cb = D("cb", [128, 8], F32, "ExternalInput")
    adaw = D("adaw", [1024, 1024], F32, "ExternalInput")
    adab = D("adab", [128, 8], F32, "ExternalInput")
    gluw = D("gluw", [512, 2048], F32, "ExternalInput")
    upw = D("upw", [512, 1024], F32, "ExternalInput")
    wout = D("wout", [1024, 1024], F32, "ExternalInput")
    x1T = D("x1T", [1024, T], F32, "ExternalOutput")

    glu_bf = P.sb("glu_bf", [128, 4, 2048], BF16)
    up_bf = P.sb("up_bf", [128, 4, 1024], BF16)
    wo_bf = P.sb("wo_bf", [128, 8, 1024], BF16)
    stg = [P.sb(f"stg{i}", [128, 2048], F32) for i in range(2)]
    c_sb = P.sb("c_sb", [128, 8], F32)
    ca_sb = P.sb("ca_sb", [128, 8], F32)
    adab_sb = P.sb("adab_sb", [128, 8], F32)
    g1 = P.sb("g1", [128, 8], F32)
    xb = [P.sb(f"xb{i}", [128, 8, 512], F32) for i in range(2)]
    ysb = [P.sb(f"ysb{i}", [128, 4, 512], BF16) for i in range(2)]
    ob = [P.sb(f"ob{i}", [128, 4, 512], BF16) for i in range(2)]
    gb = [P.sb(f"gb{i}", [128, 2, 512], F32) for i in range(2)]
    sg = [P.sb(f"sg{i}", [128, 512], F32) for i in range(2)]
    t1 = [P.sb(f"t1_{i}", [128, 512], F32) for i in range(2)]
    t2 = [P.sb(f"t2_{i}", [128, 512], F32) for i in range(2)]
    mg = P.sb("mg", [128, 8, 512], BF16)
    xo = [P.sb(f"xo{i}", [128, 512], F32) for i in range(2)]
    pb = [P.ps(f"pb{i}", [128, 512]) for i in range(7)]
    pm = P.ps("pm", [128, 8])
    bank = [0]

    def nb():
        bank[0] = (bank[0] + 1) % 7
        return bank[0]

    for nm, dst, src in (('c', c_sb, cb), ('adab', adab_sb, adab)):
        P.op('sp', (lambda dst, src: lambda e: e.dma_start(out=dst[:], in_=src))(dst, src), writes=[nm], dma='sm_' + nm)
    P.op('act', lambda e: e.activation(out=ca_sb[:], in_=c_sb[:], func=AF.Silu), reads=['c'], writes=['ca'])
    for q2 in range(2):
        xs = q2 % 2
        P.op('sp', (lambda xs, q2: lambda e: e.dma_start(out=xb[xs][:], in_=adaw[:, q2 * 512:(q2 + 1) * 512].rearrange("(kc p) t -> p kc t", p=128)))(xs, q2),
             writes=[('xb', xs)], dma=f'xb{xs}')
        for mm in range(4):
            m = q2 * 4 + mm
            for kc in range(8):
                P.op('pe', (lambda xs, kc, m, mm: lambda e: e.matmul(pm[:, m:m + 1], lhsT=xb[xs][:, kc, mm * 128:(mm + 1) * 128],
                                                                  rhs=ca_sb[:, kc:kc + 1], start=(kc == 0), stop=(kc == 7)))(xs, kc, m, mm),
                     reads=[('xb', xs), 'ca'], writes=['pm'])
    P.op('dve', lambda e: e.tensor_tensor(out=g1[:], in0=pm[:], in1=adab_sb[:], op=ALU.add), reads=['pm', 'adab'], writes=['g1'])

    si = 0
    def load_w(src, rows_kc, ncols, dst, tag):
        nonlocal si
        for kc in range(rows_kc):
            for c0 in range(0, ncols, 2048):
                cw = min(2048, ncols - c0)
                s = si % 2
                si += 1
                P.op('sp', (lambda s, kc, c0, cw: lambda e: e.dma_start(out=stg[s][:, 0:cw], in_=src[kc * 128:(kc + 1) * 128, c0:c0 + cw]))(s, kc, c0, cw),
                     writes=[('stg', s)], dma=f'stg{s}')
                P.op('pool', (lambda s, kc, c0, cw: lambda e: e.tensor_copy(out=dst[:, kc, c0:c0 + cw], in_=stg[s][:, 0:cw]))(s, kc, c0, cw),
                     reads=[('stg', s)], writes=[tag])
    load_w(gluw, 4, 2048, glu_bf, 'glu')
    load_w(upw, 4, 1024, up_bf, 'up')
    load_w(wout, 8, 1024, wo_bf, 'wo')

    gi = 0
    oi = 0
    for tb in range(NB):
        xs = tb % 2
        tsl = slice(tb * 512, (tb + 1) * 512)
        P.op('sp', (lambda xs, tsl: lambda e: e.dma_start(out=xb[xs][:], in_=xT[:, tsl].rearrange("(kc p) t -> p kc t", p=128)))(xs, tsl),
             writes=[('xb', xs)], dma=f'xb{xs}')
        P.op('sp', (lambda xs, tsl: lambda e: e.dma_start(out=ysb[xs][:], in_=ysT[:, tsl].rearrange("(kc p) t -> p kc t", p=128)))(xs, tsl),
             writes=[('ysb', xs)], dma=f'ysb{xs}')
        P.op('sp', (lambda xs, tsl: lambda e: e.dma_start(out=ob[xs][:], in_=oT[:, tsl].rearrange("(kc p) t -> p kc t", p=128)))(xs, tsl),
             writes=[('ob', xs)], dma=f'ob{xs}')
        for m in range(8):
            gs_ = gi % 2
            gi += 1
            P.op('sp', (lambda gs_, m, tsl: lambda e: e.dma_start(out=gb[gs_][:], in_=gT[:, tsl].rearrange("(two r) t -> r two t", two=2)[m * 128:(m + 1) * 128]))(gs_, m, tsl),
                 writes=[('gb', gs_)], dma=f'gb{gs_}')
            bv, bg, ba = nb(), nb(), nb()
            for kc in range(4):
                P.op('pe', (lambda bv, kc, m, xs: lambda e: e.matmul(pb[bv][:], lhsT=glu_bf[:, kc, m * 128:(m + 1) * 128], rhs=ysb[xs][:, kc, :],
                                                                    start=(kc == 0), stop=(kc == 3)))(bv, kc, m, xs),
                     reads=['glu', ('ysb', xs)], writes=[('pb', bv)])
            for kc in range(4):
                P.op('pe', (lambda bg, kc, m, xs: lambda e: e.matmul(pb[bg][:], lhsT=glu_bf[:, kc, 1024 + m * 128:1024 + (m + 1) * 128], rhs=ysb[xs][:, kc, :],
                                                                    start=(kc == 0), stop=(kc == 3)))(bg, kc, m, xs),
                     reads=['glu', ('ysb', xs)], writes=[('pb', bg)])
            for kc in range(4):
                P.op('pe', (lambda ba, kc, m, xs: lambda e: e.matmul(pb[ba][:], lhsT=up_bf[:, kc, m * 128:(m + 1) * 128], rhs=ob[xs][:, kc, :],
                                                                    start=(kc == 0), stop=(kc == 3)))(ba, kc, m, xs),
                     reads=['up', ('ob', xs)], writes=[('pb', ba)])
            s = m % 2
            P.op('act', (lambda s, bg: lambda e: e.activation(out=sg[s][:], in_=pb[bg][:], func=AF.Sigmoid))(s, bg),
                 reads=[('pb', bg)], writes=[('sg', s)])
            P.op('dve', (lambda s, bv: lambda e: e.tensor_tensor(out=t1[s][:], in0=pb[bv][:], in1=sg[s][:], op=ALU.mult))(s, bv),
                 reads=[('pb', bv), ('sg', s)], writes=[('t1', s)])
            P.op('pool', (lambda s, gs_: lambda e: e.tensor_tensor(out=t1[s][:], in0=t1[s][:], in1=gb[gs_][:, 0, :], op=ALU.mult))(s, gs_),
                 reads=[('t1', s), ('gb', gs_)], writes=[('t1', s)])
            P.op('dve', (lambda s, ba, gs_: lambda e: e.tensor_tensor(out=t2[s][:], in0=pb[ba][:], in1=gb[gs_][:, 1, :], op=ALU.mult))(s, ba, gs_),
                 reads=[('pb', ba), ('gb', gs_)], writes=[('t2', s)])
            P.op('pool', (lambda s, m: lambda e: e.tensor_tensor(out=mg[:, m, :], in0=t1[s][:], in1=t2[s][:], op=ALU.add))(s, m),
                 reads=[('t1', s), ('t2', s)], writes=[('mg', m)])
        for m in range(8):
            bo = nb()
            for kc in range(8):
                P.op('pe', (lambda bo, kc, m: lambda e: e.matmul(pb[bo][:], lhsT=wo_bf[:, kc, m * 128:(m + 1) * 128], rhs=mg[:, kc, :],
                                                                start=(kc == 0), stop=(kc == 7)))(bo, kc, m),
                     reads=['wo', ('mg', kc)], writes=[('pb', bo)])
            o = oi % 2
            oi += 1
            P.op('dve', (lambda o, bo, m, xs: lambda e: e.scalar_tensor_tensor(out=xo[o][:], in0=pb[bo][:], scalar=g1[:, m:m + 1], in1=xb[xs][:, m, :],
                                                                              op0=ALU.mult, op1=ALU.add))(o, bo, m, xs),
                 reads=[('pb', bo), 'g1', ('xb', xs)], writes=[('xo', o)])
            P.op('sp', (lambda o, m, tsl: lambda e: e.dma_start(out=x1T[m * 128:(m + 1) * 128, tsl], in_=xo[o][:]))(o, m, tsl),
                 reads=[('xo', o)], dma=f'xo{o}')
    if own:
        P.end_phase(final=True)
        return nc
    return {"x1T": x1T}


def layout_C1(inp, l, r, xT_full, ysT_full, oT_full, gT_core):
    b, j = r // 4, r % 4
    f = lambda a: np.ascontiguousarray(a, dtype=np.float32)
    sl = slice(j * T, (j + 1) * T)
    return {
        "xT": f(xT_full[b][:, sl]),
        "ysT": np.ascontiguousarray(ysT_full[b][:, sl]),
        "oT": np.ascontiguousarray(oT_full[b][:, sl]),
        "gT": gT_core,
        "cb": f(inp['c'][b].reshape(8, 128).T),
        "adaw": f(inp['ada_w'][l][:, 2048:3072]),
        "adab": f(inp['ada_b'][l][2048:3072].reshape(8, 128).T),
        "gluw": f(inp['ssm_glu_w'][l]),
        "upw": f(inp['attn_up_w'][l]),
        "wout": f(inp['w_out'][l]),
    }


T = 4096
NE = 16384
NSL = 22
GS = 8
NEG = -1.0e30
GELU_C = 2.0 * math.sqrt(2.0 / math.pi)


def emit_conv(P, utab, vtab, uvbf):
    NCH = 8
    RCH = NE // NCH
    for ci in range(NCH):
        rs_ = slice(ci * RCH, (ci + 1) * RCH)
        P.op('pool', (lambda rs_: lambda e: e.dma_start(out=uvbf[rs_, 0:1024], in_=utab[rs_, :]))(rs_), writes=[('uvbf', ci, 0)], dma='conv')
        P.op('pool', (lambda rs_: lambda e: e.dma_start(out=uvbf[rs_, 1024:2048], in_=vtab[rs_, :]))(rs_), writes=[('uvbf', ci, 1)], dma='conv')


def conv_tensors(P, pre):
    D = mkD(P.nc, pre, None)
    return {"utab": D("utab", [NE, 1024], F32, "ExternalInput"), "vtab": D("vtab", [NE, 1024], F32, "ExternalInput"),
            "uvbf": D("uvbf", [NE, 2048], BF16, "Internal")}


def build_C2(ntile=32, P=None, pre="", ext=None, conv_done=False):
    own = P is None
    if own:
        P = Prog(bass.Bass("TRN2", target_bir_lowering=False))
    nc = P.nc
    D = mkD(nc, pre, ext)
    x1T = D("x1T", [1024, T], F32, "ExternalInput")
    cb = D("cb", [128, 8], F32, "ExternalInput")
    adaw = D("adaw", [1024, 3072], F32, "ExternalInput")
    adab = D("adab", [128, 16], F32, "ExternalInput")
    g2b = D("g2b", [1, 1024], F32, "ExternalInput")
    n2w = D("n2w", [128, 8], F32, "ExternalInput")
    wq = D("wq", [1024, 2048], F32, "ExternalInput")
    kTd = D("kT", [128, 2, 128], F32, "ExternalInput")
    utab = D("utab", [NE, 1024], F32, "ExternalInput")
    vtab = D("vtab", [NE, 1024], F32, "ExternalInput")
    x2 = D("x2", [T, 1024], F32, "ExternalOutput")
    x2T = D("x2T", [1024, T], F32, "ExternalOutput")
    uvbf = D("uvbf", [NE, 2048], BF16, "Internal")

    wq_bf = P.sb("wq_bf", [128, 8, 2048], BF16)
    c_sb = P.sb("c_sb", [128, 8], F32)
    ca_sb = P.sb("ca_sb", [128, 8], F32)
    ca_bc = P.sb("ca_bc", [128, 8, 128], F32)
    adab_sb = P.sb("adab_sb", [128, 16], F32)
    n2w_sb = P.sb("n2w_sb", [128, 8], F32)
    kT_sb = P.sb("kT_sb", [128, 2, 128], F32)
    modT = P.sb("modT", [128, 16], F32)
    wmod = P.sb("wmod", [128, 8], F32)
    g2bc = P.sb("g2bc", [128, 1024], F32)
    ones = P.sb("ones", [128, 128], F32)
    epsb = P.sb("epsb", [128, 1], F32)
    ident = P.sb("ident", [128, 128], F32)
    ident_bf = P.sb("ident_bf", [128, 128], BF16)
    iot = P.sb("iot", [128, 128], F32)
    xb = [P.sb(f"xb{i}", [128, 8, 128], F32) for i in range(2)]
    sq = [P.sb(f"sq{i}", [128, 128], F32) for i in range(2)]
    rstd = P.sb("rstd", [128, 128], F32)
    tmp = [P.sb(f"tmp{i}", [128, 128], F32) for i in range(2)]
    h2T = P.sb("h2T", [128, 8, 128], F32)
    h2Tbf = P.sb("h2Tbf", [128, 8, 128], BF16)
    h2_tok = [P.sb(f"h2_tok{i}", [128, 1024], F32) for i in range(2)]
    x1_tok = [P.sb(f"x1_tok{i}", [128, 1024], F32) for i in range(2)]
    qc = [P.sb(f"qc{i}", [128, 128], F32) for i in range(3)]
    s_sb = P.sb("s_sb", [128, 16, 128], F32)
    s2 = P.sb("s2", [128, 128], F32)
    v16 = P.sb("v16", [128, 16, 16], F32)
    i16 = P.sb("i16", [128, 16, 16], U32)
    if32 = P.sb("if32", [128, 16, 16], F32)
    i1s = P.sb("i1s", [128, 8, 16], F32)
    cand = [P.sb(f"cand{i}", [128, 16, 16], F32) for i in range(2)]
    cand2 = P.sb("cand2", [128, 256], F32)
    sc = P.sb("sc", [128, 8, 16], F32)
    cpos = P.sb("cpos", [128, 8, 16], U32)
    cia = P.sb("cia", [128, 8, 16], U32)
    cib = P.sb("cib", [128, 8, 16], U32)
    caf = P.sb("caf", [128, 8, 16], F32)
    cbf = P.sb("cbf", [128, 8, 16], F32)
    e1f = P.sb("e1f", [128, 8, 16], F32)
    e2f = P.sb("e2f", [128, 8, 16], F32)
    c4u = P.sb("c4u", [128, 1], U32)
    c15u = P.sb("c15u", [128, 1], U32)
    iot16 = P.sb("iot16", [128, 16], F32)
    ef = P.sb("ef", [128, 128], F32)
    idx = [P.sb(f"idx{i}", [128, 128], I32) for i in range(2)]
    gex = P.sb("gex", [128, 8, 16], F32)
    gsum = P.sb("gsum", [128, 8], F32)
    gw = [P.sb(f"gw{i}", [128, 128], F32) for i in range(2)]
    act = P.sb("act", [128, 128], F32)
    gt = P.sb("gt", [128, 128], F32)
    gsg = P.sb("gsg", [128, 128], F32)
    wts = P.sb("wts", [128, 128], F32)
    uv = [P.sb(f"uv{i}", [128, 2048], BF16) for i in range(NSL)]
    bigk = [uv[k][:].bitcast(F32)[:, 0:512] for k in range(8)]
    oh = s_sb[:].rearrange("p a b -> p (a b)").rearrange("p (h k a) -> p h k a", h=8, k=16)
    dg = [P.sb(f"dg{i}", [128, 128], BF16) for i in range(NSL)]
    junk = P.sb("junk", [128, 1024], BF16)
    xo = P.sb("xo", [128, 1024], F32)
    xoT = P.sb("xoT", [128, 8, 128], F32)
    pb = [P.ps(f"pb{i}", [128, 512]) for i in range(4)]
    po = [[P.ps(f"po{i}_{h}", [128, 512]) for h in range(2)] for i in range(2)]
    pm = pb[0]
    bank = [0]

    def nb():
        bank[0] = (bank[0] + 1) % 4
        return bank[0]

    NCH = 8
    RCH = NE // NCH
    if not conv_done:
        emit_conv(P, utab, vtab, uvbf)
    alluv = [('uvbf', ci, k) for ci in range(NCH) for k in range(2)]

    P.op('pool', lambda e: e.memset(ones[:], 1.0), writes=['ones'])
    P.op('pool', lambda e: e.memset(c4u[:], 4), writes=['c4u'])
    P.op('pool', lambda e: e.memset(c15u[:], 15), writes=['c15u'])
    P.op('pool', lambda e: e.iota(iot16[:], pattern=[[1, 16]], base=0, channel_multiplier=0, allow_small_or_imprecise_dtypes=True), writes=['iot16'])
    P.op('pool', lambda e: e.memset(epsb[:], 1e-6), writes=['epsb'])
    P.op('pool', lambda e: e.iota(iot[:], pattern=[[1, 128]], base=0, channel_multiplier=-1, allow_small_or_imprecise_dtypes=True), writes=['iot'])
    P.op('dve', lambda e: e.tensor_single_scalar(out=ident[:], in_=iot[:], scalar=0.0, op=ALU.is_equal), reads=['iot'], writes=['ident'])
    P.op('dve', lambda e: e.tensor_copy(out=ident_bf[:], in_=ident[:]), reads=['ident'], writes=['ident_bf'])
    for nm, dst, src in (('c', c_sb, cb), ('adab', adab_sb, adab), ('n2w', n2w_sb, n2w), ('kT', kT_sb, kTd)):
        P.op('sp', (lambda dst, src: lambda e: e.dma_start(out=dst[:], in_=src))(dst, src), writes=[nm], dma='sm_' + nm)
    P.op('act', lambda e: e.activation(out=ca_sb[:], in_=c_sb[:], func=AF.Silu), reads=['c'], writes=['ca'])
    for kc in range(8):
        P.op('dve', (lambda kc: lambda e: e.tensor_copy(out=ca_bc[:, kc, :], in_=ca_sb[:, kc:kc + 1].to_broadcast([128, 128])))(kc),
             reads=['ca'], writes=['ca_bc'])
    for q4 in range(4):
        for kc in range(8):
            P.op('sp', (lambda q4, kc: lambda e: e.dma_start(out=bigk[kc], in_=adaw[kc * 128:(kc + 1) * 128, q4 * 512:(q4 + 1) * 512]))(q4, kc),
                 writes=[('uv', kc)], dma=f'uv{kc}')
        for mm in range(4):
            m = q4 * 4 + mm
            for kc in range(8):
                P.op('pe', (lambda kc, m, mm: lambda e: e.matmul(pm[:, m:m + 1], lhsT=bigk[kc][:, mm * 128:(mm + 1) * 128],
                                                              rhs=ca_sb[:, kc:kc + 1], start=(kc == 0), stop=(kc == 7)))(kc, m, mm),
                     reads=[('uv', kc), 'ca'], writes=[('pb', 0)])
    P.op('dve', lambda e: e.tensor_tensor(out=modT[:], in0=pm[:, 0:16], in1=adab_sb[:], op=ALU.add), reads=[('pb', 0), 'adab'], writes=['modT'])
    P.op('dve', lambda e: e.scalar_tensor_tensor(out=wmod[:], in0=modT[:, 8:16], scalar=1.0, in1=n2w_sb[:], op0=ALU.add, op1=ALU.mult),
         reads=['modT', 'n2w'], writes=['wmod'])
    P.op('sp', lambda e: e.dma_start(out=xo[:], in_=g2b.partition_broadcast(128)), writes=['xo'], dma='xo')
    for q2 in range(2):
        bq = 1 + q2
        for kc in range(8):
            P.op('sp', (lambda q2, kc: lambda e: e.dma_start(out=bigk[kc], in_=adaw[kc * 128:(kc + 1) * 128, 2048 + q2 * 512:2048 + (q2 + 1) * 512]))(q2, kc),
                 writes=[('uv', kc)], dma=f'uv{kc}')
        for kc in range(8):
            P.op('pe', (lambda kc, bq: lambda e: e.matmul(pb[bq][:], lhsT=ca_bc[:, kc, :], rhs=bigk[kc], start=(kc == 0), stop=(kc == 7)))(kc, bq),
                 reads=[('uv', kc), 'ca_bc'], writes=[('pb', bq)])
        P.op('dve', (lambda q2, bq: lambda e: e.tensor_tensor(out=g2bc[:, q2 * 512:(q2 + 1) * 512], in0=pb[bq][:], in1=xo[:, q2 * 512:(q2 + 1) * 512], op=ALU.add))(q2, bq),
             reads=[('pb', bq), 'xo'], writes=['g2bc'])
    for kc in range(8):
        for hh in range(2):
            P.op('pool', (lambda kc, hh: lambda e: e.dma_start(out=wq_bf[:, kc, hh * 1024:(hh + 1) * 1024], in_=wq[kc * 128:(kc + 1) * 128, hh * 1024:(hh + 1) * 1024]))(kc, hh),
                 writes=['wq'], dma='wqc')

    def stage_S(t):
        th = []
        A = th.append
        xs = t % 2
        tsl = slice(t * 128, (t + 1) * 128)
        h2t, x1t, idxt, gwt = h2_tok[xs], x1_tok[xs], idx[xs], gw[xs]
        A(lambda: P.op('sp', lambda e: e.dma_start(out=xb[xs][:], in_=x1T[:, tsl].rearrange("(kc p) t -> p kc t", p=128)),
                       writes=[('xb', xs)], dma=f'xb{xs}'))
        bn = nb()
        for kc in range(8):
            s = kc % 2
            A((lambda kc, s: lambda: P.op('act', lambda e: e.activation(out=sq[s][:], in_=xb[xs][:, kc, :], func=AF.Square),
                                          reads=[('xb', xs)], writes=[('sq', s)]))(kc, s))
            A((lambda kc, s: lambda: P.op('pe', lambda e: e.matmul(pb[bn][:, 0:128], lhsT=ones[:], rhs=sq[s][:], start=(kc == 0), stop=(kc == 7)),
                                          reads=[('sq', s), 'ones'], writes=[('pb', bn)]))(kc, s))
        A(lambda: P.op('act', lambda e: e.activation(out=rstd[:], in_=pb[bn][:, 0:128], func=AF.Sqrt, bias=epsb[:], scale=1.0 / 1024.0),
                       reads=[('pb', bn), 'epsb'], writes=['rstd']))
        A(lambda: P.op('dve', lambda e: e.reciprocal(out=rstd[:], in_=rstd[:]), reads=['rstd'], writes=['rstd']))
        for kc in range(8):
            s = kc % 2
            A((lambda kc, s: lambda: P.op('dve', lambda e: e.tensor_tensor(out=tmp[s][:], in0=xb[xs][:, kc, :], in1=rstd[:], op=ALU.mult),
                                          reads=[('xb', xs), 'rstd'], writes=[('tmp', s)]))(kc, s))
            A((lambda kc, s: lambda: P.op('act', lambda e: e.activation(out=h2T[:, kc, :], in_=tmp[s][:], func=AF.Identity,
                                                                       bias=modT[:, kc:kc + 1], scale=wmod[:, kc:kc + 1]),
                                          reads=[('tmp', s), 'modT', 'wmod'], writes=[('h2T', kc)]))(kc, s))
            A((lambda kc: lambda: P.op('pool', lambda e: e.tensor_copy(out=h2Tbf[:, kc, :], in_=h2T[:, kc, :]), reads=[('h2T', kc)], writes=[('h2Tbf', kc)]))(kc))
        for src, srcn, dst, dstn in ((h2T, 'h2T', h2t, ('h2_tok', xs)), (xb[xs], ('xb', xs), x1t, ('x1_tok', xs))):
            for half in range(2):
                bt = nb()
                for k4 in range(4):
                    kc = half * 4 + k4
                    rd = [(srcn, kc)] if srcn == 'h2T' else [srcn]
                    A((lambda src, kc, k4, bt, rd: lambda: P.op('pe', lambda e: e.transpose(out=pb[bt][:, k4 * 128:(k4 + 1) * 128], in_=src[:, kc, :], identity=ident[:]),
                                                               reads=rd + ['ident'], writes=[('pb', bt)]))(src, kc, k4, bt, rd))
                A((lambda dst, dstn, half, bt: lambda: P.op('act', lambda e: e.copy(out=dst[:, half * 512:(half + 1) * 512], in_=pb[bt][:]),
                                                           reads=[('pb', bt)], writes=[dstn]))(dst, dstn, half, bt))
        bs = [None]
        for hs in range(16):
            side = hs % 2
            bq = nb()
            for kc in range(8):
                A((lambda bq, kc, hs: lambda: P.op('pe', lambda e: e.matmul(pb[bq][:, 0:128], lhsT=wq_bf[:, kc, hs * 128:(hs + 1) * 128], rhs=h2Tbf[:, kc, :],
                                                                          start=(kc == 0), stop=(kc == 7)),
                                                  reads=['wq', ('h2Tbf', kc)], writes=[('pb', bq)]))(bq, kc, hs))
            q_ = hs % 3
            A((lambda q_, bq: lambda: P.op('act', lambda e: e.copy(out=qc[q_][:], in_=pb[bq][:, 0:128]), reads=[('pb', bq)], writes=[('qc', q_)]))(q_, bq))
            if hs % 4 == 0:
                bs[0] = nb()
            bsv = bs[0]
            A((lambda bsv, q_, hs, side: lambda: P.op('pe', lambda e: e.matmul(pb[bsv][:, (hs % 4) * 128:(hs % 4 + 1) * 128], lhsT=qc[q_][:], rhs=kT_sb[:, side, :],
                                                                             start=True, stop=True),
                                                     reads=[('qc', q_), 'kT'], writes=[('pb', bsv)]))(bsv, q_, hs, side))
            if hs % 4 == 3:
                g4 = hs // 4
                A((lambda bsv, g4: lambda: P.op('act', lambda e: e.copy(out=s_sb[:, g4 * 4:(g4 + 1) * 4, :], in_=pb[bsv][:].rearrange("p (a b) -> p a b", a=4)),
                                               reads=[('pb', bsv)], writes=[('s_sb', g4)]))(bsv, g4))
        for hs in range(16):
            rs_ = [('s_sb', hs // 4)]
            A((lambda hs, rs_: lambda: P.op('dve', lambda e: e.max(out=v16[:, hs, 0:8], in_=s_sb[:, hs, :]), reads=rs_, writes=[('v16', hs)]))(hs, rs_))
            A((lambda hs, rs_: lambda: P.op('dve', lambda e: e.max_index(out=i16[:, hs, 0:8], in_max=v16[:, hs, 0:8], in_values=s_sb[:, hs, :]),
                                           reads=rs_ + [('v16', hs)], writes=[('i16', hs)]))(hs, rs_))
            A((lambda hs, rs_: lambda: P.op('dve', lambda e: e.match_replace(out=s2[:], in_to_replace=v16[:, hs, 0:8], in_values=s_sb[:, hs, :], imm_value=NEG),
                                           reads=rs_ + [('v16', hs)], writes=['s2']))(hs, rs_))
            A((lambda hs: lambda: P.op('dve', lambda e: e.max(out=v16[:, hs, 8:16], in_=s2[:]), reads=['s2'], writes=[('v16', hs)]))(hs))
            A((lambda hs: lambda: P.op('dve', lambda e: e.max_index(out=i16[:, hs, 8:16], in_max=v16[:, hs, 8:16], in_values=s2[:]),
                                      reads=['s2', ('v16', hs)], writes=[('i16', hs)]))(hs))
        allv = [('v16', hs) for hs in range(16)]
        alli = [('i16', hs) for hs in range(16)]
        A(lambda: P.op('dve', lambda e: e.tensor_copy(out=if32[:], in_=i16[:]), reads=alli, writes=['if32']))
        A(lambda: P.op('dve', lambda e: e.tensor_scalar_mul(out=i1s[:], in0=if32[:].rearrange("p (h s) k -> p h s k", s=2)[:, :, 0, :], scalar1=128.0),
                       reads=['if32'], writes=['i1s']))
        for h in range(8):
            cs = h % 2
            cf = cand[cs][:].rearrange("p a b -> p (a b)")
            A((lambda h, cs: lambda: P.op('dve', lambda e: e.tensor_tensor(out=cand[cs][:], in0=v16[:, 2 * h, :].unsqueeze(2).to_broadcast([128, 16, 16]),
                                                                          in1=v16[:, 2 * h + 1, :].unsqueeze(1).to_broadcast([128, 16, 16]), op=ALU.add),
                                         reads=allv, writes=[('cand', cs)]))(h, cs))
            A((lambda h, cs, cf: lambda: P.op('dve', lambda e: e.max(out=sc[:, h, 0:8], in_=cf), reads=[('cand', cs)], writes=[('sc', h)]))(h, cs, cf))
            A((lambda h, cs, cf: lambda: P.op('dve', lambda e: e.max_index(out=cpos[:, h, 0:8], in_max=sc[:, h, 0:8], in_values=cf),
                                             reads=[('cand', cs), ('sc', h)], writes=[('ci', h)]))(h, cs, cf))
            A((lambda h, cs, cf: lambda: P.op('dve', lambda e: e.match_replace(out=cand2[:], in_to_replace=sc[:, h, 0:8], in_values=cf, imm_value=NEG),
                                             reads=[('cand', cs), ('sc', h)], writes=['cand2']))(h, cs, cf))
            A((lambda h: lambda: P.op('dve', lambda e: e.max(out=sc[:, h, 8:16], in_=cand2[:]), reads=['cand2'], writes=[('sc', h)]))(h))
            A((lambda h: lambda: P.op('dve', lambda e: e.max_index(out=cpos[:, h, 8:16], in_max=sc[:, h, 8:16], in_values=cand2[:]),
                                     reads=['cand2', ('sc', h)], writes=[('ci', h)]))(h))
        allsc = [('sc', h) for h in range(8)]
        ohr = [('s_sb', g4) for g4 in range(4)]
        allci = [('ci', h) for h in range(8)]
        A(lambda: P.op('dve', lambda e: e.tensor_single_scalar(out=cia[:], in_=cpos[:], scalar=c4u[:, 0:1], op=ALU.logical_shift_right), reads=allci + ['c4u'], writes=['cia']))
        A(lambda: P.op('dve', lambda e: e.tensor_single_scalar(out=cib[:], in_=cpos[:], scalar=c15u[:, 0:1], op=ALU.bitwise_and), reads=allci + ['c15u'], writes=['cib']))
        A(lambda: P.op('dve', lambda e: e.tensor_copy(out=caf[:], in_=cia[:]), reads=['cia'], writes=['caf']))
        A(lambda: P.op('dve', lambda e: e.tensor_copy(out=cbf[:], in_=cib[:]), reads=['cib'], writes=['cbf']))
        if3 = if32[:].rearrange("p (h s) k -> p h s k", s=2)
        for nm, src, tab in (('a', caf, i1s[:]), ('b', cbf, if3[:, :, 1, :])):
            A((lambda nm, src: lambda: P.op('dve', lambda e: e.tensor_tensor(out=oh[:], in0=src[:].unsqueeze(3).to_broadcast([128, 8, 16, 16]),
                                                                          in1=iot16[:].unsqueeze(1).unsqueeze(1).to_broadcast([128, 8, 16, 16]), op=ALU.is_equal),
                                           reads=['caf', 'cbf', 'iot16'], writes=ohr))(nm, src))
            A((lambda nm, tab: lambda: P.op('dve', lambda e: e.tensor_tensor(out=oh[:], in0=oh[:], in1=tab.unsqueeze(2).to_broadcast([128, 8, 16, 16]), op=ALU.mult),
                                           reads=ohr + ['i1s', 'if32'], writes=ohr))(nm, tab))
            A((lambda nm: lambda: P.op('dve', lambda e: e.tensor_reduce(out=(e1f if nm == 'a' else e2f)[:], in_=oh[:], axis=AX.X, op=ALU.add),
                                      reads=ohr, writes=['e' + nm]))(nm))
        A(lambda: P.op('dve', lambda e: e.tensor_tensor(out=ef[:].rearrange("p (h k) -> p h k", h=8), in0=e1f[:], in1=e2f[:], op=ALU.add), reads=['ea', 'eb'], writes=['ef']))
        A(lambda: P.op('dve', lambda e: e.tensor_scalar(out=ef[:], in0=ef[:], scalar1=float(NE - 1), scalar2=0.0, op0=ALU.min, op1=ALU.max), reads=['ef'], writes=['ef']))
        A(lambda: P.op('dve', lambda e: e.tensor_copy(out=idxt[:], in_=ef[:]), reads=['ef'], writes=[('idx', xs)]))
        A(lambda: P.op('dve', lambda e: e.tensor_tensor(out=gex[:], in0=sc[:], in1=sc[:, :, 0:1].to_broadcast([128, 8, 16]), op=ALU.subtract),
                       reads=allsc, writes=['gex']))
        A(lambda: P.op('act', lambda e: e.activation(out=gex[:], in_=gex[:], func=AF.Exp), reads=['gex'], writes=['gex']))
        A(lambda: P.op('dve', lambda e: e.tensor_reduce(out=gsum[:], in_=gex[:], axis=AX.X, op=ALU.add), reads=['gex'], writes=['gsum']))
        A(lambda: P.op('dve', lambda e: e.reciprocal(out=gsum[:], in_=gsum[:]), reads=['gsum'], writes=['gsum']))
        A(lambda: P.op('dve', lambda e: e.tensor_tensor(out=gwt[:].rearrange("p (h k) -> p h k", h=8), in0=gex[:], in1=gsum[:].unsqueeze(2).to_broadcast([128, 8, 16]), op=ALU.mult),
                       reads=['gex', 'gsum'], writes=[('gw', xs)]))
        return th

    gctr = [0]

    def stage_UW(t, pending):
        xs = t % 2
        h2t, x1t, idxt, gwt = h2_tok[xs], x1_tok[xs], idx[xs], gw[xs]
        pot = po[xs]
        NG = 128 // GS
        per = (len(pending) + NG - 1) // NG if pending else 0
        prev_post = [None]
        for g in range(NG):
            js = list(range(g * GS, (g + 1) * GS))
            gsl = slice(g * GS, (g + 1) * GS)
            slots = {}
            for j in js:
                sl_ = gctr[0] % NSL
                gctr[0] += 1
                slots[j] = sl_
                P.op('pool', (lambda sl_, j: lambda e: e.indirect_dma_start(out=uv[sl_][:], out_offset=None, in_=uvbf,
                                                                          in_offset=bass.IndirectOffsetOnAxis(ap=idxt[:, j:j + 1], axis=0)))(sl_, j),
                     reads=[('idx', xs)] + alluv, writes=[('uv', sl_)], dma=f'uv{sl_}')
            for j in js:
                sl_ = slots[j]
                P.op('dve', (lambda sl_, j: lambda e: e.scalar_tensor_tensor(out=junk[:], in0=uv[sl_][:, 0:1024], scalar=1.0, in1=h2t[:], op0=ALU.mult, op1=ALU.mult,
                                                                           accum_out=act[:, j:j + 1]))(sl_, j),
                     reads=[('uv', sl_), ('h2_tok', xs)], writes=[('act', g)])
            P.op('dve', (lambda gsl: lambda e: e.tensor_tensor(out=gt[:, gsl], in0=act[:, gsl], in1=act[:, gsl], op=ALU.mult))(gsl), reads=[('act', g)], writes=[('gt', g)])
            P.op('dve', (lambda gsl: lambda e: e.tensor_scalar(out=gt[:, gsl], in0=gt[:, gsl], scalar1=0.044715, scalar2=1.0, op0=ALU.mult, op1=ALU.add))(gsl),
                 reads=[('gt', g)], writes=[('gt', g)])
            P.op('dve', (lambda gsl: lambda e: e.tensor_tensor(out=gt[:, gsl], in0=gt[:, gsl], in1=act[:, gsl], op=ALU.mult))(gsl), reads=[('gt', g), ('act', g)], writes=[('gt', g)])
            P.op('act', (lambda gsl: lambda e: e.activation(out=gsg[:, gsl], in_=gt[:, gsl], func=AF.Sigmoid, scale=GELU_C))(gsl), reads=[('gt', g)], writes=[('gsg', g)])

            def post(g=g, gsl=gsl, js=js, slots=slots):
                P.op('dve', lambda e: e.tensor_tensor(out=gt[:, gsl], in0=gsg[:, gsl], in1=act[:, gsl], op=ALU.mult), reads=[('gsg', g), ('act', g)], writes=[('gt', g)])
                P.op('dve', lambda e: e.tensor_tensor(out=wts[:, gsl], in0=gt[:, gsl], in1=gwt[:, gsl], op=ALU.mult), reads=[('gt', g), ('gw', xs)], writes=[('wts', g)])
                for j in js:
                    sl_ = slots[j]
                    P.op('act', (lambda sl_, j: lambda e: e.activation(out=dg[sl_][:], in_=ident_bf[:], func=AF.Identity, scale=wts[:, j:j + 1]))(sl_, j),
                         reads=[('wts', g), 'ident_bf'], writes=[('dg', sl_)])
                    for half in range(2):
                        P.op('pe', (lambda sl_, j, half: lambda e: e.matmul(pot[half][:], lhsT=dg[sl_][:], rhs=uv[sl_][:, 1024 + half * 512:1024 + (half + 1) * 512],
                                                                          start=(j == 0), stop=(j == 127)))(sl_, j, half),
                             reads=[('dg', sl_), ('uv', sl_)], writes=[('po', xs, half)])
            if prev_post[0] is not None:
                prev_post[0]()
            prev_post[0] = post
            if pending:
                for _ in range(per):
                    if pending:
                        pending.pop(0)()
        prev_post[0]()
        while pending:
            pending.pop(0)()
        for half in range(2):
            hs_ = slice(half * 512, (half + 1) * 512)
            P.op('dve', (lambda half, hs_: lambda e: e.tensor_tensor(out=xo[:, hs_], in0=pot[half][:], in1=g2bc[:, hs_], op=ALU.mult))(half, hs_),
                 reads=[('po', xs, half), 'g2bc'], writes=['xo'])
            P.op('pool', (lambda hs_: lambda e: e.tensor_tensor(out=xo[:, hs_], in0=xo[:, hs_], in1=x1t[:, hs_], op=ALU.add))(hs_),
                 reads=['xo', ('x1_tok', xs)], writes=['xo'])
        P.op('sp', lambda e: e.dma_start(out=x2[t * 128:(t + 1) * 128, :], in_=xo[:]), reads=['xo'], dma='xo')
        for half in range(2):
            bt = nb()
            for k4 in range(4):
                kc = half * 4 + k4
                P.op('pe', (lambda kc, k4, bt: lambda e: e.transpose(out=pb[bt][:, k4 * 128:(k4 + 1) * 128], in_=xo[:, kc * 128:(kc + 1) * 128], identity=ident[:]))(kc, k4, bt),
                     reads=['xo', 'ident'], writes=[('pb', bt)])
            P.op('act', (lambda half, bt: lambda e: e.copy(out=xoT[:, half * 4:(half + 1) * 4, :], in_=pb[bt][:].rearrange("p (a b) -> p a b", a=4)))(half, bt),
                 reads=[('pb', bt)], writes=['xoT'])
        P.op('sp', lambda e: e.dma_start(out=x2T[:, t * 128:(t + 1) * 128].rearrange("(kc p) t -> p kc t", p=128), in_=xoT[:]), reads=['xoT'], dma='xoT')

    for thunk in stage_S(0):
        thunk()
    for t in range(ntile):
        pending = stage_S(t + 1) if t + 1 < ntile else []
        stage_UW(t, pending)
    if own:
        P.end_phase(final=True)
        return nc
    return {"x2T": x2T}


def layout_C2(inp, l, r, x1T_core):
    b, j = r // 4, r % 4
    f = lambda a: np.ascontiguousarray(a, dtype=np.float32)
    return {
        "x1T": None if x1T_core is None else f(x1T_core),
        "cb": f(inp['c'][b].reshape(8, 128).T),
        "adaw": f(inp['ada_w'][l][:, 3072:6144]),
        "adab": f(inp['ada_b'][l][3072:5120].reshape(16, 128).T),
        "g2b": f(inp['ada_b'][l][5120:6144].reshape(1, 1024)),
        "n2w": f(inp['norm2_w'][l].reshape(8, 128).T),
        "wq": f(inp['peer_wq'][l]),
        "kT": f(np.stack([inp['peer_k1'][l].T, inp['peer_k2'][l].T], axis=1)),
        "utab": f(inp['peer_u'][l]),
        "vtab": f(inp['peer_v'][l]),
    }


_CACHE = {}


def _prog(key, fn):
    if key not in _CACHE:
        _CACHE[key] = fn()
    return _CACHE[key]


def _run(nc, in_maps):
    res = run_bass_kernel_spmd(nc, in_maps, core_ids=list(range(8)))
    return res.results


def _pfx(d, pre):
    return {pre + k: v for k, v in d.items()}


def _build_LB(l):
    P = Prog(bass.Bass("TRN2", target_bir_lowering=False))
    build_B1(P=P, pre="b1_")
    P.end_phase()
    build_B2(l, P=P, pre="b2_")
    P.end_phase(final=True)
    return P.nc


def _build_LC(with_a):
    P = Prog(bass.Bass("TRN2", target_bir_lowering=False))
    ct = conv_tensors(P, "c2_")
    emit_conv(P, ct["utab"], ct["vtab"], ct["uvbf"])
    o1 = build_C1(P=P, pre="c1_")
    P.end_phase()
    o2 = build_C2(P=P, pre="c2_", ext={"x1T": o1["x1T"], **ct}, conv_done=True)
    if with_a:
        P.end_phase()
        build_A(P=P, pre="a_", ext={"xT": o2["x2T"]})
    P.end_phase(final=True)
    return P.nc


def kernel(**inp):
    inp = {k: np.asarray(v) for k, v in inp.items()}
    x = inp['x']
    xT_full = [np.ascontiguousarray(x[b].T) for b in range(2)]
    ra = _run(_prog('A', build_A), [layout_A(inp, 0, r, xT_full) for r in range(8)])
    apre = ''
    x2 = None
    for l in range(2):
        cat = lambda key, b: np.concatenate([ra[b * 4 + j][apre + key] for j in range(4)], axis=1)
        uT_full = [cat('uT', b) for b in range(2)]
        qT_full = [cat('qT', b) for b in range(2)]
        kT_full = [cat('kT', b) for b in range(2)]
        vT_full = [cat('vT', b) for b in range(2)]
        rb = _run(_prog(('LB', l), lambda: _build_LB(l)),
                  [{**_pfx(layout_B1(inp, l, r, uT_full), 'b1_'), **_pfx(layout_B2(inp, l, r, qT_full, kT_full, vT_full), 'b2_')} for r in range(8)])
        ysT_full = [np.concatenate([rb[b * 4 + j]['b1_yT'] for j in range(4)], axis=0) for b in range(2)]
        oT_full = [np.concatenate([rb[b * 4 + j]['b2_oT'] for j in range(4)], axis=0) for b in range(2)]
        with_a = (l == 0)
        maps = []
        for r in range(8):
            m = _pfx(layout_C1(inp, l, r, xT_full, ysT_full, oT_full, ra[r][apre + 'gT']), 'c1_')
            c2 = layout_C2(inp, l, r, None)
            del c2['x1T']
            m.update(_pfx(c2, 'c2_'))
            if with_a:
                a = layout_A(inp, l + 1, r, None)
                del a['xT']
                m.update(_pfx(a, 'a_'))
            maps.append(m)
        rc = _run(_prog(('LC', with_a), lambda: _build_LC(with_a)), maps)
        x2 = [np.concatenate([rc[b * 4 + j]['c2_x2'] for j in range(4)], axis=0) for b in range(2)]
        xT_full = [np.concatenate([rc[b * 4 + j]['c2_x2T'] for j in range(4)], axis=1) for b in range(2)]
        ra, apre = rc, 'a_'
    return np.stack(x2, axis=0).astype(np.float32)
```
